# Optimizing a Trainium2 kernel written in Bass

```python
import jax, jax.numpy as jnp
from jax import lax
import numpy as np

D_MODEL = 1024
BATCH = 8
SEQ = 2048
DEPTH = 2

CONV_A_CH = 512
CONV_A_WIDTH = 31
CONV_A_LN_EPS = 1e-5
RWKV_HEADS = 8
RWKV_HEAD = 64
RWKV_DIM = RWKV_HEADS * RWKV_HEAD
LORA_W = 64
LORA_A = 64
LORA_G = 128
RWKV_GN_EPS = 64e-5
LRU_DIM = 1024
LRU_HEADS = 8
LRU_BLOCK = LRU_DIM // LRU_HEADS
LRU_CONV = 4
LRU_C = 8.0
N_EXPERTS = 64
TOP_K = 8
N_GROUPS = 8
TOPK_GROUPS = 4
EXPERT_FF = 256
SHARED_FF = 256
ROUTED_SCALE = 2.5
MOE_BLOCK = 128
NORM_EPS = 1e-6
N_BRANCH = 3
N_MOD = 6

COLS_A = 2 * CONV_A_CH
COLS_B = 3 * RWKV_DIM + LORA_W + LORA_A + LORA_G
COLS_C = 2 * LRU_DIM
COLS_G = N_BRANCH * D_MODEL
IN_COLS = COLS_A + COLS_B + COLS_C + COLS_G
MIX_SPLITS = (COLS_A, COLS_A + COLS_B, COLS_A + COLS_B + COLS_C)
RWKV_SPLITS = (RWKV_DIM, 2 * RWKV_DIM, 3 * RWKV_DIM, 3 * RWKV_DIM + LORA_W,
               3 * RWKV_DIM + LORA_W + LORA_A)

kernel_name = 'adaln_hybrid_conv_rwkv7_rglru_moe'


def rmsnorm(x, g):
    xf = x.astype(jnp.float32)
    y = xf * lax.rsqrt(jnp.mean(xf * xf, axis=-1, keepdims=True) + NORM_EPS)
    return (y * g.astype(jnp.float32)).astype(x.dtype)


def layernorm(x, g, b, eps):
    xf = x.astype(jnp.float32)
    mu = jnp.mean(xf, axis=-1, keepdims=True)
    var = jnp.mean(jnp.square(xf - mu), axis=-1, keepdims=True)
    y = (xf - mu) * lax.rsqrt(var + eps)
    return (y * g.astype(jnp.float32) + b.astype(jnp.float32)).astype(x.dtype)


def causal_dwconv(x, w, b):
    width, ch = w.shape
    y = lax.conv_general_dilated(
        x, w[:, None, :].astype(x.dtype), window_strides=(1,), padding=[(width - 1, 0)],
        dimension_numbers=('NWC', 'WIO', 'NWC'), feature_group_count=ch)
    return y + b


def token_shift(t):
    return jnp.pad(t, ((0, 0), (1, 0), (0, 0)))[:, :-1]


def conformer_conv(p, conv_w, conv_b, ln_g, ln_b, proj):
    u = p[..., :CONV_A_CH] * jax.nn.sigmoid(p[..., CONV_A_CH:])
    u = causal_dwconv(u, conv_w, conv_b)
    u = layernorm(u, ln_g, ln_b, CONV_A_LN_EPS)
    return jax.nn.silu(u) @ proj


def rwkv7_time_mix(p, mu, w0, w_up, a0, a_up, g_up, k_k, k_a, r_k, gn_g, gn_b, proj):
    bsz, seq, _ = p.shape
    p = p + (token_shift(p) - p) * mu
    r, k, v, xw, xa, xg = jnp.split(p, RWKV_SPLITS, axis=-1)
    w = -jax.nn.softplus(-(w0 + jnp.tanh(xw) @ w_up)) - 0.5
    decay = jnp.exp(-jnp.exp(w.astype(jnp.float32)))
    a = jax.nn.sigmoid(a0 + xa @ a_up)
    g = jax.nn.sigmoid(xg) @ g_up
    heads = lambda t: t.reshape(bsz, seq, RWKV_HEADS, RWKV_HEAD).astype(jnp.float32)
    kk = heads(k * k_k)
    kk = kk / jnp.maximum(jnp.sqrt(jnp.sum(kk * kk, axis=-1, keepdims=True)), 1e-12)
    k = k * (1.0 + (a - 1.0) * k_a)
    rh, wh, kh, vh, ah = heads(r), heads(decay), heads(k), heads(v), heads(a)
    seq_major = lambda t: jnp.transpose(t, (1, 0, 2, 3))
    xs = (seq_major(rh), seq_major(wh), seq_major(kh), seq_major(vh),
          seq_major(kk), seq_major(kk * ah))

    def step(state, inp):
        r_t, w_t, k_t, v_t, kk_t, b_t = inp
        sa = jnp.einsum('bhvk,bhk->bhv', state, kk_t)
        state = (state * w_t[:, :, None, :] - sa[..., None] * b_t[:, :, None, :]
                 + v_t[..., None] * k_t[:, :, None, :])
        y_t = jnp.einsum('bhvk,bhk->bhv', state, r_t)
        return state, y_t

    s0 = jnp.zeros((bsz, RWKV_HEADS, RWKV_HEAD, RWKV_HEAD), jnp.float32)
    _, y = lax.scan(step, s0, xs)
    y = jnp.transpose(y, (1, 0, 2, 3))
    mu_y = jnp.mean(y, axis=-1, keepdims=True)
    var_y = jnp.mean(jnp.square(y - mu_y), axis=-1, keepdims=True)
    y = ((y - mu_y) * lax.rsqrt(var_y + RWKV_GN_EPS)).reshape(bsz, seq, RWKV_DIM)
    y = y * gn_g.astype(jnp.float32) + gn_b.astype(jnp.float32)
    bonus = jnp.sum(rh * kh * r_k.astype(jnp.float32), axis=-1, keepdims=True) * vh
    y = y + bonus.reshape(bsz, seq, RWKV_DIM)
    return (y.astype(p.dtype) * g) @ proj


def rglru_branch(p, conv_w, conv_b, wa, ba, wx, bx, lam, proj):
    bsz, seq, _ = p.shape
    y_gate = jax.nn.gelu(p[..., :LRU_DIM], approximate=True)
    xc = causal_dwconv(p[..., LRU_DIM:], conv_w, conv_b)
    xb = xc.reshape(bsz, seq, LRU_HEADS, LRU_BLOCK)
    gate_a = jax.nn.sigmoid(jnp.einsum('bshi,hij->bshj', xb, wa).reshape(bsz, seq, LRU_DIM) + ba)
    gate_x = jax.nn.sigmoid(jnp.einsum('bshi,hij->bshj', xb, wx).reshape(bsz, seq, LRU_DIM) + bx)
    log_a = -LRU_C * gate_a.astype(jnp.float32) * jax.nn.softplus(-lam.astype(jnp.float32))
    a = jnp.exp(log_a)
    mult = jnp.sqrt(-jnp.expm1(2.0 * log_a))
    b = xc.astype(jnp.float32) * gate_x.astype(jnp.float32) * mult

    def combine(left, right):
        a_l, b_l = left
        a_r, b_r = right
        return a_l * a_r, a_r * b_l + b_r

    _, h = lax.associative_scan(combine, (a, b), axis=1)
    return (h.astype(p.dtype) * y_gate) @ proj


def moe_ffn(h, router_w, router_bias, w1, w3, w2, sw1, sw3, sw2):
    bsz, seq, dm = h.shape
    n_tok = bsz * seq
    xt = h.reshape(n_tok, dm)
    scores = jax.nn.sigmoid(xt.astype(jnp.float32) @ router_w.astype(jnp.float32))
    biased = scores + router_bias.astype(jnp.float32)
    per_group = N_EXPERTS // N_GROUPS
    grp_score = jnp.sum(lax.top_k(biased.reshape(n_tok, N_GROUPS, per_group), 2)[0], axis=-1)
    _, grp_idx = lax.top_k(grp_score, TOPK_GROUPS)
    grp_mask = jnp.any(grp_idx[..., None] == jnp.arange(N_GROUPS), axis=1)
    masked = jnp.where(jnp.repeat(grp_mask, per_group, axis=1), biased, -jnp.inf)
    _, top_idx = lax.top_k(masked, TOP_K)
    top_w = jnp.take_along_axis(scores, top_idx, axis=1)
    top_w = top_w / jnp.sum(top_w, axis=-1, keepdims=True) * ROUTED_SCALE

    nk = n_tok * TOP_K
    e_flat = top_idx.reshape(nk).astype(jnp.int32)
    tok_flat = jnp.arange(nk, dtype=jnp.int32) // TOP_K
    w_flat = top_w.reshape(nk)
    order = jnp.argsort(e_flat)
    e_sorted = e_flat[order]
    counts = jnp.zeros((N_EXPERTS,), jnp.int32).at[e_flat].add(1)
    starts = jnp.cumsum(counts) - counts
    padded = (counts + MOE_BLOCK - 1) // MOE_BLOCK * MOE_BLOCK
    pad_end = jnp.cumsum(padded)
    pad_start = pad_end - padded
    dest = pad_start[e_sorted] + jnp.arange(nk, dtype=jnp.int32) - starts[e_sorted]
    n_blocks = -(-(nk + N_EXPERTS * (MOE_BLOCK - 1)) // MOE_BLOCK)
    n_rows = n_blocks * MOE_BLOCK
    row_tok = jnp.full((n_rows,), n_tok, jnp.int32).at[dest].set(tok_flat[order])
    row_w = jnp.zeros((n_rows,), jnp.float32).at[dest].set(w_flat[order])
    block_exp = jnp.minimum(
        jnp.searchsorted(pad_end, jnp.arange(n_blocks, dtype=jnp.int32) * MOE_BLOCK, side='right'),
        N_EXPERTS - 1)
    x_pad = jnp.concatenate([xt, jnp.zeros((1, dm), xt.dtype)], axis=0)

    def block_step(acc, inp):
        tok, wt, e = inp
        xb = x_pad[tok]
        hb = jax.nn.silu(xb @ w1[e]) * (xb @ w3[e])
        yb = (hb @ w2[e]).astype(jnp.float32) * wt[:, None]
        return acc.at[tok].add(yb), None

    acc0 = jnp.zeros((n_tok + 1, dm), jnp.float32)
    acc, _ = lax.scan(block_step, acc0, (row_tok.reshape(n_blocks, MOE_BLOCK),
                                         row_w.reshape(n_blocks, MOE_BLOCK), block_exp))
    routed = acc[:n_tok].astype(xt.dtype)
    shared = (jax.nn.silu(xt @ sw1) * (xt @ sw3)) @ sw2
    return (routed + shared).reshape(bsz, seq, dm)


def setup_inputs(seed: int = 0) -> dict:
    key = jax.random.key(seed)
    ks = iter(jax.random.split(key, 64))
    nrm = lambda shape, scale: jax.random.normal(next(ks), shape, jnp.float32) * scale
    L, D = DEPTH, D_MODEL
    u = jax.random.uniform(next(ks), (L, LRU_DIM), jnp.float32, 0.9, 0.999)
    s = u ** (1.0 / LRU_C)
    return {
        'x': nrm((BATCH, SEQ, D), 1.0),
        'c': nrm((BATCH, D), 1.0),
        'ada_w': nrm((L, D, N_MOD * D), 0.3 * D ** -0.5),
        'ada_b': nrm((L, N_MOD * D), 0.02),
        'norm1': 1.0 + nrm((L, D), 0.02),
        'norm2': 1.0 + nrm((L, D), 0.02),
        'w_in': nrm((L, D, IN_COLS), D ** -0.5),
        'conv_a_w': nrm((L, CONV_A_WIDTH, CONV_A_CH), CONV_A_WIDTH ** -0.5),
        'conv_a_b': nrm((L, CONV_A_CH), 0.02),
        'ln_a_g': 1.0 + nrm((L, CONV_A_CH), 0.02),
        'ln_a_b': nrm((L, CONV_A_CH), 0.02),
        'proj_a': nrm((L, CONV_A_CH, D), CONV_A_CH ** -0.5),
        'mu_b': jax.random.uniform(next(ks), (L, COLS_B), jnp.float32),
        'w0': jax.random.uniform(next(ks), (L, RWKV_DIM), jnp.float32, -6.0, -1.0),
        'w_up': nrm((L, LORA_W, RWKV_DIM), 0.5 * LORA_W ** -0.5),
        'a0': nrm((L, RWKV_DIM), 0.1),
        'a_up': nrm((L, LORA_A, RWKV_DIM), 0.5 * LORA_A ** -0.5),
        'g_up': nrm((L, LORA_G, RWKV_DIM), LORA_G ** -0.5),
        'k_k': 0.85 + nrm((L, RWKV_DIM), 0.05),
        'k_a': 1.0 + nrm((L, RWKV_DIM), 0.05),
        'r_k': nrm((L, RWKV_HEADS, RWKV_HEAD), 0.1),
        'gn_b_g': 1.0 + nrm((L, RWKV_DIM), 0.02),
        'gn_b_b': nrm((L, RWKV_DIM), 0.02),
        'proj_b': nrm((L, RWKV_DIM, D), RWKV_DIM ** -0.5),
        'conv_c_w': nrm((L, LRU_CONV, LRU_DIM), LRU_CONV ** -0.5),
        'conv_c_b': nrm((L, LRU_DIM), 0.02),
        'lru_wa': nrm((L, LRU_HEADS, LRU_BLOCK, LRU_BLOCK), LRU_BLOCK ** -0.5),
        'lru_ba': nrm((L, LRU_DIM), 0.02),
        'lru_wx': nrm((L, LRU_HEADS, LRU_BLOCK, LRU_BLOCK), LRU_BLOCK ** -0.5),
        'lru_bx': nrm((L, LRU_DIM), 0.02),
        'lru_lambda': jnp.log(s) - jnp.log1p(-s),
        'proj_c': nrm((L, LRU_DIM, D), LRU_DIM ** -0.5),
        'w_out': nrm((L, D, D), D ** -0.5),
        'router_w': nrm((L, D, N_EXPERTS), D ** -0.5),
        'router_bias': nrm((L, N_EXPERTS), 0.01),
        'exp_w1': nrm((L, N_EXPERTS, D, EXPERT_FF), D ** -0.5),
        'exp_w3': nrm((L, N_EXPERTS, D, EXPERT_FF), D ** -0.5),
        'exp_w2': nrm((L, N_EXPERTS, EXPERT_FF, D), EXPERT_FF ** -0.5),
        'sh_w1': nrm((L, D, SHARED_FF), D ** -0.5),
        'sh_w3': nrm((L, D, SHARED_FF), D ** -0.5),
        'sh_w2': nrm((L, SHARED_FF, D), SHARED_FF ** -0.5),
        'final_norm': 1.0 + nrm((D,), 0.02),
    }


def reference(x, c, ada_w, ada_b, norm1, norm2, w_in, conv_a_w, conv_a_b, ln_a_g, ln_a_b,
              proj_a, mu_b, w0, w_up, a0, a_up, g_up, k_k, k_a, r_k, gn_b_g, gn_b_b, proj_b,
              conv_c_w, conv_c_b, lru_wa, lru_ba, lru_wx, lru_bx, lru_lambda, proj_c, w_out,
              router_w, router_bias, exp_w1, exp_w3, exp_w2, sh_w1, sh_w3, sh_w2, final_norm):
    cond = jax.nn.silu(c)
    for l in range(DEPTH):
        mod = (cond @ ada_w[l] + ada_b[l])[:, None, :]
        sh1, sc1, g1, sh2, sc2, g2 = jnp.split(mod, N_MOD, axis=-1)
        h = rmsnorm(x, norm1[l]) * (1.0 + sc1) + sh1
        p = h @ w_in[l]
        pa, pb, pc, pg = jnp.split(p, MIX_SPLITS, axis=-1)
        oa = conformer_conv(pa, conv_a_w[l], conv_a_b[l], ln_a_g[l], ln_a_b[l], proj_a[l])
        ob = rwkv7_time_mix(pb, mu_b[l], w0[l], w_up[l], a0[l], a_up[l], g_up[l], k_k[l],
                            k_a[l], r_k[l], gn_b_g[l], gn_b_b[l], proj_b[l])
        oc = rglru_branch(pc, conv_c_w[l], conv_c_b[l], lru_wa[l], lru_ba[l], lru_wx[l],
                          lru_bx[l], lru_lambda[l], proj_c[l])
        gate = jax.nn.sigmoid(pg).reshape(pg.shape[:-1] + (N_BRANCH, D_MODEL))
        merged = gate[..., 0, :] * oa + gate[..., 1, :] * ob + gate[..., 2, :] * oc
        x = x + g1 * (merged @ w_out[l])
        h = rmsnorm(x, norm2[l]) * (1.0 + sc2) + sh2
        x = x + g2 * moe_ffn(h, router_w[l], router_bias[l], exp_w1[l], exp_w3[l], exp_w2[l],
                             sh_w1[l], sh_w3[l], sh_w2[l])
    return rmsnorm(x, final_norm)
```

```python
import numpy as np
from contextlib import ExitStack
import concourse.bass as bass
import concourse.mybir as mybir
from concourse.bass_utils import run_bass_kernel_spmd

F32 = mybir.dt.float32
BF16 = mybir.dt.bfloat16
AF = mybir.ActivationFunctionType
ALU = mybir.AluOpType

EPOCH = 30000
NDS = 16


class Tok:
    __slots__ = ("eng", "sem", "sid", "val")

    def __init__(self, eng, sem, sid, val):
        self.eng, self.sem, self.sid, self.val = eng, sem, sid, val


class V:
    __slots__ = ("buf", "ap", "key")

    def __init__(self, buf, ap, key):
        self.buf, self.ap, self.key = buf, ap, key


class _Keyed:
    def __init__(self, buf, key):
        self.buf, self.key = buf, key

    def __getitem__(self, idx):
        return V(self.buf, self.buf.t[idx], self.key)


class Buf:
    def __init__(self, t, name, psum=False):
        self.t = t
        self.name = name
        self.psum = psum
        self.st = {}

    def __getitem__(self, idx):
        return V(self, self.t[idx], None)

    def k(self, key):
        return _Keyed(self, key)

    def view(self, ap, key=None):
        return V(self, ap, key)

    def _keys(self, key):
        if key is None:
            return list(self.st.keys())
        ks = []
        if key in self.st:
            ks.append(key)
        if None in self.st:
            ks.append(None)
        return ks

    def rdeps(self, key):
        if self.psum:
            return self.wdeps(key)
        return [self.st[k][0] for k in self._keys(key) if self.st[k][0] is not None]

    def wdeps(self, key):
        out = []
        for k in self._keys(key):
            w, rs = self.st[k]
            if w is not None:
                out.append(w)
            out.extend(rs)
        return out

    def add_read(self, key, tok):
        self.st.setdefault(key, [None, []])[1].append(tok)

    def add_write(self, key, tok):
        if key is None:
            self.st = {None: [tok, []]}
        else:
            self.st[key] = [tok, []]


class Prog:
    def __init__(self):
        self.nc = bass.Bass("TRN2", target_bir_lowering=False)
        nc = self.nc
        self.es = ExitStack()
        self.eng = {"pe": nc.tensor, "act": nc.scalar, "dve": nc.vector,
                    "pool": nc.gpsimd, "sp": nc.sync}
        self.esem = {e: [] for e in self.eng}
        self.ecnt = {e: 0 for e in self.eng}
        self.seen = {e: {} for e in self.eng}
        self.dsem = [self.es.enter_context(nc.semaphore(f"dq{i}")) for i in range(4 * NDS)]
        self.dcnt = [0] * (4 * NDS)
        self.drr = {"hw": 0, "sw": 0, "rg": 0, "rh": 0}
        self.nbuf = 0
        self.pad_pe = 0
        self.pad_fn = None
        self.psum_rot = []
        self.psum_i = 0
        self.ninst = 0

    def sb(self, shape, dtype, name=None, stack=None):
        self.nbuf += 1
        name = f"{name or 'sb'}_{self.nbuf}"
        t = (stack or self.es).enter_context(self.nc.sbuf_tensor(name, list(shape), dtype))
        return Buf(t, name)

    def ps(self, shape, dtype=F32, name=None, stack=None):
        self.nbuf += 1
        name = f"{name or 'ps'}_{self.nbuf}"
        t = (stack or self.es).enter_context(self.nc.psum_tensor(name, list(shape), dtype))
        return Buf(t, name, psum=True)

    def dram(self, name, shape, dtype, kind):
        t = self.nc.dram_tensor(name, list(shape), dtype, kind=kind)
        return Buf(t.ap(), name)

    def _etok(self, e, n):
        idx = (n - 1) // EPOCH
        while len(self.esem[e]) <= idx:
            s = self.es.enter_context(self.nc.semaphore(f"e_{e}_{len(self.esem[e])}"))
            self.esem[e].append(s)
        return Tok(e, self.esem[e][idx], ("e", e, idx), (n - 1) % EPOCH + 1)

    def wait(self, e, tok):
        if tok is None:
            return
        if tok.eng == "pe" and e == "pe":
            return
        if self.seen[e].get(tok.sid, 0) >= tok.val:
            return
        self.eng[e].wait_ge(tok.sem, tok.val)
        self.seen[e][tok.sid] = tok.val

    def issue(self, e, fn, reads, writes):
        toks = []
        for v in reads:
            toks.extend(v.buf.rdeps(v.key))
        for v in writes:
            toks.extend(v.buf.wdeps(v.key))
        if e == "pe" and self.pad_pe and self.pad_fn is not None:
            if any(t is not None and t.eng != "pe" and self.seen[e].get(t.sid, 0) < t.val for t in toks):
                n = self.pad_pe
                self.pad_pe = 0
                for _ in range(n):
                    self.pad_fn()
                self.pad_pe = n
        for t in toks:
            self.wait(e, t)
        inst = fn()
        self.ecnt[e] += 1
        self.ninst += 1
        tok = self._etok(e, self.ecnt[e])
        inst.then_inc(tok.sem, 1)
        for v in reads:
            v.buf.add_read(v.key, tok)
        for v in writes:
            v.buf.add_write(v.key, tok)
        return tok

    def op(self, e, fname, out, *args, **kw):
        reads = [a for a in args if isinstance(a, V)] + [a for a in kw.values() if isinstance(a, V)]
        cv = lambda a: a.ap if isinstance(a, V) else a
        f = getattr(self.eng[e], fname)
        fn = lambda: f(cv(out), *[cv(a) for a in args], **{k: cv(v) for k, v in kw.items()})
        return self.issue(e, fn, reads, [out])

    def mm(self, out, lhsT, rhs, start=True, stop=True, **kw):
        fn = lambda: self.nc.tensor.matmul(out.ap, lhsT.ap, rhs.ap, start=start, stop=stop, **kw)
        return self.issue("pe", fn, [lhsT, rhs], [out])

    def transpose(self, out, in_, ident):
        fn = lambda: self.nc.tensor.transpose(out.ap, in_.ap, ident.ap)
        return self.issue("pe", fn, [in_, ident], [out])

    def dma(self, q, out, in_, ring=False, **kw):
        toks = list(in_.buf.rdeps(in_.key)) + list(out.buf.wdeps(out.key))
        kind = ("rg" if ring else "sw") if q == "pool" else ("rh" if ring else "hw")
        i = self.drr[kind] + {"hw": 0, "sw": NDS, "rg": 2 * NDS, "rh": 3 * NDS}[kind]
        self.drr[kind] = (self.drr[kind] + 1) % NDS
        if self.dcnt[i] > 0:
            toks.append(Tok("dma", self.dsem[i], ("d", i), 16 * self.dcnt[i]))
        for t in toks:
            self.wait(q, t)
        inst = self.eng[q].dma_start(out=out.ap, in_=in_.ap, **kw)
        self.dcnt[i] += 1
        self.ninst += 1
        inst.then_inc(self.dsem[i], 16)
        tok = Tok("dma", self.dsem[i], ("d", i), 16 * self.dcnt[i])
        in_.buf.add_read(in_.key, tok)
        out.buf.add_write(out.key, tok)
        return tok

    def barrier(self):
        toks = [self._etok(e, self.ecnt[e]) for e in self.eng if self.ecnt[e] > 0]
        toks += [Tok("dma", self.dsem[i], ("d", i), 16 * self.dcnt[i]) for i in range(2 * NDS) if self.dcnt[i] > 0]
        for e in self.eng:
            for t in toks:
                if t.eng == e:
                    continue
                self.wait(e, t)

    def finish(self):
        toks = [Tok("dma", self.dsem[i], ("d", i), 16 * self.dcnt[i]) for i in range(4 * NDS) if self.dcnt[i] > 0]
        toks += [self._etok(e, self.ecnt[e]) for e in self.eng if self.ecnt[e] > 0 and e != "sp"]
        for t in toks:
            self.wait("sp", t)


def run_rr(gens, width):
    pending = list(gens)
    active = []
    while pending or active:
        while pending and len(active) < width:
            active.append(pending.pop(0))
        for g in list(active):
            try:
                next(g)
            except StopIteration:
                active.remove(g)


class WRing:
    def __init__(self, P, nslot, stack):
        self.P = P
        self.n = nslot
        self.slots = [P.sb([128, 8, 512], BF16, f"wr{i}", stack) for i in range(nslot)]
        self.plan = []
        self.issued = 0
        self.got = 0
        self.rel = []

    def add(self, name, loader):
        self.plan.append((name, loader))
        self.rel.append(False)

    def pump(self):
        while self.issued < len(self.plan):
            j = self.issued
            if j >= self.n and not self.rel[j - self.n]:
                break
            self.plan[j][1](self.slots[j % self.n])
            self.issued += 1

    def get(self, name):
        j = self.got
        assert self.plan[j][0] == name, (self.plan[j][0], name)
        self.pump()
        assert self.issued > j, ("weight ring stall", name)
        self.got += 1
        return j, self.slots[j % self.n]

    def release(self, j):
        self.rel[j] = True
        self.pump()


D = 1024
S = 2048
T = 512
NCH = S // T
KC = 8
PADN = 3
TB = 128
L = 64
EPS = 1e-6
CDEC = 0.6065306597126334

VOFF = {}
_o = 0
for _n, _w in (("ada_b", 48), ("norm1", 8), ("norm2", 8), ("caw", 124), ("cab", 4), ("lng", 4), ("lnb", 4),
               ("ccw", 32), ("ccb", 8), ("lba", 8), ("lbx", 8), ("lam", 8), ("mu_w", 1), ("mu_a", 1), ("mu_g", 1),
               ("mu_r", 8), ("mu_k", 8), ("mu_v", 8), ("w0", 8), ("a0", 8), ("k_k", 8), ("k_a", 8),
               ("r_k", 8), ("gn_g", 8), ("gn_b", 8)):
    VOFF[_n] = (_o, _w)
    _o += _w
NV = _o
COFF = {"ident": (0, 128), "ones": (128, 128), "mut": (256, 64), "mlt": (320, 64), "mui": (384, 64), "rst": (448, 64)}
NCST = 512


def rwkv_chunk(P, l, c, st, E):
    bk, vec, drvB, cst, identb = E["bk"], E["vec"], E["drvB"], E["cst"], E["identb"]
    ring, hT, merged, lor, gup = E["ring"], E["hT"], E["merged"], E["lor"], E["gup"]
    prevq, pextw, pexta, pextg, Sf, Sb = E["prevq"], E["pextw"], E["pexta"], E["pextg"], E["Sf"], E["Sb"]
    MUT, MLT, MUI, IDH, RST, bankT = E["MUT"], E["MLT"], E["MUI"], E["IDH"], E["RST"], E["bankT"]
    src, w_in_ap = E["src_ap_buf"], E["w_in_ap"]
    vcol, drv = E["vcol"], E["drv"]
    w_up_d, a_up_d, g_up_d, proj_b_d = E["w_up_d"], E["a_up_d"], E["g_up_d"], E["proj_b_d"]
    ones = lambda n=128, m=128: cst[0:n, 128:128 + m]
    HB = [64, 8, TB]

    def bc(name, n=TB):
        o, w = VOFF[name]
        b = vec[l]
        return V(b, b.t[0:64, o:o + 8].unsqueeze(2).to_broadcast([64, 8, n]), None)

    def bcd(j0, n=TB):
        b = drvB[l]
        return V(b, b.t[0:64, j0:j0 + 8].unsqueeze(2).to_broadcast([64, 8, n]), None)

    jr, wr_ = ring.get("B_r"); jk, wk_ = ring.get("B_k"); jv_, wv_ = ring.get("B_v"); jl, wl_ = ring.get("B_lora")
    yg = P.sb([64, 8, T], BF16, "b_yg", st)
    F = {n: P.sb(HB, F32, f"b_{n}", st) for n in ("r", "k", "v", "a", "sg", "e1", "e2", "e3", "kk", "t0", "g")}
    F["t1"] = F["sg"]; F["bon"] = F["a"]; F["y"] = F["r"]
    pq = P.sb([64, 8, 1 + TB], F32, "b_pq", st)
    mu3 = {"w": (vcol(l, "mu_w", 0, 1, 64), drv[l][0:64, 32:33]), "a": (vcol(l, "mu_a", 0, 1, 64), drv[l][0:64, 33:34]),
           "g": (vcol(l, "mu_g"), drv[l][:, 34:35])}
    H = {n: P.sb(HB, BF16, f"b_{n}", st) for n in ("rt", "kkt", "bt", "kt", "vb")}
    X = {n: P.sb([64, 512], BF16, f"b_{n}", st) for n in ("XTn", "UT")}
    XJ = []
    for j_ in range(TB // L):
        d_ = {n: P.sb([64, 512], BF16, f"b_{n}{j_}", st) for n in ("Am", "ATm", "NKm", "MBR", "MKR", "A2", "A2T", "A2b", "A2Tb", "Tb", "VT", "bT", "kT")}
        d_["Tf"] = P.sb([64, 512], F32, f"b_Tf{j_}", st)
        XJ.append(d_)
    lt = {n: P.sb([128, TB], F32, f"b_l{n}", st) for n in ("w", "a", "g", "t")}
    lb = {n: P.sb([128, TB], BF16, f"b_lb{n}", st) for n in ("w", "a", "g")}
    colbase = {"r": (wr_, 0), "k": (wk_, 0), "v": (wv_, 0)}
    v4 = lambda b, hh: V(b, b.t[0:64, 0:512].rearrange("p (h t) -> p h t", h=4), None)
    fl = lambda b: V(b, b.t[0:64].rearrange("p h t -> p (h t)"), None)
    hv = lambda b, hh: b[0:64, hh * 4:(hh + 1) * 4, :]

    for blk in range(T // TB):
        t0 = blk * TB
        hs = lambda kc: hT[:, kc, t0:t0 + TB]
        for q in "rkv":
            ws, cb = colbase[q]
            for hh in range(2):
                b_ = bk()
                for hl in range(4):
                    h = hh * 4 + hl
                    for kc in range(KC):
                        P.mm(b_[0:64, hl * TB:(hl + 1) * TB], ws[:, kc, cb + h * 64:cb + (h + 1) * 64], hs(kc), kc == 0, kc == KC - 1)
                P.op("act", "activation", pq[0:64, hh * 4:(hh + 1) * 4, 1:1 + TB], v4(b_, hh), AF.Copy)
            qi = "rkv".index(q)
            P.op("pool", "tensor_copy", pq[0:64, :, 0:1], V(prevq, prevq.t[0:64, qi, :].unsqueeze(2), None))
            mu = {"r": "mu_r", "k": "mu_k", "v": "mu_v"}[q]
            om = {"r": 0, "k": 8, "v": 16}[q]
            eq_ = "dve" if q == "r" else "pool"
            tq_ = F["t0"] if q == "r" else F["kk"]
            P.op(eq_, "tensor_tensor", tq_[:], pq[0:64, :, 0:TB], bc(mu), ALU.mult)
            P.op(eq_, "tensor_tensor", F[q][:], pq[0:64, :, 1:1 + TB], bcd(om), ALU.mult)
            P.op(eq_, "tensor_tensor", F[q][:], F[q][:], tq_[:], ALU.add)
            P.op("act", "activation", V(prevq, prevq.t[0:64, qi, :].unsqueeze(2), None), pq[0:64, :, TB:TB + 1], AF.Copy)
        b_ = bk()
        for kc in range(KC):
            P.mm(b_[0:64, 0:TB], wl_[:, kc, 0:64], hs(kc), kc == 0, kc == KC - 1)
        for kc in range(KC):
            P.mm(b_[0:64, TB:2 * TB], wl_[:, kc, 64:128], hs(kc), kc == 0, kc == KC - 1)
        for kc in range(KC):
            P.mm(b_[:, 2 * TB:3 * TB], wl_[:, kc, 128:256], hs(kc), kc == 0, kc == KC - 1)
        for (pe_, n, off, np_) in ((pextw, "w", 0, 64), (pexta, "a", TB, 64), (pextg, "g", 2 * TB, 128)):
            P.op("act", "activation", pe_[0:np_, 1:1 + TB], b_[0:np_, off:off + TB], AF.Copy)
            P.op("dve", "tensor_scalar", lt["t"][0:np_, :], pe_[0:np_, 0:TB], mu3[n][0], None, ALU.mult)
            P.op("dve", "scalar_tensor_tensor", lt[n][0:np_, :], pe_[0:np_, 1:1 + TB], mu3[n][1], lt["t"][0:np_, :], ALU.mult, ALU.add)
            P.op("act", "activation", pe_[0:np_, 0:1], pe_[0:np_, TB:TB + 1], AF.Copy)
        P.op("act", "activation", lb["w"][0:64, :], lt["w"][0:64, :], AF.Tanh)
        P.op("act", "activation", lb["a"][0:64, :], lt["a"][0:64, :], AF.Copy)
        P.op("act", "activation", lb["g"][:], lt["g"][:], AF.Sigmoid)
        for hh in range(2):
            bw = bk(); ba = bk(); bg = bk()
            for hl in range(4):
                h = hh * 4 + hl
                P.mm(bw[0:64, hl * TB:(hl + 1) * TB], lor[0:64, h * 64:(h + 1) * 64], lb["w"][0:64, :])
                P.mm(ba[0:64, hl * TB:(hl + 1) * TB], lor[0:64, 512 + h * 64:512 + (h + 1) * 64], lb["a"][0:64, :])
                P.mm(bg[0:64, hl * TB:(hl + 1) * TB], gup[:, h * 64:(h + 1) * 64], lb["g"][:])
            o, _ = VOFF["w0"]
            w0b = V(vec[l], vec[l].t[0:64, o + hh * 4:o + hh * 4 + 4].unsqueeze(2).to_broadcast([64, 4, TB]), None)
            o, _ = VOFF["a0"]
            a0b = V(vec[l], vec[l].t[0:64, o + hh * 4:o + hh * 4 + 4].unsqueeze(2).to_broadcast([64, 4, TB]), None)
            P.op("dve", "tensor_tensor", hv(F["sg"], hh), v4(bw, hh), w0b, ALU.add)
            P.op("dve", "tensor_tensor", hv(F["a"], hh), v4(ba, hh), a0b, ALU.add)
            P.op("act", "activation", hv(F["g"], hh), v4(bg, hh), AF.Copy)
        P.op("act", "activation", F["sg"][:], F["sg"][:], AF.Sigmoid)
        P.op("act", "activation", F["a"][:], F["a"][:], AF.Sigmoid)
        P.op("dve", "tensor_tensor_scan", fl(F["e1"]), RST[0:64, :], fl(F["sg"]), 0.0, ALU.mult, ALU.add)
        P.op("dve", "tensor_tensor", F["e3"][:], F["e1"][:], F["sg"][:], ALU.subtract)
        P.op("act", "activation", F["e2"][:], F["e1"][:], AF.Exp, scale=CDEC)
        P.op("act", "activation", F["e1"][:], F["e1"][:], AF.Exp, scale=-CDEC)
        P.op("act", "activation", F["e3"][:], F["e3"][:], AF.Exp, scale=-CDEC)
        P.op("dve", "tensor_tensor", F["kk"][:], F["k"][:], bc("k_k"), ALU.mult)
        P.op("act", "activation", F["t0"][:], F["kk"][:], AF.Square)
        for hh in range(2):
            b_ = bk()
            P.mm(b_[0:64, :], ones(64, 64), V(F["t0"], F["t0"].t[0:64, hh * 4:(hh + 1) * 4, :].rearrange("p h t -> p (h t)"), None))
            P.op("dve", "tensor_scalar", hv(F["t1"], hh), v4(b_, hh), 1e-24, None, ALU.max)
        P.op("act", "activation", F["t1"][:], F["t1"][:], AF.Ln)
        P.op("act", "activation", F["t1"][:], F["t1"][:], AF.Exp, scale=-0.5)
        P.op("dve", "tensor_tensor", F["kk"][:], F["kk"][:], F["t1"][:], ALU.mult)
        P.op("dve", "tensor_scalar", F["t0"][:], F["a"][:], -1.0, None, ALU.add)
        P.op("dve", "tensor_tensor", F["t0"][:], F["t0"][:], bc("k_a"), ALU.mult)
        P.op("dve", "scalar_tensor_tensor", fl(F["k"]), fl(F["t0"]), 1.0, fl(F["k"]), ALU.add, ALU.mult)
        P.op("dve", "tensor_tensor", H["rt"][:], F["r"][:], F["e1"][:], ALU.mult)
        P.op("pool", "tensor_tensor", H["kkt"][:], F["kk"][:], F["e3"][:], ALU.mult)
        P.op("dve", "tensor_tensor", F["t0"][:], F["kk"][:], F["a"][:], ALU.mult)
        P.op("pool", "tensor_tensor", H["bt"][:], F["t0"][:], F["e2"][:], ALU.mult)
        P.op("dve", "tensor_tensor", H["kt"][:], F["k"][:], F["e2"][:], ALU.mult)
        P.op("act", "activation", H["vb"][:], F["v"][:], AF.Copy)
        P.op("dve", "tensor_tensor", F["t0"][:], F["r"][:], F["k"][:], ALU.mult)
        P.op("dve", "tensor_tensor", F["t0"][:], F["t0"][:], bc("r_k"), ALU.mult)
        for hh in range(2):
            b_ = bk()
            P.mm(b_[0:64, :], ones(64, 64), V(F["t0"], F["t0"].t[0:64, hh * 4:(hh + 1) * 4, :].rearrange("p h t -> p (h t)"), None))
            P.op("dve", "tensor_tensor", hv(F["bon"], hh), v4(b_, hh), hv(F["v"], hh), ALU.mult)
        hm = lambda b, h: b[0:64, h * 64:(h + 1) * 64]

        def nonseq(j):
            sl = slice(j * L, (j + 1) * L)
            Xj = XJ[j]
            for (srcb, dstb, half) in ((H["vb"], Xj["VT"], 0), (H["bt"], Xj["bT"], 1)):
                for h in range(8):
                    P.transpose(bankT[0:64, half * 512 + h * 64:half * 512 + (h + 1) * 64], srcb[0:64, h, sl], identb[0:64, 0:64])
                P.op("act", "activation", dstb[:], bankT[0:64, half * 512:(half + 1) * 512], AF.Copy)
            yield
            for h in range(8):
                P.transpose(bankT[0:64, h * 64:(h + 1) * 64], H["kt"][0:64, h, sl], identb[0:64, 0:64])
            P.op("act", "activation", Xj["kT"][:], bankT[0:64, 0:512], AF.Copy)
            yield
            specs = (("Am", H["bt"], H["kkt"], MUT), ("ATm", H["kkt"], H["bt"], MLT), ("NKm", H["kt"], H["kkt"], MUT),
                     ("MBR", H["bt"], H["rt"], MUI), ("MKR", H["kt"], H["rt"], MUI))
            pend = []
            for (nm, lh, rh, msk) in specs[:2]:
                b_ = bk()
                for h in range(8):
                    P.mm(hm(b_, h), lh[0:64, h, sl], rh[0:64, h, sl])
                pend.append((nm, b_, msk))
            yield
            for (nm, b_, msk) in pend:
                P.op("dve", "tensor_tensor", Xj[nm][:], b_[0:64, :], msk[:], ALU.mult)
            P.op("dve", "tensor_tensor", Xj["Tf"][:], IDH[:], Xj["Am"][:], ALU.subtract)
            P.op("act", "activation", Xj["Tb"][:], Xj["Tf"][:], AF.Copy)
            pend = []
            for (nm, lh, rh, msk) in specs[2:]:
                b_ = bk()
                for h in range(8):
                    P.mm(hm(b_, h), lh[0:64, h, sl], rh[0:64, h, sl])
                pend.append((nm, b_, msk))
            yield
            for (nm, b_, msk) in pend:
                P.op("dve", "tensor_tensor", Xj[nm][:], b_[0:64, :], msk[:], ALU.mult)
            Ak, AkT = Xj["Am"], Xj["ATm"]
            for lev in range(5):
                b2t = bk()
                for h in range(8):
                    P.mm(hm(b2t, h), hm(Ak, h), hm(AkT, h))
                if lev < 4:
                    b2 = bk()
                    for h in range(8):
                        P.mm(hm(b2, h), hm(AkT, h), hm(Ak, h))
                yield
                n2, n2t = ("A2", "A2T") if lev % 2 == 0 else ("A2b", "A2Tb")
                P.op("act", "activation", Xj[n2t][:], b2t[0:64, :], AF.Copy)
                if lev < 4:
                    P.op("dve", "tensor_copy", Xj[n2][:], b2[0:64, :])
                yield
                btp = bk()
                for h in range(8):
                    P.mm(hm(btp, h), hm(Xj[n2t], h), hm(Xj["Tb"], h))
                yield
                P.op("dve", "tensor_tensor", Xj["Tf"][:], Xj["Tf"][:], btp[0:64, :], ALU.add)
                P.op("act", "activation", Xj["Tb"][:], Xj["Tf"][:], AF.Copy)
                Ak, AkT = Xj[n2], Xj[n2t]

        def seq(j):
            sl = slice(j * L, (j + 1) * L)
            Xj = XJ[j]
            bx = bk()
            for h in range(8):
                P.mm(hm(bx, h), H["kkt"][0:64, h, sl], hm(Sb, h), True, False)
                P.mm(hm(bx, h), hm(Xj["NKm"], h), hm(Xj["VT"], h), False, True)
            P.op("act", "activation", X["XTn"][:], bx[0:64, :], AF.Identity, scale=-1.0)
            bu = bk()
            for h in range(8):
                P.mm(hm(bu, h), hm(Xj["Tb"], h), hm(X["XTn"], h))
            P.op("act", "activation", X["UT"][:], bu[0:64, :], AF.Copy)
            by = bk()
            for h in range(8):
                P.mm(hm(by, h), hm(Sb, h), H["rt"][0:64, h, sl], True, False)
                P.mm(hm(by, h), hm(X["UT"], h), hm(Xj["MBR"], h), False, False)
                P.mm(hm(by, h), hm(Xj["VT"], h), hm(Xj["MKR"], h), False, True)
            P.op("act", "activation", F["y"][0:64, :, sl], V(by, by.t[0:64, 0:512].rearrange("p (h t) -> p h t", h=8), None), AF.Copy)
            bs_ = bk()
            for h in range(8):
                P.mm(hm(bs_, h), hm(Xj["bT"], h), hm(X["UT"], h), True, False)
                P.mm(hm(bs_, h), hm(Xj["kT"], h), hm(Xj["VT"], h), False, True)
            P.op("dve", "tensor_tensor", Sf[:], Sf[:], bs_[0:64, :], ALU.add)
            pl = V(F["e1"], F["e1"].t[0:64, :, j * L + L - 1:j * L + L].to_broadcast([64, 8, 64]), None)
            P.op("dve", "tensor_tensor", V(Sf, Sf.t[0:64, :].rearrange("p (h v) -> p h v", h=8), None),
                 V(Sf, Sf.t[0:64, :].rearrange("p (h v) -> p h v", h=8), None), pl, ALU.mult)
            P.op("act", "activation", Sb[:], Sf[:], AF.Copy)

        run_rr([nonseq(j) for j in range(TB // L)], 2)
        for j in range(TB // L):
            seq(j)
        P.op("act", "activation", F["t0"][:], F["y"][:], AF.Square)
        for hh in range(2):
            b1 = bk(); b2_ = bk()
            P.mm(b1[0:64, :], ones(64, 64), V(F["y"], F["y"].t[0:64, hh * 4:(hh + 1) * 4, :].rearrange("p h t -> p (h t)"), None))
            P.mm(b2_[0:64, :], ones(64, 64), V(F["t0"], F["t0"].t[0:64, hh * 4:(hh + 1) * 4, :].rearrange("p h t -> p (h t)"), None))
            P.op("act", "activation", hv(F["t1"], hh), v4(b1, hh), AF.Identity, scale=1.0 / 64)
            P.op("act", "activation", hv(F["e2"], hh), v4(b2_, hh), AF.Identity, scale=1.0 / 64)
        P.op("dve", "tensor_tensor", F["e3"][:], F["t1"][:], F["t1"][:], ALU.mult)
        P.op("dve", "tensor_tensor", F["e2"][:], F["e2"][:], F["e3"][:], ALU.subtract)
        P.op("act", "activation", F["e2"][:], F["e2"][:], AF.Ln, bias=64e-5)
        P.op("act", "activation", F["e2"][:], F["e2"][:], AF.Exp, scale=-0.5)
        P.op("dve", "tensor_tensor", F["y"][:], F["y"][:], F["t1"][:], ALU.subtract)
        P.op("dve", "tensor_tensor", F["y"][:], F["y"][:], F["e2"][:], ALU.mult)
        P.op("dve", "tensor_tensor", F["y"][:], F["y"][:], bc("gn_g"), ALU.mult)
        P.op("dve", "tensor_tensor", F["y"][:], F["y"][:], bc("gn_b"), ALU.add)
        P.op("dve", "tensor_tensor", F["y"][:], F["y"][:], F["bon"][:], ALU.add)
        P.op("dve", "tensor_tensor", yg[0:64, :, t0:t0 + TB], F["y"][:], F["g"][:], ALU.mult)
    for j_ in (jr, jk, jv_, jl):
        ring.release(j_)
    E["proj_and_gate"](l, st, ring, "B", 8, 64, lambda k: yg[0:64, k, :], True, hT, merged, False)


def build(dbg=(), stop=None, nlayers=2):
    P = Prog()
    nc = P.nc

    def din(name, shape):
        return P.dram(name, shape, F32, "ExternalInput")

    xT_d = din("xT", [D, S]); cT_d = din("cT", [128, 8]); vec_d = din("vec", [2, 128, NV])
    fn_d = din("fnorm", [128, 8]); rb_d = din("rbias", [2, 128, 64]); cst_d = din("cst", [128, NCST])
    ada_w_d = din("ada_w", [2, D, 6 * D]); w_in_d = din("w_in", [2, D, 7936])
    proj_a_d = din("proj_a", [2, 512, D]); proj_b_d = din("proj_b", [2, 512, D]); proj_c_d = din("proj_c", [2, D, D])
    w_out_d = din("w_out", [2, D, D]); w_up_d = din("w_up", [2, 64, 512]); a_up_d = din("a_up", [2, 64, 512])
    g_up_d = din("g_up", [2, 128, 512]); lwa_d = din("lru_wa", [2, 8, 128, 128]); lwx_d = din("lru_wx", [2, 8, 128, 128])
    rw_d = din("router_w", [2, D, 64]); e1_d = din("exp_w1", [2, 64, D, 256]); e3_d = din("exp_w3", [2, 64, D, 256])
    e2_d = din("exp_w2", [2, 64, 256, D]); s1_d = din("sh_w1", [2, D, 256]); s3_d = din("sh_w3", [2, D, 256])
    s2_d = din("sh_w2", [2, 256, D])
    out_d = P.dram("outT", [D, S], F32, "ExternalOutput")
    dbg_d = {n: P.dram("dbg_" + n, [D, S], F32, "ExternalOutput") for n in dbg}

    xs = P.dram("xs_scratch", [D, S], F32, "Internal")
    cst = P.sb([128, NCST], F32, "cst")
    vec = [P.sb([128, NV], F32, f"vec{l}") for l in range(2)]
    modT = [P.sb([128, 48], F32, f"modT{l}") for l in range(2)]
    drv = [P.sb([128, 40], F32, f"drv{l}") for l in range(2)]
    drvB = [P.sb([64, 24], F32, f"drvB{l}") for l in range(2)]
    identb = P.sb([128, 128], BF16, "identb")
    MUT = P.sb([64, 512], BF16, "MUT"); MLT = P.sb([64, 512], BF16, "MLT"); MUI = P.sb([64, 512], BF16, "MUI")
    IDH = P.sb([64, 512], BF16, "IDH"); RST = P.sb([64, 8 * TB], BF16, "RST")
    banks = [P.ps([128, 512], F32, f"bank{i}") for i in range(8)]
    bankT = Buf(banks[7].t.bitcast(BF16), "bankT", psum=True)
    rot = [0]

    def bk():
        b = banks[rot[0] % 6]
        rot[0] += 1
        return b

    fill_rhs = P.sb([128, 512], BF16, "fill_rhs")
    P.op("pool", "memset", fill_rhs[:], 0.0)
    P.pad_fn = lambda: P.mm(banks[6][:], identb[:], fill_rhs[:])
    ident = lambda n=128: cst[0:n, 0:n]
    ones = lambda n=128, m=128: cst[0:n, 128:128 + m]

    def vcol(l, name, j=0, n=1, parts=128):
        o, w = VOFF[name]
        return vec[l][0:parts, o + j:o + j + n]

    def ld_w(dst, src_ap, q="pool"):
        return P.dma(q, dst, V(src_ap_buf, src_ap, None))

    src_ap_buf = Buf(None, "wsrc")

    def dump(name, src_fn):
        if name in dbg_d:
            for kc in range(KC):
                P.dma("sp", dbg_d[name].view(dbg_d[name].t[kc * 128:(kc + 1) * 128, :]), src_fn(kc))

    P.dma("sp", cst[:], cst_d[:])
    P.dma("sp", xs[:], xT_d[:])
    for l in range(2):
        P.dma("sp", vec[l][:], vec_d.view(vec_d.t[l]))
    P.op("dve", "tensor_copy", identb[:], cst[:, 0:128])
    for h in range(8):
        for (dst, nm) in ((MUT, "mut"), (MLT, "mlt"), (MUI, "mui"), (IDH, "ident")):
            o = COFF[nm][0]
            P.op("dve", "tensor_copy", dst[0:64, h * 64:(h + 1) * 64], cst[0:64, o:o + 64])
    for j in range(8 * TB // 64):
        P.op("dve", "tensor_copy", RST[0:64, j * 64:(j + 1) * 64], cst[0:64, 448:512])

    with ExitStack() as st:
        cT = P.sb([128, 8], F32, "cT", st); cond2 = P.sb([128, 8, 2], F32, "cond2", st)
        aw = [P.sb([128, KC, 512], F32, f"aw{i}", st) for i in range(2)]
        P.dma("sp", cT[:], cT_d[:])
        P.op("act", "activation", cond2[:, :, 0], cT[:], AF.Silu)
        P.op("act", "activation", cond2[:, :, 1], cT[:], AF.Silu)
        for l in range(nlayers):
            pm = bk()
            for g in range(12):
                a = aw[g % 2]
                P.dma("sp", a[:], V(src_ap_buf, ada_w_d.t[l][:, g * 512:(g + 1) * 512].rearrange("(k p) n -> p k n", p=128), None))
                for j in range(4):
                    col = (g * 4 + j) * 2
                    for kc in range(KC):
                        P.mm(pm[:, col:col + 2], a[:, kc, j * 128:(j + 1) * 128], cond2[:, kc, :], kc == 0, kc == KC - 1)
            P.op("dve", "tensor_tensor", modT[l][:], V(pm, pm.t[:, 0:96].rearrange("p (j two) -> p j two", two=2)[:, :, 0], None),
                 vcol(l, "ada_b", 0, 48), ALU.add)
            P.op("dve", "tensor_scalar", drv[l][:, 0:8], modT[l][:, 8:16], 1.0, None, ALU.add)
            P.op("dve", "tensor_tensor", drv[l][:, 0:8], drv[l][:, 0:8], vcol(l, "norm1", 0, 8), ALU.mult)
            P.op("dve", "tensor_scalar", drv[l][:, 8:16], modT[l][:, 32:40], 1.0, None, ALU.add)
            P.op("dve", "tensor_tensor", drv[l][:, 8:16], drv[l][:, 8:16], vcol(l, "norm2", 0, 8), ALU.mult)
            P.op("act", "activation", drv[l][:, 16:24], vcol(l, "lam", 0, 8), AF.Exp, scale=-1.0)
            P.op("act", "activation", drv[l][:, 16:24], drv[l][:, 16:24], AF.Ln, bias=1.0)
            P.op("dve", "tensor_scalar", drv[l][:, 24:32], drv[l][:, 16:24], -16.0, None, ALU.mult)
            P.op("dve", "tensor_scalar", drv[l][:, 16:24], drv[l][:, 16:24], -8.0, None, ALU.mult)
            P.op("dve", "tensor_scalar", drv[l][:, 32:35], vcol(l, "mu_w", 0, 3), -1.0, 1.0, ALU.mult, ALU.add)
            P.op("dve", "tensor_scalar", drvB[l][0:64, 0:24], vcol(l, "mu_r", 0, 24, 64), -1.0, 1.0, ALU.mult, ALU.add)
        P.barrier()

    def rmsnorm_mod(xsrc, gm, sh, dst_fn, st):
        sq = [P.sb([128, T], F32, f"nsq{i}", st) for i in range(2)]
        rs = P.sb([128, T], F32, "nrs", st)
        ss = bk()
        for kc in range(KC):
            q = sq[kc % 2]
            P.op("act", "activation", q[:], xsrc(kc), AF.Square)
            P.mm(ss[:], ones(), q[:], kc == 0, kc == KC - 1)
        P.op("act", "activation", rs[:], ss[:], AF.Ln, scale=1.0 / D, bias=EPS)
        P.op("act", "activation", rs[:], rs[:], AF.Exp, scale=-0.5)
        for kc in range(KC):
            q = sq[kc % 2]
            P.op("dve", "tensor_tensor", q[:], xsrc(kc), rs[:], ALU.mult)
            if gm is None:
                P.op("act", "activation", dst_fn(kc), q[:], AF.Identity, scale=sh(kc))
            else:
                P.op("act", "activation", dst_fn(kc), q[:], AF.Identity, scale=gm(kc), bias=sh(kc))

    GB = 4864

    def dump_merged(l, cs, merged):
        n = f"mg{l}"
        if n in dbg_d:
            for kc in range(KC):
                P.dma("sp", dbg_d[n].view(dbg_d[n].t[kc * 128:(kc + 1) * 128, cs]), merged[:, kc, :])
            P.barrier()


    def w_in_ap(l, c0, n):
        return w_in_d.t[l][:, c0:c0 + n].rearrange("(k p) n -> p k n", p=128)

    def proj_and_gate(l, st, ring, mname, nk, np_, rhs_fn, split, hT, merged, first):
        gt = [P.sb([128, T], F32, f"pg_gt{i}", st) for i in range(2)]
        tmp = [P.sb([128, T], F32, f"pg_tmp{i}", st) for i in range(2)]
        pj = pp = None
        for half in range(2):
            if split or half == 0:
                if pj is not None:
                    ring.release(pj)
                pj, pp = ring.get(f"{mname}_proj{half}" if split else f"{mname}_proj")
            gj, gp = ring.get(f"{mname}_g{half}")
            for ocl in range(4):
                oc = half * 4 + ocl
                pso = bk(); psg = bk()
                for k in range(nk):
                    if split:
                        lh = pp[0:np_, k, ocl * 128:(ocl + 1) * 128]
                    else:
                        lh = V(pp, pp.t[0:np_].rearrange("p k n -> p (k n)")[:, k * 1024 + oc * 128:k * 1024 + (oc + 1) * 128], None)
                    P.mm(pso[:], lh, rhs_fn(k), k == 0, k == nk - 1)
                for kc in range(KC):
                    P.mm(psg[:], gp[:, kc, ocl * 128:(ocl + 1) * 128], hT[:, kc, :], kc == 0, kc == KC - 1)
                g = gt[oc % 2]
                P.op("act", "activation", g[:], psg[:], AF.Sigmoid)
                if first:
                    P.op("dve", "tensor_tensor", merged.k(oc)[:, oc, :], pso[:], g[:], ALU.mult)
                else:
                    t_ = tmp[oc % 2]
                    P.op("dve", "tensor_tensor", t_[:], pso[:], g[:], ALU.mult)
                    P.op("pool", "tensor_tensor", merged.k(oc)[:, oc, :], merged.k(oc)[:, oc, :], t_[:], ALU.add)
            ring.release(gj)
        ring.release(pj)

    wcache = P.dram("wcache", [23, 128, 4096], BF16, "Internal")

    def plan_chunk(ring, l, c):
        full = lambda slot: (slot, slot.t[:].rearrange("p k n -> p (k n)"), 4096, 128)
        pieces = []

        def add(name, sv, src):
            pieces.append((name, sv, src))

        wl = lambda c0, n: ((lambda slot: (slot.t[:, :, 0:n], 128, 8, n)), w_in_ap(l, c0, n))
        kp = lambda d, h: ((lambda slot: (slot.t[:, :, :], 128, 8, 512)), d.t[l][:, h * 512:(h + 1) * 512].rearrange("(k p) n -> p k n", p=128))
        pbp = lambda h: ((lambda slot: (slot.t[0:64, :, :], 64, 8, 512)), proj_b_d.t[l][:, h * 512:(h + 1) * 512].rearrange("(hh i) n -> i hh n", i=64))
        add("A_val", *wl(0, 512)); add("A_sig", *wl(512, 512))
        add("A_proj", (lambda slot: (slot.t[:].rearrange("p k n -> p (k n)").rearrange("p (k n) -> p k n", k=4), 128, 4, 1024)),
            proj_a_d.t[l].rearrange("(k p) n -> p k n", p=128))
        add("A_g0", *wl(GB, 512)); add("A_g1", *wl(GB + 512, 512))
        add("C_y0", *wl(2816, 512)); add("C_x0", *wl(3840, 512)); add("C_y1", *wl(2816 + 512, 512)); add("C_x1", *wl(3840 + 512, 512))
        add("C_proj0", *kp(proj_c_d, 0)); add("C_g0", *wl(GB + 2048, 512))
        add("C_proj1", *kp(proj_c_d, 1)); add("C_g1", *wl(GB + 2048 + 512, 512))
        add("B_r", *wl(1024, 512)); add("B_k", *wl(1536, 512)); add("B_v", *wl(2048, 512)); add("B_lora", *wl(2560, 256))
        add("B_proj0", *pbp(0)); add("B_g0", *wl(GB + 1024, 512))
        add("B_proj1", *pbp(1)); add("B_g1", *wl(GB + 1024 + 512, 512))
        add("O_0", *kp(w_out_d, 0)); add("O_1", *kp(w_out_d, 1))
        for idx, (name, sv, src) in enumerate(pieces):
            def loader(slot, idx=idx, sv=sv, src=src):
                ap, np_, a, b = sv(slot)
                cv = wcache.t[idx, 0:np_, 0:a * b].rearrange("p (k n) -> p k n", k=a)
                if c == 0:
                    P.dma("pool", V(slot, ap, None), V(src_ap_buf, src, None), ring=True)
                    P.dma("sp", V(wcache, cv, idx), V(slot, ap, None), ring=True)
                else:
                    P.dma("sp", V(slot, ap, None), V(wcache, cv, idx), ring=True)
            ring.add(name, loader)

    for l in range(nlayers):
        with ExitStack() as lst:
            uext = P.sb([128, 4, 30 + T], BF16, "uext", lst)
            xh = P.sb([128, 8, 3], BF16, "xh", lst)
            hC = P.sb([128, 8], F32, "hC", lst)
            prevq = P.sb([64, 3, 8], F32, "prevq", lst)
            pextw = P.sb([64, 1 + TB], F32, "pextw", lst); pexta = P.sb([64, 1 + TB], F32, "pexta", lst)
            pextg = P.sb([128, 1 + TB], F32, "pextg", lst)
            Sf = P.sb([64, 512], F32, "Sf", lst); Sb = P.sb([64, 512], BF16, "Sb", lst)
            lw = P.sb([128, 2, 8, 128], BF16, "c_lw", lst)
            P.dma("pool", lw[:, 0], V(src_ap_buf, lwa_d.t[l].rearrange("h i j -> i h j"), None))
            P.dma("pool", lw[:, 1], V(src_ap_buf, lwx_d.t[l].rearrange("h i j -> i h j"), None))
            lor = P.sb([128, 1024], BF16, "b_lor", lst)
            gup = P.sb([128, 512], BF16, "b_gup", lst)
            P.dma("pool", lor[0:64, 0:512], V(src_ap_buf, w_up_d.t[l], None))
            P.dma("pool", lor[0:64, 512:1024], V(src_ap_buf, a_up_d.t[l], None))
            P.dma("pool", gup[:], V(src_ap_buf, g_up_d.t[l], None))
            P.op("pool", "memset", uext[:, :, 0:30], 0.0)
            P.op("pool", "memset", xh[:], 0.0)
            P.op("pool", "memset", hC[:], 0.0)
            P.op("pool", "memset", prevq[:], 0.0)
            P.op("pool", "memset", pextw[:, 0:1], 0.0); P.op("pool", "memset", pexta[:, 0:1], 0.0)
            P.op("pool", "memset", pextg[:, 0:1], 0.0)
            P.op("pool", "memset", Sf[:], 0.0); P.op("pool", "memset", Sb[:], 0.0)
            mc = lambda j, n=1, l=l: modT[l][:, j:j + n]

            with ExitStack() as mst:
                P.pad_pe = PADN
                ring = WRing(P, 5, mst)
                for _c in range(NCH):
                    plan_chunk(ring, l, _c)
                ring.pump()
                hT = P.sb([128, KC, T], BF16, "hT", mst)
                merged = P.sb([128, KC, T], F32, "merged", mst)
                for c in range(NCH):
                    cs = slice(c * T, (c + 1) * T)
                    with ExitStack() as st:
                        xch = P.sb([128, KC, T], F32, "xch", st)
                        P.dma("sp", xch[:], xs.view(xs.t[:, cs].rearrange("(k p) t -> p k t", p=128)))
                        rmsnorm_mod(lambda kc: xch[:, kc, :], lambda kc: drv[l][:, kc:kc + 1], lambda kc: mc(kc), lambda kc: hT[:, kc, :], st)
                        P.barrier()
                    if f"h{l}" in dbg_d:
                        with ExitStack() as st:
                            tf = P.sb([128, KC, T], F32, "dbgtf", st)
                            P.op("dve", "tensor_copy", tf[:], hT[:])
                            for kc in range(KC):
                                P.dma("sp", dbg_d[f"h{l}"].view(dbg_d[f"h{l}"].t[kc * 128:(kc + 1) * 128, cs]), tf[:, kc, :])
                            P.barrier()
                    with ExitStack() as st:
                        jv, wv = ring.get("A_val"); jg, wg = ring.get("A_sig")
                        sg = [P.sb([128, T], F32, f"a_sg{i}", st) for i in range(2)]
                        cv = P.sb([128, 4, T], F32, "a_cv", st)
                        dg = P.sb([128, 31, 128], BF16, "a_dg", st)
                        sA = P.sb([128, 4, T], BF16, "a_sA", st)
                        dgs = [dg, P.sb([128, 31, 128], BF16, "a_dg2", st)]

                        def a_in(pc):
                            psv = bk(); psg = bk()
                            for kc in range(KC):
                                P.mm(psv[:], wv[:, kc, pc * 128:(pc + 1) * 128], hT[:, kc, :], kc == 0, kc == KC - 1)
                            for kc in range(KC):
                                P.mm(psg[:], wg[:, kc, pc * 128:(pc + 1) * 128], hT[:, kc, :], kc == 0, kc == KC - 1)
                            yield
                            g = sg[pc % 2]
                            P.op("act", "activation", g[:], psg[:], AF.Sigmoid)
                            yield
                            P.op("dve", "tensor_tensor", uext.k(pc)[:, pc, 30:30 + T], psv[:], g[:], ALU.mult)

                        def a_conv(pc):
                            d_ = dgs[pc % 2]
                            for j in range(31):
                                P.op("dve" if j % 2 else "pool", "tensor_scalar", d_.k(j)[:, j, :], identb[:], vcol(l, "caw", pc * 31 + j), None, ALU.mult)
                            yield
                            psc = bk()
                            for j in range(31):
                                P.mm(psc[:], d_.k(j)[:, j, :], uext.k(pc)[:, pc, j:j + T], j == 0, j == 30)
                            yield
                            P.op("act", "activation", cv.k(pc)[:, pc, :], psc[:], AF.Identity, bias=vcol(l, "cab", pc))

                        run_rr([a_in(pc) for pc in range(4)], 2)
                        ring.release(jv); ring.release(jg)
                        run_rr([a_conv(pc) for pc in range(4)], 2)
                        P.op("dve", "tensor_copy", uext[:, :, 0:30], uext[:, :, T:T + 30])
                        pss = bk(); pss2 = bk()
                        for pc in range(4):
                            q = sg[pc % 2]
                            P.mm(pss[:], ones(), cv.k(pc)[:, pc, :], pc == 0, pc == 3)
                            P.op("act", "activation", q[:], cv.k(pc)[:, pc, :], AF.Square)
                            P.mm(pss2[:], ones(), q[:], pc == 0, pc == 3)
                        mean = P.sb([128, T], F32, "a_mean", st); rstd = P.sb([128, T], F32, "a_rstd", st)
                        P.op("act", "activation", mean[:], pss[:], AF.Identity, scale=1.0 / 512)
                        P.op("dve", "tensor_tensor", rstd[:], mean[:], mean[:], ALU.mult)
                        P.op("dve", "scalar_tensor_tensor", rstd[:], pss2[:], 1.0 / 512, rstd[:], ALU.mult, ALU.subtract)
                        P.op("act", "activation", rstd[:], rstd[:], AF.Ln, bias=1e-5)
                        P.op("act", "activation", rstd[:], rstd[:], AF.Exp, scale=-0.5)
                        for pc in range(4):
                            q = sg[pc % 2]
                            P.op("dve", "tensor_tensor", q[:], cv.k(pc)[:, pc, :], mean[:], ALU.subtract)
                            P.op("dve", "tensor_tensor", q[:], q[:], rstd[:], ALU.mult)
                            P.op("act", "activation", sA.k(pc)[:, pc, :], q[:], AF.Silu, scale=vcol(l, "lng", pc), bias=vcol(l, "lnb", pc))
                        proj_and_gate(l, st, ring, "A", 4, 128, lambda k: sA.k(k)[:, k, :], False, hT, merged, True)
                        P.barrier()
                    if stop == "A":
                        dump_merged(l, cs, merged)
                        continue
                    with ExitStack() as st:
                        hy = P.sb([128, 8, T], BF16, "c_hy", st)
                        CW = 3
                        xe = [P.sb([128, 3 + T], BF16, f"c_xe{i}", st) for i in range(CW)]
                        dgcs = [P.sb([128, 4, 128], BF16, f"c_dg{i}", st) for i in range(CW)]
                        W = {n: [P.sb([128, T], F32, f"c_{n}{i}", st) for i in range(CW)] for n in ("yg", "t1", "t2", "xc", "ga", "gx", "ys")}
                        xcb = [P.sb([128, T], BF16, f"c_xcb{i}", st) for i in range(CW)]
                        cpieces = {}

                        def c_pc(pc):
                            i2 = pc % CW
                            dgc = dgcs[i2]
                            if pc % 4 == 0:
                                cpieces[pc // 4] = (ring.get(f"C_y{pc // 4}"), ring.get(f"C_x{pc // 4}"))
                            (jy, wy), (jx, wx) = cpieces[pc // 4]
                            pcl = pc % 4
                            psy = bk(); psx = bk()
                            for kc in range(KC):
                                P.mm(psy[:], wy[:, kc, pcl * 128:(pcl + 1) * 128], hT[:, kc, :], kc == 0, kc == KC - 1)
                            for kc in range(KC):
                                P.mm(psx[:], wx[:, kc, pcl * 128:(pcl + 1) * 128], hT[:, kc, :], kc == 0, kc == KC - 1)
                            if pcl == 3:
                                ring.release(jy); ring.release(jx)
                            for j in range(4):
                                P.op("pool", "tensor_scalar", dgc.k(j)[:, j, :], identb[:], vcol(l, "ccw", pc * 4 + j), None, ALU.mult)
                            P.op("pool", "tensor_copy", xe[i2][:, 0:3], xh.k(pc)[:, pc, :])
                            yield
                            yg, t1, t2 = W["yg"][i2], W["t1"][i2], W["t2"][i2]
                            ys = W["ys"][i2]
                            P.op("act", "activation", t1[:], psy[:], AF.Square)
                            P.op("dve", "tensor_copy", ys[:], psy[:])
                            P.op("act", "activation", xe[i2][:, 3:3 + T], psx[:], AF.Copy)
                            yield
                            P.op("dve", "tensor_scalar", t1[:], t1[:], 0.044715, 1.0, ALU.mult, ALU.add)
                            psc = bk()
                            for j in range(4):
                                P.mm(psc[:], dgc.k(j)[:, j, :], xe[i2][:, j:j + T], j == 0, j == 3)
                            P.op("pool", "tensor_copy", xh.k(pc)[:, pc, :], xe[i2][:, T:T + 3])
                            yield
                            P.op("dve", "tensor_tensor", t1[:], t1[:], ys[:], ALU.mult)
                            xc = W["xc"][i2]
                            P.op("act", "activation", xc[:], psc[:], AF.Identity, bias=vcol(l, "ccb", pc))
                            yield
                            P.op("act", "activation", t1[:], t1[:], AF.Sigmoid, scale=1.5957691216057308)
                            P.op("dve", "tensor_copy", xcb[i2][:], xc[:])
                            yield
                            P.op("dve", "tensor_tensor", yg[:], t1[:], ys[:], ALU.mult)
                            psa = bk(); psb = bk()
                            P.mm(psa[:], lw[:, 0, pc, :], xcb[i2][:])
                            P.mm(psb[:], lw[:, 1, pc, :], xcb[i2][:])
                            yield
                            ga, gx = W["ga"][i2], W["gx"][i2]
                            P.op("act", "activation", ga[:], psa[:], AF.Sigmoid, bias=vcol(l, "lba", pc))
                            P.op("act", "activation", gx[:], psb[:], AF.Sigmoid, bias=vcol(l, "lbx", pc))
                            yield
                            P.op("act", "activation", t2[:], ga[:], AF.Exp, scale=drv[l][:, 24 + pc:25 + pc])
                            P.op("dve", "tensor_tensor", gx[:], gx[:], xc[:], ALU.mult)
                            yield
                            P.op("act", "activation", t2[:], t2[:], AF.Sqrt, scale=-1.0, bias=1.0)
                            P.op("act", "activation", ga[:], ga[:], AF.Exp, scale=drv[l][:, 16 + pc:17 + pc])
                            yield
                            P.op("dve", "tensor_tensor", gx[:], gx[:], t2[:], ALU.mult)
                            yield
                            P.op("dve", "tensor_tensor_scan", t2[:], ga[:], gx[:], hC.k(pc)[:, pc:pc + 1], ALU.mult, ALU.add)
                            yield
                            P.op("act", "activation", hC.k(pc)[:, pc:pc + 1], t2[:, T - 1:T], AF.Copy)
                            P.op("dve", "tensor_tensor", hy.k(pc)[:, pc, :], t2[:], yg[:], ALU.mult)

                        run_rr([c_pc(pc) for pc in range(8)], CW)
                        proj_and_gate(l, st, ring, "C", 8, 128, lambda k: hy.k(k)[:, k, :], True, hT, merged, False)
                        P.barrier()
                    if stop == "C":
                        dump_merged(l, cs, merged)
                        continue
                    with ExitStack() as st:
                        E = dict(bk=bk, vec=vec, drvB=drvB, cst=cst, identb=identb, ring=ring, lor=lor, gup=gup, hT=hT, merged=merged, prevq=prevq,
                                 pextw=pextw, pexta=pexta, pextg=pextg, Sf=Sf, Sb=Sb, MUT=MUT, MLT=MLT, MUI=MUI, IDH=IDH, RST=RST,
                                 bankT=bankT, src_ap_buf=src_ap_buf, w_in_ap=w_in_ap, vcol=vcol, drv=drv, w_up_d=w_up_d, a_up_d=a_up_d,
                                 g_up_d=g_up_d, proj_b_d=proj_b_d, proj_and_gate=proj_and_gate)
                        rwkv_chunk(P, l, c, st, E)
                        P.barrier()
                    dump_merged(l, cs, merged)
                    with ExitStack() as st:
                        xch = P.sb([128, KC, T], F32, "xch2", st)
                        P.dma("sp", xch[:], xs.view(xs.t[:, cs].rearrange("(k p) t -> p k t", p=128)))
                        mergedb = P.sb([128, KC, T], BF16, "mergedb", st)
                        for kc in range(KC):
                            P.op("act", "activation", mergedb.k(kc)[:, kc, :], merged.k(kc)[:, kc, :], AF.Copy)
                        for oc in range(8):
                            if oc % 4 == 0:
                                if oc:
                                    ring.release(jo)
                                jo, wo = ring.get(f"O_{oc // 4}")
                            ocl = oc % 4
                            pso = bk()
                            for kc in range(KC):
                                P.mm(pso[:], wo[:, kc, ocl * 128:(ocl + 1) * 128], mergedb.k(kc)[:, kc, :], kc == 0, kc == KC - 1)
                            P.op("dve", "scalar_tensor_tensor", xch.k(oc)[:, oc, :], pso[:], mc(16 + oc), xch.k(oc)[:, oc, :], ALU.mult, ALU.add)
                        ring.release(jo)
                        P.dma("sp", xs.view(xs.t[:, cs].rearrange("(k p) t -> p k t", p=128)), xch[:])
                        P.barrier()
                P.barrier()
            P.pad_pe = 0
            dump(f"x1_{l}", lambda kc: xs.view(xs.t[kc * 128:(kc + 1) * 128, :]))
            if stop in ("A", "C", "mix"):
                P.barrier()
                continue
            with ExitStack() as mst:
                h2 = P.sb([128, KC, S], BF16, "h2", mst)
                xT = P.sb([128, KC, S], F32, "xT", mst)
                cwT = P.sb([64, 2, S], BF16, "cwT", mst)
                rwb = P.sb([128, KC, 64], BF16, "rwb", mst)
                rbias = P.sb([128, 64], F32, "rbias", mst)
                P.dma("pool", rwb[:], V(src_ap_buf, rw_d.t[l].rearrange("(k p) n -> p k n", p=128), None))
                P.dma("sp", rbias[:], rb_d.view(rb_d.t[l]))
                for kc in range(KC):
                    P.dma("sp", xT.k(kc)[:, kc, :], xs.view(xs.t[kc * 128:(kc + 1) * 128, :]))
                for c in range(NCH):
                    cs = slice(c * T, (c + 1) * T)
                    with ExitStack() as st:
                        rmsnorm_mod(lambda kc: xT.k(kc)[:, kc, cs], lambda kc: drv[l][:, 8 + kc:9 + kc], lambda kc: mc(24 + kc), lambda kc: h2.k((kc, c))[:, kc, cs], st)
                        R = {n: P.sb([128, 64], F32, f"r_{n}", st) for n in ("sc", "bs", "b2", "mk", "sel")}
                        r8 = {n: P.sb([128, 8], F32, f"r8_{n}", st) for n in ("m1", "m2", "o8", "gm", "o8b", "ss")}
                        for tt in range(4):
                            ts_ = slice(c * T + tt * 128, c * T + (tt + 1) * 128)
                            psl = bk()
                            for kc in range(KC):
                                P.mm(psl[:, 0:64], h2.k((kc, c))[:, kc, ts_], rwb[:, kc, :], kc == 0, kc == KC - 1)
                            v3 = lambda b: V(b, b.t[:, :].rearrange("p (g e) -> p g e", e=8), None)
                            bc8 = lambda b: V(b, b.t[:, 0:8].unsqueeze(2).to_broadcast([128, 8, 8]), None)
                            P.op("act", "activation", R["sc"][:], psl[:, 0:64], AF.Sigmoid)
                            P.op("dve", "tensor_tensor", R["bs"][:], R["sc"][:], rbias[:], ALU.add)
                            P.op("dve", "tensor_reduce", r8["m1"][:], v3(R["bs"]), mybir.AxisListType.X, ALU.max)
                            P.op("dve", "tensor_tensor", v3(R["b2"]), v3(R["bs"]), bc8(r8["m1"]), ALU.is_equal)
                            P.op("dve", "scalar_tensor_tensor", R["b2"][:], R["b2"][:], -1e9, R["bs"][:], ALU.mult, ALU.add)
                            P.op("dve", "tensor_reduce", r8["m2"][:], v3(R["b2"]), mybir.AxisListType.X, ALU.max)
                            P.op("dve", "tensor_tensor", r8["m1"][:], r8["m1"][:], r8["m2"][:], ALU.add)
                            P.op("dve", "max", r8["o8"][:], r8["m1"][:])
                            P.op("dve", "tensor_scalar", r8["gm"][:], r8["m1"][:], r8["o8"][:, 3:4], None, ALU.is_ge)
                            P.op("dve", "tensor_scalar", r8["gm"][:], r8["gm"][:], -1.0, 1e9, ALU.add, ALU.mult)
                            P.op("dve", "tensor_tensor", v3(R["mk"]), v3(R["bs"]), bc8(r8["gm"]), ALU.add)
                            P.op("dve", "max", r8["o8b"][:], R["mk"][:])
                            P.op("dve", "tensor_scalar", R["sel"][:], R["mk"][:], r8["o8b"][:, 7:8], None, ALU.is_ge)
                            P.op("dve", "tensor_tensor", R["sel"][:], R["sel"][:], R["sc"][:], ALU.mult)
                            P.op("dve", "tensor_reduce", r8["ss"][:, 0:1], R["sel"][:], mybir.AxisListType.X, ALU.add)
                            P.op("dve", "reciprocal", r8["ss"][:, 0:1], r8["ss"][:, 0:1])
                            P.op("dve", "tensor_scalar", R["sel"][:], R["sel"][:], r8["ss"][:, 0:1], 2.5, ALU.mult, ALU.mult)
                            pst = bk()
                            P.transpose(pst[0:64, 0:128], R["sel"][:], ident())
                            P.op("act", "activation", cwT.k(c * 4 + tt)[0:64, 0, ts_], pst[0:64, 0:128], AF.Copy)
                            P.op("dve", "tensor_tensor", cwT.k(c * 4 + tt)[0:64, 1, ts_], pst[0:64, 0:128], cwT.k(c * 4 + tt)[0:64, 0, ts_], ALU.subtract)
                        P.barrier()
                if f"h2_{l}" in dbg_d:
                    with ExitStack() as st:
                        tf = P.sb([128, S], F32, "dbgtf2", st)
                        for kc in range(KC):
                            P.op("dve", "tensor_copy", tf[:], h2[:, kc, :])
                            P.dma("sp", dbg_d[f"h2_{l}"].view(dbg_d[f"h2_{l}"].t[kc * 128:(kc + 1) * 128, :]), tf[:])
                        P.barrier()
                if f"cw{l}" in dbg_d:
                    pass
                with ExitStack() as st:
                    EW = [P.sb([128, 6144], BF16, f"ew{i}", st) for i in range(2)]
                    SEL = [P.sb([64, 128], BF16, f"sel{i}", st) for i in range(2)]
                    cwb = [P.sb([128, T], F32, f"cwb{i}", st) for i in range(2)]
                    sa = [P.sb([128, T], F32, f"m_sa{i}", st) for i in range(2)]
                    gT = [P.sb([128, 2, T], BF16, f"m_gT{i}", st) for i in range(2)]
                    NE = 65 if stop != "noexp" else 0
                    mb = banks
                    porot = [0]

                    def ew_views(e):
                        ew = EW[e % 2]
                        w1t = lambda k, f: V(ew, ew.t[:, k * 256 + f * 128:k * 256 + (f + 1) * 128], None)
                        w3t = lambda k, f: V(ew, ew.t[:, 2048 + k * 256 + f * 128:2048 + k * 256 + (f + 1) * 128], None)
                        w2t = lambda f, oc: V(ew, ew.t[:, 4096 + f * 1024 + oc * 128:4096 + f * 1024 + (oc + 1) * 128], None)
                        return ew, w1t, w3t, w2t

                    def load_expert(e):
                        ew = EW[e % 2]
                        w1 = V(ew, ew.t[:, 0:2048].rearrange("p (k n) -> p k n", k=8), None)
                        w3 = V(ew, ew.t[:, 2048:4096].rearrange("p (k n) -> p k n", k=8), None)
                        w2 = V(ew, ew.t[:, 4096:6144].rearrange("p (k n) -> p k n", k=2), None)
                        if e < 64:
                            s1, s3, s2 = e1_d.t[l][e], e3_d.t[l][e], e2_d.t[l][e]
                        else:
                            s1, s3, s2 = s1_d.t[l], s3_d.t[l], s2_d.t[l]
                        P.dma("pool", w1, V(src_ap_buf, s1.rearrange("(k p) n -> p k n", p=128), None))
                        P.dma("pool", w3, V(src_ap_buf, s3.rearrange("(k p) n -> p k n", p=128), None))
                        P.dma("pool", w2, V(src_ap_buf, s2.rearrange("(k p) n -> p k n", p=128), None))

                    def stage1(i):
                        e, c = divmod(i, NCH)
                        cs = slice(c * T, (c + 1) * T)
                        ew, w1t, w3t, w2t = ew_views(e)
                        i2 = i % 2
                        if c == 0:
                            if e < 64:
                                P.op("dve", "tensor_scalar", SEL[e % 2][:], ones(64, 128), cst[0:64, e:e + 1], None, ALU.mult)
                        if e < 64:
                            P.mm(mb[0][:], SEL[e % 2][:], cwT[0:64, 0, cs], True, False)
                            P.mm(mb[0][:], SEL[e % 2][:], cwT[0:64, 1, cs], False, True)
                            P.op("act", "activation", cwb[i2][:], mb[0][:], AF.Copy)
                        for f in range(2):
                            pa = mb[1 + 2 * f]; pb = mb[2 + 2 * f]
                            for kc in range(KC):
                                P.mm(pa[:], w1t(kc, f), h2[:, kc, cs], kc == 0, kc == KC - 1)
                            for kc in range(KC):
                                P.mm(pb[:], w3t(kc, f), h2[:, kc, cs], kc == 0, kc == KC - 1)
                            s_ = sa[f]
                            P.op("act", "activation", s_[:], pa[:], AF.Silu)
                            if e < 64:
                                P.op("dve", "tensor_tensor", s_[:], s_[:], pb[:], ALU.mult)
                                P.op("pool", "tensor_tensor", gT[i2].k(f)[:, f, :], s_[:], cwb[i2][:], ALU.mult)
                            else:
                                P.op("dve", "tensor_tensor", gT[i2].k(f)[:, f, :], s_[:], pb[:], ALU.mult)

                    def stage2(i):
                        e, c = divmod(i, NCH)
                        cs = slice(c * T, (c + 1) * T)
                        ew, w1t, w3t, w2t = ew_views(e)
                        i2 = i % 2
                        for oc in range(8):
                            po = mb[5 + porot[0] % 3]
                            porot[0] += 1
                            for f in range(2):
                                P.mm(po[:], w2t(f, oc), gT[i2].k(f)[:, f, :], f == 0, f == 1)
                            P.op("dve", "scalar_tensor_tensor", xT.k(oc)[:, oc, cs], po[:], mc(40 + oc), xT.k(oc)[:, oc, cs], ALU.mult, ALU.add)

                    NI = NE * NCH
                    if NI:
                        load_expert(0)
                        load_expert(1)
                        stage1(0)
                    for i in range(NI):
                        if i + 1 < NI:
                            stage1(i + 1)
                        stage2(i)
                        if i % NCH == NCH - 1 and i // NCH + 2 < NE:
                            load_expert(i // NCH + 2)
                    P.barrier()
                for kc in range(KC):
                    P.dma("sp", xs.view(xs.t[kc * 128:(kc + 1) * 128, :]), xT.k(kc)[:, kc, :])
                P.barrier()
            dump(f"x2_{l}", lambda kc: xs.view(xs.t[kc * 128:(kc + 1) * 128, :]))
            P.barrier()

    with ExitStack() as st:
        fn = P.sb([128, 8], F32, "fn", st)
        ob = [P.sb([128, T], F32, f"ob{i}", st) for i in range(4)]
        P.dma("sp", fn[:], fn_d[:])
        cnt = [0]
        for c in range(NCH):
            cs = slice(c * T, (c + 1) * T)
            with ExitStack() as st2:
                def dst(kc):
                    return ob[kc % 4][:]
                xf = P.sb([128, KC, T], F32, "xfin", st2)
                P.dma("sp", xf[:], xs.view(xs.t[:, cs].rearrange("(k p) t -> p k t", p=128)))
                sq = [P.sb([128, T], F32, f"fsq{i}", st2) for i in range(2)]
                rs = P.sb([128, T], F32, "frs", st2)
                ss = bk()
                for kc in range(KC):
                    q = sq[kc % 2]
                    P.op("act", "activation", q[:], xf[:, kc, :], AF.Square)
                    P.mm(ss[:], ones(), q[:], kc == 0, kc == KC - 1)
                P.op("act", "activation", rs[:], ss[:], AF.Ln, scale=1.0 / D, bias=EPS)
                P.op("act", "activation", rs[:], rs[:], AF.Exp, scale=-0.5)
                for kc in range(KC):
                    q = sq[kc % 2]
                    o = ob[kc % 4]
                    P.op("dve", "tensor_tensor", q[:], xf[:, kc, :], rs[:], ALU.mult)
                    P.op("act", "activation", o[:], q[:], AF.Identity, scale=fn[:, kc:kc + 1])
                    P.dma("sp", out_d.view(out_d.t[kc * 128:(kc + 1) * 128, cs]), o[:])
                P.barrier()
    P.finish()
    return P


def _pack_vec(inp, l):
    v = np.zeros((128, NV), np.float32)

    def put(name, arr):
        o, w = VOFF[name]
        arr = np.asarray(arr, np.float32)
        assert arr.shape[1] == w, (name, arr.shape)
        v[:arr.shape[0], o:o + w] = arr

    c128 = lambda a, n: np.asarray(a).reshape(n, 128).T
    c64 = lambda a: np.asarray(a).reshape(8, 64).T
    put("ada_b", c128(inp["ada_b"][l], 48)); put("norm1", c128(inp["norm1"][l], 8)); put("norm2", c128(inp["norm2"][l], 8))
    put("caw", np.asarray(inp["conv_a_w"][l]).T.reshape(4, 128, 31).transpose(1, 0, 2).reshape(128, 124))
    put("cab", c128(inp["conv_a_b"][l], 4)); put("lng", c128(inp["ln_a_g"][l], 4)); put("lnb", c128(inp["ln_a_b"][l], 4))
    put("ccw", np.asarray(inp["conv_c_w"][l]).T.reshape(8, 128, 4).transpose(1, 0, 2).reshape(128, 32))
    put("ccb", c128(inp["conv_c_b"][l], 8)); put("lba", c128(inp["lru_ba"][l], 8)); put("lbx", c128(inp["lru_bx"][l], 8))
    put("lam", c128(inp["lru_lambda"][l], 8))
    mu = np.asarray(inp["mu_b"][l])
    put("mu_w", mu[1536:1600].reshape(64, 1)); put("mu_a", mu[1600:1664].reshape(64, 1)); put("mu_g", mu[1664:1792].reshape(128, 1))
    put("mu_r", c64(mu[0:512])); put("mu_k", c64(mu[512:1024])); put("mu_v", c64(mu[1024:1536]))
    for n, k in (("w0", "w0"), ("a0", "a0"), ("k_k", "k_k"), ("k_a", "k_a"), ("gn_g", "gn_b_g"), ("gn_b", "gn_b_b")):
        put(n, c64(inp[k][l]))
    put("r_k", np.asarray(inp["r_k"][l]).T)
    return v


def _consts():
    c = np.zeros((128, NCST), np.float32)
    c[:, 0:128] = np.eye(128, dtype=np.float32)
    c[:, 128:256] = 1.0
    s = np.arange(64)[:, None]; t = np.arange(64)[None, :]
    c[0:64, 256:320] = (s < t); c[0:64, 320:384] = (s > t); c[0:64, 384:448] = (s <= t)
    c[:, 448:512] = 1.0; c[:, 448] = 0.0
    return c


_PROG_CACHE = {}


def _get_prog(dbg=(), stop=None, nlayers=2):
    key = (tuple(dbg), stop, nlayers)
    if key not in _PROG_CACHE:
        _PROG_CACHE[key] = build(dbg, stop, nlayers)
    return _PROG_CACHE[key]


def _in_maps(inp, cores):
    f = lambda a: np.ascontiguousarray(np.asarray(a, np.float32))
    vec = np.stack([_pack_vec(inp, l) for l in range(2)])
    shared = {
        "vec": vec, "fnorm": f(np.asarray(inp["final_norm"]).reshape(8, 128).T),
        "rbias": f(np.broadcast_to(np.asarray(inp["router_bias"])[:, None, :], (2, 128, 64))), "cst": _consts(),
        "ada_w": f(inp["ada_w"]), "w_in": f(inp["w_in"]), "proj_a": f(inp["proj_a"]), "proj_b": f(inp["proj_b"]),
        "proj_c": f(inp["proj_c"]), "w_out": f(inp["w_out"]), "w_up": f(inp["w_up"]), "a_up": f(inp["a_up"]),
        "g_up": f(inp["g_up"]), "lru_wa": f(inp["lru_wa"]), "lru_wx": f(inp["lru_wx"]), "router_w": f(inp["router_w"]),
        "exp_w1": f(inp["exp_w1"]), "exp_w3": f(inp["exp_w3"]), "exp_w2": f(inp["exp_w2"]),
        "sh_w1": f(inp["sh_w1"]), "sh_w3": f(inp["sh_w3"]), "sh_w2": f(inp["sh_w2"]),
    }
    maps = []
    for b in cores:
        m = dict(shared)
        m["xT"] = f(np.asarray(inp["x"][b]).T)
        m["cT"] = f(np.asarray(inp["c"][b]).reshape(8, 128).T)
        maps.append(m)
    return maps


def kernel(**inputs):
    P = _get_prog()
    maps = _in_maps(inputs, range(8))
    res = run_bass_kernel_spmd(P.nc, maps, core_ids=list(range(8)))
    out = np.stack([np.asarray(r["outT"]).T for r in res.results]).astype(np.float32)
    return out
```

```python
import numpy as np
from contextlib import ExitStack
import concourse.bass as bass
import concourse.mybir as mybir
from concourse.bass_utils import run_bass_kernel_spmd

F32 = mybir.dt.float32
BF16 = mybir.dt.bfloat16
AF = mybir.ActivationFunctionType
ALU = mybir.AluOpType

EPOCH = 30000
NDS = 24


class Tok:
    __slots__ = ("eng", "sem", "sid", "val")

    def __init__(self, eng, sem, sid, val):
        self.eng, self.sem, self.sid, self.val = eng, sem, sid, val


class V:
    __slots__ = ("buf", "ap", "key")

    def __init__(self, buf, ap, key):
        self.buf, self.ap, self.key = buf, ap, key


class _Keyed:
    def __init__(self, buf, key):
        self.buf, self.key = buf, key

    def __getitem__(self, idx):
        return V(self.buf, self.buf.t[idx], self.key)


class Buf:
    def __init__(self, t, name, psum=False):
        self.t = t
        self.name = name
        self.psum = psum
        self.st = {}

    def __getitem__(self, idx):
        return V(self, self.t[idx], None)

    def k(self, key):
        return _Keyed(self, key)

    def view(self, ap, key=None):
        return V(self, ap, key)

    def _keys(self, key):
        if key is None:
            return list(self.st.keys())
        ks = []
        if key in self.st:
            ks.append(key)
        if None in self.st:
            ks.append(None)
        return ks

    def rdeps(self, key):
        if self.psum:
            return self.wdeps(key)
        return [self.st[k][0] for k in self._keys(key) if self.st[k][0] is not None]

    def wdeps(self, key):
        out = []
        for k in self._keys(key):
            w, rs = self.st[k]
            if w is not None:
                out.append(w)
            out.extend(rs)
        return out

    def add_read(self, key, tok):
        self.st.setdefault(key, [None, []])[1].append(tok)

    def add_write(self, key, tok):
        if key is None:
            self.st = {None: [tok, []]}
        else:
            self.st[key] = [tok, []]


class Prog:
    def __init__(self):
        self.nc = bass.Bass("TRN2", target_bir_lowering=False)
        nc = self.nc
        self.es = ExitStack()
        self.eng = {"pe": nc.tensor, "act": nc.scalar, "dve": nc.vector,
                    "pool": nc.gpsimd, "sp": nc.sync}
        self.esem = {e: [] for e in self.eng}
        self.ecnt = {e: 0 for e in self.eng}
        self.seen = {e: {} for e in self.eng}
        self.dsem = [self.es.enter_context(nc.semaphore(f"dq{i}")) for i in range(4 * NDS)]
        self.dcnt = [0] * (4 * NDS)
        self.drr = {"hw": 0, "sw": 0, "rg": 0, "rh": 0}
        self.nbuf = 0
        self.psum_rot = []
        self.psum_i = 0
        self.ninst = 0

    def sb(self, shape, dtype, name=None, stack=None):
        self.nbuf += 1
        name = f"{name or 'sb'}_{self.nbuf}"
        t = (stack or self.es).enter_context(self.nc.sbuf_tensor(name, list(shape), dtype))
        return Buf(t, name)

    def ps(self, shape, dtype=F32, name=None, stack=None):
        self.nbuf += 1
        name = f"{name or 'ps'}_{self.nbuf}"
        t = (stack or self.es).enter_context(self.nc.psum_tensor(name, list(shape), dtype))
        return Buf(t, name, psum=True)

    def dram(self, name, shape, dtype, kind):
        t = self.nc.dram_tensor(name, list(shape), dtype, kind=kind)
        return Buf(t.ap(), name)

    def _etok(self, e, n):
        idx = (n - 1) // EPOCH
        while len(self.esem[e]) <= idx:
            s = self.es.enter_context(self.nc.semaphore(f"e_{e}_{len(self.esem[e])}"))
            self.esem[e].append(s)
        return Tok(e, self.esem[e][idx], ("e", e, idx), (n - 1) % EPOCH + 1)

    def wait(self, e, tok):
        if tok is None:
            return
        if tok.eng == "pe" and e == "pe":
            return
        if self.seen[e].get(tok.sid, 0) >= tok.val:
            return
        self.eng[e].wait_ge(tok.sem, tok.val)
        self.seen[e][tok.sid] = tok.val

    def issue(self, e, fn, reads, writes):
        toks = []
        for v in reads:
            toks.extend(v.buf.rdeps(v.key))
        for v in writes:
            toks.extend(v.buf.wdeps(v.key))
        for t in toks:
            self.wait(e, t)
        inst = fn()
        self.ecnt[e] += 1
        self.ninst += 1
        tok = self._etok(e, self.ecnt[e])
        inst.then_inc(tok.sem, 1)
        for v in reads:
            v.buf.add_read(v.key, tok)
        for v in writes:
            v.buf.add_write(v.key, tok)
        return tok

    def op(self, e, fname, out, *args, **kw):
        reads = [a for a in args if isinstance(a, V)] + [a for a in kw.values() if isinstance(a, V)]
        cv = lambda a: a.ap if isinstance(a, V) else a
        f = getattr(self.eng[e], fname)
        fn = lambda: f(cv(out), *[cv(a) for a in args], **{k: cv(v) for k, v in kw.items()})
        return self.issue(e, fn, reads, [out])

    def mm(self, out, lhsT, rhs, start=True, stop=True, **kw):
        fn = lambda: self.nc.tensor.matmul(out.ap, lhsT.ap, rhs.ap, start=start, stop=stop, **kw)
        return self.issue("pe", fn, [lhsT, rhs], [out])

    def transpose(self, out, in_, ident):
        fn = lambda: self.nc.tensor.transpose(out.ap, in_.ap, ident.ap)
        return self.issue("pe", fn, [in_, ident], [out])

    def dma(self, q, out, in_, ring=False, **kw):
        toks = list(in_.buf.rdeps(in_.key)) + list(out.buf.wdeps(out.key))
        kind = ("rg" if ring else "sw") if q == "pool" else ("rh" if ring else "hw")
        i = self.drr[kind] + {"hw": 0, "sw": NDS, "rg": 2 * NDS, "rh": 3 * NDS}[kind]
        self.drr[kind] = (self.drr[kind] + 1) % NDS
        if self.dcnt[i] > 0:
            toks.append(Tok("dma", self.dsem[i], ("d", i), 16 * self.dcnt[i]))
        for t in toks:
            self.wait(q, t)
        inst = self.eng[q].dma_start(out=out.ap, in_=in_.ap, **kw)
        self.dcnt[i] += 1
        self.ninst += 1
        inst.then_inc(self.dsem[i], 16)
        tok = Tok("dma", self.dsem[i], ("d", i), 16 * self.dcnt[i])
        in_.buf.add_read(in_.key, tok)
        out.buf.add_write(out.key, tok)
        return tok

    def barrier(self):
        toks = [self._etok(e, self.ecnt[e]) for e in self.eng if self.ecnt[e] > 0]
        toks += [Tok("dma", self.dsem[i], ("d", i), 16 * self.dcnt[i]) for i in range(2 * NDS) if self.dcnt[i] > 0]
        for e in self.eng:
            for t in toks:
                if t.eng == e:
                    continue
                self.wait(e, t)

    def finish(self):
        toks = [Tok("dma", self.dsem[i], ("d", i), 16 * self.dcnt[i]) for i in range(4 * NDS) if self.dcnt[i] > 0]
        toks += [self._etok(e, self.ecnt[e]) for e in self.eng if self.ecnt[e] > 0 and e != "sp"]
        for t in toks:
            self.wait("sp", t)


def run_rr(gens, width):
    pending = list(gens)
    active = []
    while pending or active:
        while pending and len(active) < width:
            active.append(pending.pop(0))
        for g in list(active):
            try:
                next(g)
            except StopIteration:
                active.remove(g)


class WRing:
    def __init__(self, P, nslot, stack):
        self.P = P
        self.n = nslot
        self.slots = [P.sb([128, 8, 512], BF16, f"wr{i}", stack) for i in range(nslot)]
        self.plan = []
        self.issued = 0
        self.got = 0
        self.rel = []

    def add(self, name, loader):
        self.plan.append((name, loader))
        self.rel.append(False)

    def pump(self):
        while self.issued < len(self.plan):
            j = self.issued
            if j >= self.n and not self.rel[j - self.n]:
                break
            self.plan[j][1](self.slots[j % self.n])
            self.issued += 1

    def get(self, name):
        j = self.got
        assert self.plan[j][0] == name, (self.plan[j][0], name)
        self.pump()
        assert self.issued > j, ("weight ring stall", name)
        self.got += 1
        return j, self.slots[j % self.n]

    def release(self, j):
        self.rel[j] = True
        self.pump()


D = 1024
S = 2048
T = 512
NCH = S // T
KC = 8
TB = 128
L = 64
EPS = 1e-6
CDEC = 0.6065306597126334

VOFF = {}
_o = 0
for _n, _w in (("ada_b", 48), ("norm1", 8), ("norm2", 8), ("caw", 124), ("cab", 4), ("lng", 4), ("lnb", 4),
               ("ccw", 32), ("ccb", 8), ("lba", 8), ("lbx", 8), ("lam", 8), ("mu_w", 1), ("mu_a", 1), ("mu_g", 1),
               ("mu_r", 8), ("mu_k", 8), ("mu_v", 8), ("w0", 8), ("a0", 8), ("k_k", 8), ("k_a", 8),
               ("r_k", 8), ("gn_g", 8), ("gn_b", 8)):
    VOFF[_n] = (_o, _w)
    _o += _w
NV = _o
COFF = {"ident": (0, 128), "ones": (128, 128), "mut": (256, 64), "mlt": (320, 64), "mui": (384, 64), "rst": (448, 64)}
NCST = 512


def rwkv_chunk(P, l, c, st, E):
    bk, vec, drvB, cst, identb, onesb = E["bk"], E["vec"], E["drvB"], E["cst"], E["identb"], E["onesb"]
    ring, hT, merged, lor, gup = E["ring"], E["hT"], E["merged"], E["lor"], E["gup"]
    prevq, pextw, pexta, pextg, Sf, Sb = E["prevq"], E["pextw"], E["pexta"], E["pextg"], E["Sf"], E["Sb"]
    MUT, MLT, MUI, IDH, RST, bankT = E["MUT"], E["MLT"], E["MUI"], E["IDH"], E["RST"], E["bankT"]
    src, w_in_ap = E["src_ap_buf"], E["w_in_ap"]
    vcol, drv = E["vcol"], E["drv"]
    w_up_d, a_up_d, g_up_d, proj_b_d = E["w_up_d"], E["a_up_d"], E["g_up_d"], E["proj_b_d"]
    ones = lambda n=128, m=128: cst[0:n, 128:128 + m]
    HB = [64, 8, TB]

    def bc(name, n=TB):
        o, w = VOFF[name]
        b = vec[l]
        return V(b, b.t[0:64, o:o + 8].unsqueeze(2).to_broadcast([64, 8, n]), None)

    def bcd(j0, n=TB):
        b = drvB[l]
        return V(b, b.t[0:64, j0:j0 + 8].unsqueeze(2).to_broadcast([64, 8, n]), None)

    jr, wr_ = ring.get("B_r"); jk, wk_ = ring.get("B_k"); jv_, wv_ = ring.get("B_v"); jl, wl_ = ring.get("B_lora")
    yg = P.sb([64, 8, T], BF16, "b_yg", st)
    F = {n: P.sb(HB, F32, f"b_{n}", st) for n in ("r", "k", "v", "a", "sg", "e1", "e2", "e3", "kk", "t0", "g")}
    F["t1"] = F["sg"]; F["bon"] = F["a"]; F["y"] = F["r"]
    pq = P.sb([64, 8, 1 + TB], F32, "b_pq", st)
    mu3 = {"w": (vcol(l, "mu_w", 0, 1, 64), drv[l][0:64, 32:33]), "a": (vcol(l, "mu_a", 0, 1, 64), drv[l][0:64, 33:34]),
           "g": (vcol(l, "mu_g"), drv[l][:, 34:35])}
    H = {n: P.sb(HB, BF16, f"b_{n}", st) for n in ("rt", "kkt", "bt", "kt", "vb")}
    X = {n: P.sb([64, 512], BF16, f"b_{n}", st) for n in ("XTn", "UT")}
    XJ = []
    for j_ in range(TB // L):
        d_ = {n: P.sb([64, 512], BF16, f"b_{n}{j_}", st) for n in ("Am", "ATm", "NKm", "MBR", "MKR", "A2", "A2T", "A2b", "A2Tb", "Tb", "VT", "bT", "kT")}
        d_["Tf"] = P.sb([64, 512], F32, f"b_Tf{j_}", st)
        XJ.append(d_)
    lt = {n: P.sb([128, TB], F32, f"b_l{n}", st) for n in ("w", "a", "g", "t")}
    lb = {n: P.sb([128, TB], BF16, f"b_lb{n}", st) for n in ("w", "a", "g")}
    colbase = {"r": (wr_, 0), "k": (wk_, 0), "v": (wv_, 0)}
    v4 = lambda b, hh: V(b, b.t[0:64, 0:512].rearrange("p (h t) -> p h t", h=4), None)
    fl = lambda b: V(b, b.t[0:64].rearrange("p h t -> p (h t)"), None)
    hv = lambda b, hh: b[0:64, hh * 4:(hh + 1) * 4, :]

    for blk in range(T // TB):
        t0 = blk * TB
        hs = lambda kc: hT[:, kc, t0:t0 + TB]
        for q in "rkv":
            ws, cb = colbase[q]
            for hh in range(2):
                b_ = bk()
                for hl in range(4):
                    h = hh * 4 + hl
                    for kc in range(KC):
                        P.mm(b_[0:64, hl * TB:(hl + 1) * TB], ws[:, kc, cb + h * 64:cb + (h + 1) * 64], hs(kc), kc == 0, kc == KC - 1)
                P.op("act", "activation", pq[0:64, hh * 4:(hh + 1) * 4, 1:1 + TB], v4(b_, hh), AF.Copy)
            qi = "rkv".index(q)
            P.op("pool", "tensor_copy", pq[0:64, :, 0:1], V(prevq, prevq.t[0:64, qi, :].unsqueeze(2), None))
            mu = {"r": "mu_r", "k": "mu_k", "v": "mu_v"}[q]
            om = {"r": 0, "k": 8, "v": 16}[q]
            eq_ = "dve" if q == "r" else "pool"
            tq_ = F["t0"] if q == "r" else F["kk"]
            P.op(eq_, "tensor_tensor", tq_[:], pq[0:64, :, 0:TB], bc(mu), ALU.mult)
            P.op(eq_, "tensor_tensor", F[q][:], pq[0:64, :, 1:1 + TB], bcd(om), ALU.mult)
            P.op(eq_, "tensor_tensor", F[q][:], F[q][:], tq_[:], ALU.add)
            P.op("act", "activation", V(prevq, prevq.t[0:64, qi, :].unsqueeze(2), None), pq[0:64, :, TB:TB + 1], AF.Copy)
        b_ = bk()
        for kc in range(KC):
            P.mm(b_[0:64, 0:TB], wl_[:, kc, 0:64], hs(kc), kc == 0, kc == KC - 1)
        for kc in range(KC):
            P.mm(b_[0:64, TB:2 * TB], wl_[:, kc, 64:128], hs(kc), kc == 0, kc == KC - 1)
        for kc in range(KC):
            P.mm(b_[:, 2 * TB:3 * TB], wl_[:, kc, 128:256], hs(kc), kc == 0, kc == KC - 1)
        for (pe_, n, off, np_) in ((pextw, "w", 0, 64), (pexta, "a", TB, 64), (pextg, "g", 2 * TB, 128)):
            P.op("act", "activation", pe_[0:np_, 1:1 + TB], b_[0:np_, off:off + TB], AF.Copy)
            P.op("dve", "tensor_scalar", lt["t"][0:np_, :], pe_[0:np_, 0:TB], mu3[n][0], None, ALU.mult)
            P.op("dve", "scalar_tensor_tensor", lt[n][0:np_, :], pe_[0:np_, 1:1 + TB], mu3[n][1], lt["t"][0:np_, :], ALU.mult, ALU.add)
            P.op("act", "activation", pe_[0:np_, 0:1], pe_[0:np_, TB:TB + 1], AF.Copy)
        P.op("act", "activation", lb["w"][0:64, :], lt["w"][0:64, :], AF.Tanh)
        P.op("act", "activation", lb["a"][0:64, :], lt["a"][0:64, :], AF.Copy)
        P.op("act", "activation", lb["g"][:], lt["g"][:], AF.Sigmoid)
        for hh in range(2):
            bw = bk(); ba = bk(); bg = bk()
            for hl in range(4):
                h = hh * 4 + hl
                P.mm(bw[0:64, hl * TB:(hl + 1) * TB], lor[0:64, h * 64:(h + 1) * 64], lb["w"][0:64, :])
                P.mm(ba[0:64, hl * TB:(hl + 1) * TB], lor[0:64, 512 + h * 64:512 + (h + 1) * 64], lb["a"][0:64, :])
                P.mm(bg[0:64, hl * TB:(hl + 1) * TB], gup[:, h * 64:(h + 1) * 64], lb["g"][:])
            o, _ = VOFF["w0"]
            w0b = V(vec[l], vec[l].t[0:64, o + hh * 4:o + hh * 4 + 4].unsqueeze(2).to_broadcast([64, 4, TB]), None)
            o, _ = VOFF["a0"]
            a0b = V(vec[l], vec[l].t[0:64, o + hh * 4:o + hh * 4 + 4].unsqueeze(2).to_broadcast([64, 4, TB]), None)
            P.op("dve", "tensor_tensor", hv(F["sg"], hh), v4(bw, hh), w0b, ALU.add)
            P.op("dve", "tensor_tensor", hv(F["a"], hh), v4(ba, hh), a0b, ALU.add)
            P.op("act", "activation", hv(F["g"], hh), v4(bg, hh), AF.Copy)
        P.op("act", "activation", F["sg"][:], F["sg"][:], AF.Sigmoid)
        P.op("act", "activation", F["a"][:], F["a"][:], AF.Sigmoid)
        P.op("dve", "tensor_tensor_scan", fl(F["e1"]), RST[0:64, :], fl(F["sg"]), 0.0, ALU.mult, ALU.add)
        P.op("dve", "tensor_tensor", F["e3"][:], F["e1"][:], F["sg"][:], ALU.subtract)
        P.op("act", "activation", F["e2"][:], F["e1"][:], AF.Exp, scale=CDEC)
        P.op("act", "activation", F["e1"][:], F["e1"][:], AF.Exp, scale=-CDEC)
        P.op("act", "activation", F["e3"][:], F["e3"][:], AF.Exp, scale=-CDEC)
        P.op("dve", "tensor_tensor", F["kk"][:], F["k"][:], bc("k_k"), ALU.mult)
        P.op("act", "activation", H["rt"][:], F["kk"][:], AF.Square)
        for hh in range(2):
            b_ = bk()
            P.mm(b_[0:64, :], onesb[:], V(H["rt"], H["rt"].t[0:64, hh * 4:(hh + 1) * 4, :].rearrange("p h t -> p (h t)"), None))
            P.op("dve", "tensor_scalar", hv(F["t1"], hh), v4(b_, hh), 1e-24, None, ALU.max)
        P.op("act", "activation", F["t1"][:], F["t1"][:], AF.Ln)
        P.op("act", "activation", F["t1"][:], F["t1"][:], AF.Exp, scale=-0.5)
        P.op("dve", "tensor_tensor", F["kk"][:], F["kk"][:], F["t1"][:], ALU.mult)
        P.op("dve", "tensor_scalar", F["t0"][:], F["a"][:], -1.0, None, ALU.add)
        P.op("dve", "tensor_tensor", F["t0"][:], F["t0"][:], bc("k_a"), ALU.mult)
        P.op("dve", "scalar_tensor_tensor", fl(F["k"]), fl(F["t0"]), 1.0, fl(F["k"]), ALU.add, ALU.mult)
        P.op("dve", "tensor_tensor", H["rt"][:], F["r"][:], F["e1"][:], ALU.mult)
        P.op("pool", "tensor_tensor", H["kkt"][:], F["kk"][:], F["e3"][:], ALU.mult)
        P.op("dve", "tensor_tensor", F["t0"][:], F["kk"][:], F["a"][:], ALU.mult)
        P.op("pool", "tensor_tensor", H["bt"][:], F["t0"][:], F["e2"][:], ALU.mult)
        P.op("dve", "tensor_tensor", H["kt"][:], F["k"][:], F["e2"][:], ALU.mult)
        P.op("act", "activation", H["vb"][:], F["v"][:], AF.Copy)
        P.op("dve", "tensor_tensor", F["t0"][:], F["r"][:], F["k"][:], ALU.mult)
        P.op("dve", "tensor_tensor", F["t0"][:], F["t0"][:], bc("r_k"), ALU.mult)
        for hh in range(2):
            b_ = bk()
            P.mm(b_[0:64, :], ones(64, 64), V(F["t0"], F["t0"].t[0:64, hh * 4:(hh + 1) * 4, :].rearrange("p h t -> p (h t)"), None))
            P.op("dve", "tensor_tensor", hv(F["bon"], hh), v4(b_, hh), hv(F["v"], hh), ALU.mult)
        hm = lambda b, h: b[0:64, h * 64:(h + 1) * 64]

        def nonseq(j):
            sl = slice(j * L, (j + 1) * L)
            Xj = XJ[j]
            for (srcb, dstb, half) in ((H["vb"], Xj["VT"], 0), (H["bt"], Xj["bT"], 1)):
                for h in range(8):
                    P.transpose(bankT[0:64, half * 512 + h * 64:half * 512 + (h + 1) * 64], srcb[0:64, h, sl], identb[0:64, 0:64])
                P.op("act", "activation", dstb[:], bankT[0:64, half * 512:(half + 1) * 512], AF.Copy)
            yield
            for h in range(8):
                P.transpose(bankT[0:64, h * 64:(h + 1) * 64], H["kt"][0:64, h, sl], identb[0:64, 0:64])
            P.op("act", "activation", Xj["kT"][:], bankT[0:64, 0:512], AF.Copy)
            yield
            specs = (("Am", H["bt"], H["kkt"], MUT), ("ATm", H["kkt"], H["bt"], MLT), ("NKm", H["kt"], H["kkt"], MUT),
                     ("MBR", H["bt"], H["rt"], MUI), ("MKR", H["kt"], H["rt"], MUI))
            pend = []
            for (nm, lh, rh, msk) in specs[:2]:
                b_ = bk()
                for h in range(8):
                    P.mm(hm(b_, h), lh[0:64, h, sl], rh[0:64, h, sl])
                pend.append((nm, b_, msk))
            yield
            for (nm, b_, msk) in pend:
                P.op("dve", "tensor_tensor", Xj[nm][:], b_[0:64, :], msk[:], ALU.mult)
            P.op("dve", "tensor_tensor", Xj["Tf"][:], IDH[:], Xj["Am"][:], ALU.subtract)
            P.op("act", "activation", Xj["Tb"][:], Xj["Tf"][:], AF.Copy)
            pend = []
            for (nm, lh, rh, msk) in specs[2:]:
                b_ = bk()
                for h in range(8):
                    P.mm(hm(b_, h), lh[0:64, h, sl], rh[0:64, h, sl])
                pend.append((nm, b_, msk))
            yield
            for (nm, b_, msk) in pend:
                P.op("dve", "tensor_tensor", Xj[nm][:], b_[0:64, :], msk[:], ALU.mult)
            Ak, AkT = Xj["Am"], Xj["ATm"]
            for lev in range(5):
                b2t = bk()
                for h in range(8):
                    P.mm(hm(b2t, h), hm(Ak, h), hm(AkT, h))
                if lev < 4:
                    b2 = bk()
                    for h in range(8):
                        P.mm(hm(b2, h), hm(AkT, h), hm(Ak, h))
                yield
                n2, n2t = ("A2", "A2T") if lev % 2 == 0 else ("A2b", "A2Tb")
                P.op("act", "activation", Xj[n2t][:], b2t[0:64, :], AF.Copy)
                if lev < 4:
                    P.op("dve", "tensor_copy", Xj[n2][:], b2[0:64, :])
                yield
                btp = bk()
                for h in range(8):
                    P.mm(hm(btp, h), hm(Xj[n2t], h), hm(Xj["Tb"], h))
                yield
                P.op("dve", "tensor_tensor", Xj["Tf"][:], Xj["Tf"][:], btp[0:64, :], ALU.add)
                P.op("act", "activation", Xj["Tb"][:], Xj["Tf"][:], AF.Copy)
                Ak, AkT = Xj[n2], Xj[n2t]

        def seq(j):
            sl = slice(j * L, (j + 1) * L)
            Xj = XJ[j]
            bx = bk()
            for h in range(8):
                P.mm(hm(bx, h), H["kkt"][0:64, h, sl], hm(Sb, h), True, False)
                P.mm(hm(bx, h), hm(Xj["NKm"], h), hm(Xj["VT"], h), False, True)
            P.op("act", "activation", X["XTn"][:], bx[0:64, :], AF.Identity, scale=-1.0)
            bu = bk()
            for h in range(8):
                P.mm(hm(bu, h), hm(Xj["Tb"], h), hm(X["XTn"], h))
            P.op("act", "activation", X["UT"][:], bu[0:64, :], AF.Copy)
            by = bk()
            for h in range(8):
                P.mm(hm(by, h), hm(Sb, h), H["rt"][0:64, h, sl], True, False)
                P.mm(hm(by, h), hm(X["UT"], h), hm(Xj["MBR"], h), False, False)
                P.mm(hm(by, h), hm(Xj["VT"], h), hm(Xj["MKR"], h), False, True)
            P.op("act", "activation", F["y"][0:64, :, sl], V(by, by.t[0:64, 0:512].rearrange("p (h t) -> p h t", h=8), None), AF.Copy)
            bs_ = bk()
            for h in range(8):
                P.mm(hm(bs_, h), hm(Xj["bT"], h), hm(X["UT"], h), True, False)
                P.mm(hm(bs_, h), hm(Xj["kT"], h), hm(Xj["VT"], h), False, True)
            P.op("dve", "tensor_tensor", Sf[:], Sf[:], bs_[0:64, :], ALU.add)
            pl = V(F["e1"], F["e1"].t[0:64, :, j * L + L - 1:j * L + L].to_broadcast([64, 8, 64]), None)
            P.op("dve", "tensor_tensor", V(Sf, Sf.t[0:64, :].rearrange("p (h v) -> p h v", h=8), None),
                 V(Sf, Sf.t[0:64, :].rearrange("p (h v) -> p h v", h=8), None), pl, ALU.mult)
            P.op("act", "activation", Sb[:], Sf[:], AF.Copy)

        run_rr([nonseq(j) for j in range(TB // L)], 2)
        for j in range(TB // L):
            seq(j)
        P.op("act", "activation", H["rt"][:], F["y"][:], AF.Square)
        P.op("act", "activation", H["kkt"][:], F["y"][:], AF.Copy)
        for hh in range(2):
            b1 = bk(); b2_ = bk()
            P.mm(b1[0:64, :], onesb[:], V(H["kkt"], H["kkt"].t[0:64, hh * 4:(hh + 1) * 4, :].rearrange("p h t -> p (h t)"), None))
            P.mm(b2_[0:64, :], onesb[:], V(H["rt"], H["rt"].t[0:64, hh * 4:(hh + 1) * 4, :].rearrange("p h t -> p (h t)"), None))
            P.op("act", "activation", hv(F["t1"], hh), v4(b1, hh), AF.Identity, scale=1.0 / 64)
            P.op("act", "activation", hv(F["e2"], hh), v4(b2_, hh), AF.Identity, scale=1.0 / 64)
        P.op("dve", "tensor_tensor", F["e3"][:], F["t1"][:], F["t1"][:], ALU.mult)
        P.op("dve", "tensor_tensor", F["e2"][:], F["e2"][:], F["e3"][:], ALU.subtract)
        P.op("act", "activation", F["e2"][:], F["e2"][:], AF.Ln, bias=64e-5)
        P.op("act", "activation", F["e2"][:], F["e2"][:], AF.Exp, scale=-0.5)
        P.op("dve", "tensor_tensor", F["y"][:], F["y"][:], F["t1"][:], ALU.subtract)
        P.op("dve", "tensor_tensor", F["y"][:], F["y"][:], F["e2"][:], ALU.mult)
        P.op("dve", "tensor_tensor", F["y"][:], F["y"][:], bc("gn_g"), ALU.mult)
        P.op("dve", "tensor_tensor", F["y"][:], F["y"][:], bc("gn_b"), ALU.add)
        P.op("dve", "tensor_tensor", F["y"][:], F["y"][:], F["bon"][:], ALU.add)
        P.op("dve", "tensor_tensor", yg[0:64, :, t0:t0 + TB], F["y"][:], F["g"][:], ALU.mult)
    for j_ in (jr, jk, jv_, jl):
        ring.release(j_)
    E["proj_and_gate"](l, st, ring, "B", 8, 64, lambda k: yg[0:64, k, :], True, hT, merged, False)


def build(dbg=(), stop=None, nlayers=2):
    P = Prog()
    nc = P.nc

    def din(name, shape):
        return P.dram(name, shape, F32, "ExternalInput")

    xT_d = din("xT", [D, S]); cT_d = din("cT", [128, 8]); vec_d = din("vec", [2, 128, NV])
    fn_d = din("fnorm", [128, 8]); rb_d = din("rbias", [2, 128, 64]); cst_d = din("cst", [128, NCST])
    ada_w_d = din("ada_w", [2, D, 6 * D]); w_in_d = din("w_in", [2, D, 7936])
    proj_a_d = din("proj_a", [2, 512, D]); proj_b_d = din("proj_b", [2, 512, D]); proj_c_d = din("proj_c", [2, D, D])
    w_out_d = din("w_out", [2, D, D]); w_up_d = din("w_up", [2, 64, 512]); a_up_d = din("a_up", [2, 64, 512])
    g_up_d = din("g_up", [2, 128, 512]); lwa_d = din("lru_wa", [2, 8, 128, 128]); lwx_d = din("lru_wx", [2, 8, 128, 128])
    rw_d = din("router_w", [2, D, 64]); e1_d = din("exp_w1", [2, 64, D, 256]); e3_d = din("exp_w3", [2, 64, D, 256])
    e2_d = din("exp_w2", [2, 64, 256, D]); s1_d = din("sh_w1", [2, D, 256]); s3_d = din("sh_w3", [2, D, 256])
    s2_d = din("sh_w2", [2, 256, D])
    out_d = P.dram("outT", [D, S], F32, "ExternalOutput")
    dbg_d = {n: P.dram("dbg_" + n, [D, S], F32, "ExternalOutput") for n in dbg}

    xs = P.dram("xs_scratch", [D, S], F32, "Internal")
    cst = P.sb([128, NCST], F32, "cst")
    vec = [P.sb([128, NV], F32, f"vec{l}") for l in range(2)]
    modT = [P.sb([128, 48], F32, f"modT{l}") for l in range(2)]
    drv = [P.sb([128, 40], F32, f"drv{l}") for l in range(2)]
    drvB = [P.sb([64, 24], F32, f"drvB{l}") for l in range(2)]
    identb = P.sb([128, 128], BF16, "identb")
    onesb = P.sb([64, 64], BF16, "onesb")
    MUT = P.sb([64, 512], BF16, "MUT"); MLT = P.sb([64, 512], BF16, "MLT"); MUI = P.sb([64, 512], BF16, "MUI")
    IDH = P.sb([64, 512], BF16, "IDH"); RST = P.sb([64, 8 * TB], BF16, "RST")
    banks = [P.ps([128, 512], F32, f"bank{i}") for i in range(8)]
    bankT = Buf(banks[7].t.bitcast(BF16), "bankT", psum=True)
    rot = [0]

    def bk():
        b = banks[rot[0] % 7]
        rot[0] += 1
        return b

    ident = lambda n=128: cst[0:n, 0:n]
    ones = lambda n=128, m=128: cst[0:n, 128:128 + m]

    def vcol(l, name, j=0, n=1, parts=128):
        o, w = VOFF[name]
        return vec[l][0:parts, o + j:o + j + n]

    def ld_w(dst, src_ap, q="pool"):
        return P.dma(q, dst, V(src_ap_buf, src_ap, None))

    src_ap_buf = Buf(None, "wsrc")

    def dump(name, src_fn):
        if name in dbg_d:
            for kc in range(KC):
                P.dma("sp", dbg_d[name].view(dbg_d[name].t[kc * 128:(kc + 1) * 128, :]), src_fn(kc))

    P.dma("sp", cst[:], cst_d[:])
    P.dma("sp", xs[:], xT_d[:])
    for l in range(2):
        P.dma("sp", vec[l][:], vec_d.view(vec_d.t[l]))
    P.op("dve", "tensor_copy", identb[:], cst[:, 0:128])
    P.op("dve", "tensor_copy", onesb[:], cst[0:64, 128:192])
    for h in range(8):
        for (dst, nm) in ((MUT, "mut"), (MLT, "mlt"), (MUI, "mui"), (IDH, "ident")):
            o = COFF[nm][0]
            P.op("dve", "tensor_copy", dst[0:64, h * 64:(h + 1) * 64], cst[0:64, o:o + 64])
    for j in range(8 * TB // 64):
        P.op("dve", "tensor_copy", RST[0:64, j * 64:(j + 1) * 64], cst[0:64, 448:512])

    with ExitStack() as st:
        cT = P.sb([128, 8], F32, "cT", st); cond2 = P.sb([128, 8, 2], F32, "cond2", st)
        aw = [P.sb([128, KC, 512], F32, f"aw{i}", st) for i in range(2)]
        P.dma("sp", cT[:], cT_d[:])
        P.op("act", "activation", cond2[:, :, 0], cT[:], AF.Silu)
        P.op("act", "activation", cond2[:, :, 1], cT[:], AF.Silu)
        for l in range(nlayers):
            pm = bk()
            for g in range(12):
                a = aw[g % 2]
                P.dma("sp", a[:], V(src_ap_buf, ada_w_d.t[l][:, g * 512:(g + 1) * 512].rearrange("(k p) n -> p k n", p=128), None))
                for j in range(4):
                    col = (g * 4 + j) * 2
                    for kc in range(KC):
                        P.mm(pm[:, col:col + 2], a[:, kc, j * 128:(j + 1) * 128], cond2[:, kc, :], kc == 0, kc == KC - 1)
            P.op("dve", "tensor_tensor", modT[l][:], V(pm, pm.t[:, 0:96].rearrange("p (j two) -> p j two", two=2)[:, :, 0], None),
                 vcol(l, "ada_b", 0, 48), ALU.add)
            P.op("dve", "tensor_scalar", drv[l][:, 0:8], modT[l][:, 8:16], 1.0, None, ALU.add)
            P.op("dve", "tensor_tensor", drv[l][:, 0:8], drv[l][:, 0:8], vcol(l, "norm1", 0, 8), ALU.mult)
            P.op("dve", "tensor_scalar", drv[l][:, 8:16], modT[l][:, 32:40], 1.0, None, ALU.add)
            P.op("dve", "tensor_tensor", drv[l][:, 8:16], drv[l][:, 8:16], vcol(l, "norm2", 0, 8), ALU.mult)
            P.op("act", "activation", drv[l][:, 16:24], vcol(l, "lam", 0, 8), AF.Exp, scale=-1.0)
            P.op("act", "activation", drv[l][:, 16:24], drv[l][:, 16:24], AF.Ln, bias=1.0)
            P.op("dve", "tensor_scalar", drv[l][:, 24:32], drv[l][:, 16:24], -16.0, None, ALU.mult)
            P.op("dve", "tensor_scalar", drv[l][:, 16:24], drv[l][:, 16:24], -8.0, None, ALU.mult)
            P.op("dve", "tensor_scalar", drv[l][:, 32:35], vcol(l, "mu_w", 0, 3), -1.0, 1.0, ALU.mult, ALU.add)
            P.op("dve", "tensor_scalar", drvB[l][0:64, 0:24], vcol(l, "mu_r", 0, 24, 64), -1.0, 1.0, ALU.mult, ALU.add)
        P.barrier()

    def rmsnorm_mod(xsrc, gm, sh, dst_fn, st):
        sq = [P.sb([128, T], F32, f"nsq{i}", st) for i in range(2)]
        rs = P.sb([128, T], F32, "nrs", st)
        ss = bk()
        for kc in range(KC):
            q = sq[kc % 2]
            P.op("act", "activation", q[:], xsrc(kc), AF.Square)
            P.mm(ss[:], ones(), q[:], kc == 0, kc == KC - 1)
        P.op("act", "activation", rs[:], ss[:], AF.Ln, scale=1.0 / D, bias=EPS)
        P.op("act", "activation", rs[:], rs[:], AF.Exp, scale=-0.5)
        for kc in range(KC):
            q = sq[kc % 2]
            P.op("dve", "tensor_tensor", q[:], xsrc(kc), rs[:], ALU.mult)
            if gm is None:
                P.op("act", "activation", dst_fn(kc), q[:], AF.Identity, scale=sh(kc))
            else:
                P.op("act", "activation", dst_fn(kc), q[:], AF.Identity, scale=gm(kc), bias=sh(kc))

    GB = 4864

    def dump_merged(l, cs, merged):
        n = f"mg{l}"
        if n in dbg_d:
            for kc in range(KC):
                P.dma("sp", dbg_d[n].view(dbg_d[n].t[kc * 128:(kc + 1) * 128, cs]), merged[:, kc, :])
            P.barrier()


    def w_in_ap(l, c0, n):
        return w_in_d.t[l][:, c0:c0 + n].rearrange("(k p) n -> p k n", p=128)

    def proj_and_gate(l, st, ring, mname, nk, np_, rhs_fn, split, hT, merged, first):
        gt = [P.sb([128, T], F32, f"pg_gt{i}", st) for i in range(2)]
        tmp = [P.sb([128, T], F32, f"pg_tmp{i}", st) for i in range(2)]
        pj = pp = None
        for half in range(2):
            if split or half == 0:
                if pj is not None:
                    ring.release(pj)
                pj, pp = ring.get(f"{mname}_proj{half}" if split else f"{mname}_proj")
            gj, gp = ring.get(f"{mname}_g{half}")
            for ocl in range(4):
                oc = half * 4 + ocl
                pso = bk(); psg = bk()
                for k in range(nk):
                    if split:
                        lh = pp[0:np_, k, ocl * 128:(ocl + 1) * 128]
                    else:
                        lh = V(pp, pp.t[0:np_].rearrange("p k n -> p (k n)")[:, k * 1024 + oc * 128:k * 1024 + (oc + 1) * 128], None)
                    P.mm(pso[:], lh, rhs_fn(k), k == 0, k == nk - 1)
                for kc in range(KC):
                    P.mm(psg[:], gp[:, kc, ocl * 128:(ocl + 1) * 128], hT[:, kc, :], kc == 0, kc == KC - 1)
                g = gt[oc % 2]
                P.op("act", "activation", g[:], psg[:], AF.Sigmoid)
                if first:
                    P.op("dve", "tensor_tensor", merged.k(oc)[:, oc, :], pso[:], g[:], ALU.mult)
                else:
                    t_ = tmp[oc % 2]
                    P.op("dve", "tensor_tensor", t_[:], pso[:], g[:], ALU.mult)
                    P.op("pool", "tensor_tensor", merged.k(oc)[:, oc, :], merged.k(oc)[:, oc, :], t_[:], ALU.add)
            ring.release(gj)
        ring.release(pj)

    wcache = P.dram("wcache", [23, 128, 4096], BF16, "Internal")

    def plan_chunk(ring, l, c):
        full = lambda slot: (slot, slot.t[:].rearrange("p k n -> p (k n)"), 4096, 128)
        pieces = []

        def add(name, sv, src):
            pieces.append((name, sv, src))

        wl = lambda c0, n: ((lambda slot: (slot.t[:, :, 0:n], 128, 8, n)), w_in_ap(l, c0, n))
        kp = lambda d, h: ((lambda slot: (slot.t[:, :, :], 128, 8, 512)), d.t[l][:, h * 512:(h + 1) * 512].rearrange("(k p) n -> p k n", p=128))
        pbp = lambda h: ((lambda slot: (slot.t[0:64, :, :], 64, 8, 512)), proj_b_d.t[l][:, h * 512:(h + 1) * 512].rearrange("(hh i) n -> i hh n", i=64))
        add("A_val", *wl(0, 512)); add("A_sig", *wl(512, 512))
        add("A_proj", (lambda slot: (slot.t[:].rearrange("p k n -> p (k n)").rearrange("p (k n) -> p k n", k=4), 128, 4, 1024)),
            proj_a_d.t[l].rearrange("(k p) n -> p k n", p=128))
        add("A_g0", *wl(GB, 512)); add("A_g1", *wl(GB + 512, 512))
        add("C_y0", *wl(2816, 512)); add("C_x0", *wl(3840, 512)); add("C_y1", *wl(2816 + 512, 512)); add("C_x1", *wl(3840 + 512, 512))
        add("C_proj0", *kp(proj_c_d, 0)); add("C_g0", *wl(GB + 2048, 512))
        add("C_proj1", *kp(proj_c_d, 1)); add("C_g1", *wl(GB + 2048 + 512, 512))
        add("B_r", *wl(1024, 512)); add("B_k", *wl(1536, 512)); add("B_v", *wl(2048, 512)); add("B_lora", *wl(2560, 256))
        add("B_proj0", *pbp(0)); add("B_g0", *wl(GB + 1024, 512))
        add("B_proj1", *pbp(1)); add("B_g1", *wl(GB + 1024 + 512, 512))
        add("O_0", *kp(w_out_d, 0)); add("O_1", *kp(w_out_d, 1))
        for idx, (name, sv, src) in enumerate(pieces):
            def loader(slot, idx=idx, sv=sv, src=src):
                ap, np_, a, b = sv(slot)
                cv = wcache.t[idx, 0:np_, 0:a * b].rearrange("p (k n) -> p k n", k=a)
                if c == 0:
                    P.dma("pool", V(slot, ap, None), V(src_ap_buf, src, None), ring=True)
                    P.dma("sp", V(wcache, cv, idx), V(slot, ap, None), ring=True)
                else:
                    P.dma("sp", V(slot, ap, None), V(wcache, cv, idx), ring=True)
            ring.add(name, loader)

    for l in range(nlayers):
        with ExitStack() as lst:
            uext = P.sb([128, 4, 30 + T], BF16, "uext", lst)
            xh = P.sb([128, 8, 3], BF16, "xh", lst)
            hC = P.sb([128, 8], F32, "hC", lst)
            prevq = P.sb([64, 3, 8], F32, "prevq", lst)
            pextw = P.sb([64, 1 + TB], F32, "pextw", lst); pexta = P.sb([64, 1 + TB], F32, "pexta", lst)
            pextg = P.sb([128, 1 + TB], F32, "pextg", lst)
            Sf = P.sb([64, 512], F32, "Sf", lst); Sb = P.sb([64, 512], BF16, "Sb", lst)
            lw = P.sb([128, 2, 8, 128], BF16, "c_lw", lst)
            P.dma("pool", lw[:, 0], V(src_ap_buf, lwa_d.t[l].rearrange("h i j -> i h j"), None))
            P.dma("pool", lw[:, 1], V(src_ap_buf, lwx_d.t[l].rearrange("h i j -> i h j"), None))
            lor = P.sb([128, 1024], BF16, "b_lor", lst)
            gup = P.sb([128, 512], BF16, "b_gup", lst)
            P.dma("pool", lor[0:64, 0:512], V(src_ap_buf, w_up_d.t[l], None))
            P.dma("pool", lor[0:64, 512:1024], V(src_ap_buf, a_up_d.t[l], None))
            P.dma("pool", gup[:], V(src_ap_buf, g_up_d.t[l], None))
            P.op("pool", "memset", uext[:, :, 0:30], 0.0)
            P.op("pool", "memset", xh[:], 0.0)
            P.op("pool", "memset", hC[:], 0.0)
            P.op("pool", "memset", prevq[:], 0.0)
            P.op("pool", "memset", pextw[:, 0:1], 0.0); P.op("pool", "memset", pexta[:, 0:1], 0.0)
            P.op("pool", "memset", pextg[:, 0:1], 0.0)
            P.op("pool", "memset", Sf[:], 0.0); P.op("pool", "memset", Sb[:], 0.0)
            mc = lambda j, n=1, l=l: modT[l][:, j:j + n]

            with ExitStack() as mst:
                ring = WRing(P, 5, mst)
                for _c in range(NCH):
                    plan_chunk(ring, l, _c)
                ring.pump()
                hT = P.sb([128, KC, T], BF16, "hT", mst)
                merged = P.sb([128, KC, T], F32, "merged", mst)
                for c in range(NCH):
                    cs = slice(c * T, (c + 1) * T)
                    with ExitStack() as st:
                        xch = P.sb([128, KC, T], F32, "xch", st)
                        P.dma("sp", xch[:], xs.view(xs.t[:, cs].rearrange("(k p) t -> p k t", p=128)))
                        rmsnorm_mod(lambda kc: xch[:, kc, :], lambda kc: drv[l][:, kc:kc + 1], lambda kc: mc(kc), lambda kc: hT[:, kc, :], st)
                        P.barrier()
                    if f"h{l}" in dbg_d:
                        with ExitStack() as st:
                            tf = P.sb([128, KC, T], F32, "dbgtf", st)
                            P.op("dve", "tensor_copy", tf[:], hT[:])
                            for kc in range(KC):
                                P.dma("sp", dbg_d[f"h{l}"].view(dbg_d[f"h{l}"].t[kc * 128:(kc + 1) * 128, cs]), tf[:, kc, :])
                            P.barrier()
                    with ExitStack() as st:
                        jv, wv = ring.get("A_val"); jg, wg = ring.get("A_sig")
                        sg = [P.sb([128, T], F32, f"a_sg{i}", st) for i in range(2)]
                        cv = P.sb([128, 4, T], F32, "a_cv", st)
                        dg = P.sb([128, 31, 128], BF16, "a_dg", st)
                        sA = P.sb([128, 4, T], BF16, "a_sA", st)
                        dgs = [dg, P.sb([128, 31, 128], BF16, "a_dg2", st)]

                        def a_in(pc):
                            psv = bk(); psg = bk()
                            for kc in range(KC):
                                P.mm(psv[:], wv[:, kc, pc * 128:(pc + 1) * 128], hT[:, kc, :], kc == 0, kc == KC - 1)
                            for kc in range(KC):
                                P.mm(psg[:], wg[:, kc, pc * 128:(pc + 1) * 128], hT[:, kc, :], kc == 0, kc == KC - 1)
                            yield
                            g = sg[pc % 2]
                            P.op("act", "activation", g[:], psg[:], AF.Sigmoid)
                            yield
                            P.op("dve", "tensor_tensor", uext.k(pc)[:, pc, 30:30 + T], psv[:], g[:], ALU.mult)

                        def a_conv(pc):
                            d_ = dgs[pc % 2]
                            for j in range(31):
                                P.op("dve" if j % 2 else "pool", "tensor_scalar", d_.k(j)[:, j, :], identb[:], vcol(l, "caw", pc * 31 + j), None, ALU.mult)
                            yield
                            psc = bk()
                            for j in range(31):
                                P.mm(psc[:], d_.k(j)[:, j, :], uext.k(pc)[:, pc, j:j + T], j == 0, j == 30)
                            yield
                            P.op("act", "activation", cv.k(pc)[:, pc, :], psc[:], AF.Identity, bias=vcol(l, "cab", pc))

                        run_rr([a_in(pc) for pc in range(4)], 2)
                        ring.release(jv); ring.release(jg)
                        run_rr([a_conv(pc) for pc in range(4)], 2)
                        P.op("dve", "tensor_copy", uext[:, :, 0:30], uext[:, :, T:T + 30])
                        pss = bk(); pss2 = bk()
                        for pc in range(4):
                            q = sg[pc % 2]
                            P.mm(pss[:], ones(), cv.k(pc)[:, pc, :], pc == 0, pc == 3)
                            P.op("act", "activation", q[:], cv.k(pc)[:, pc, :], AF.Square)
                            P.mm(pss2[:], ones(), q[:], pc == 0, pc == 3)
                        mean = P.sb([128, T], F32, "a_mean", st); rstd = P.sb([128, T], F32, "a_rstd", st)
                        P.op("act", "activation", mean[:], pss[:], AF.Identity, scale=1.0 / 512)
                        P.op("dve", "tensor_tensor", rstd[:], mean[:], mean[:], ALU.mult)
                        P.op("dve", "scalar_tensor_tensor", rstd[:], pss2[:], 1.0 / 512, rstd[:], ALU.mult, ALU.subtract)
                        P.op("act", "activation", rstd[:], rstd[:], AF.Ln, bias=1e-5)
                        P.op("act", "activation", rstd[:], rstd[:], AF.Exp, scale=-0.5)
                        for pc in range(4):
                            q = sg[pc % 2]
                            P.op("dve", "tensor_tensor", q[:], cv.k(pc)[:, pc, :], mean[:], ALU.subtract)
                            P.op("dve", "tensor_tensor", q[:], q[:], rstd[:], ALU.mult)
                            P.op("act", "activation", sA.k(pc)[:, pc, :], q[:], AF.Silu, scale=vcol(l, "lng", pc), bias=vcol(l, "lnb", pc))
                        proj_and_gate(l, st, ring, "A", 4, 128, lambda k: sA.k(k)[:, k, :], False, hT, merged, True)
                        P.barrier()
                    if stop == "A":
                        dump_merged(l, cs, merged)
                        continue
                    with ExitStack() as st:
                        hy = P.sb([128, 8, T], BF16, "c_hy", st)
                        CW = 3
                        xe = [P.sb([128, 3 + T], BF16, f"c_xe{i}", st) for i in range(CW)]
                        dgcs = [P.sb([128, 4, 128], BF16, f"c_dg{i}", st) for i in range(CW)]
                        W = {n: [P.sb([128, T], F32, f"c_{n}{i}", st) for i in range(CW)] for n in ("yg", "t1", "t2", "xc", "ga", "gx", "ys")}
                        xcb = [P.sb([128, T], BF16, f"c_xcb{i}", st) for i in range(CW)]
                        cpieces = {}

                        def c_pc(pc):
                            i2 = pc % CW
                            dgc = dgcs[i2]
                            if pc % 4 == 0:
                                cpieces[pc // 4] = (ring.get(f"C_y{pc // 4}"), ring.get(f"C_x{pc // 4}"))
                            (jy, wy), (jx, wx) = cpieces[pc // 4]
                            pcl = pc % 4
                            psy = bk(); psx = bk()
                            for kc in range(KC):
                                P.mm(psy[:], wy[:, kc, pcl * 128:(pcl + 1) * 128], hT[:, kc, :], kc == 0, kc == KC - 1)
                            for kc in range(KC):
                                P.mm(psx[:], wx[:, kc, pcl * 128:(pcl + 1) * 128], hT[:, kc, :], kc == 0, kc == KC - 1)
                            if pcl == 3:
                                ring.release(jy); ring.release(jx)
                            for j in range(4):
                                P.op("pool", "tensor_scalar", dgc.k(j)[:, j, :], identb[:], vcol(l, "ccw", pc * 4 + j), None, ALU.mult)
                            P.op("pool", "tensor_copy", xe[i2][:, 0:3], xh.k(pc)[:, pc, :])
                            yield
                            yg, t1, t2 = W["yg"][i2], W["t1"][i2], W["t2"][i2]
                            ys = W["ys"][i2]
                            P.op("act", "activation", t1[:], psy[:], AF.Square)
                            P.op("dve", "tensor_copy", ys[:], psy[:])
                            P.op("act", "activation", xe[i2][:, 3:3 + T], psx[:], AF.Copy)
                            yield
                            P.op("dve", "tensor_scalar", t1[:], t1[:], 0.044715, 1.0, ALU.mult, ALU.add)
                            psc = bk()
                            for j in range(4):
                                P.mm(psc[:], dgc.k(j)[:, j, :], xe[i2][:, j:j + T], j == 0, j == 3)
                            P.op("pool", "tensor_copy", xh.k(pc)[:, pc, :], xe[i2][:, T:T + 3])
                            yield
                            P.op("dve", "tensor_tensor", t1[:], t1[:], ys[:], ALU.mult)
                            xc = W["xc"][i2]
                            P.op("act", "activation", xc[:], psc[:], AF.Identity, bias=vcol(l, "ccb", pc))
                            yield
                            P.op("act", "activation", t1[:], t1[:], AF.Sigmoid, scale=1.5957691216057308)
                            P.op("dve", "tensor_copy", xcb[i2][:], xc[:])
                            yield
                            P.op("dve", "tensor_tensor", yg[:], t1[:], ys[:], ALU.mult)
                            psa = bk(); psb = bk()
                            P.mm(psa[:], lw[:, 0, pc, :], xcb[i2][:])
                            P.mm(psb[:], lw[:, 1, pc, :], xcb[i2][:])
                            yield
                            ga, gx = W["ga"][i2], W["gx"][i2]
                            P.op("act", "activation", ga[:], psa[:], AF.Sigmoid, bias=vcol(l, "lba", pc))
                            P.op("act", "activation", gx[:], psb[:], AF.Sigmoid, bias=vcol(l, "lbx", pc))
                            yield
                            P.op("act", "activation", t2[:], ga[:], AF.Exp, scale=drv[l][:, 24 + pc:25 + pc])
                            P.op("dve", "tensor_tensor", gx[:], gx[:], xc[:], ALU.mult)
                            yield
                            P.op("act", "activation", t2[:], t2[:], AF.Sqrt, scale=-1.0, bias=1.0)
                            P.op("act", "activation", ga[:], ga[:], AF.Exp, scale=drv[l][:, 16 + pc:17 + pc])
                            yield
                            P.op("dve", "tensor_tensor", gx[:], gx[:], t2[:], ALU.mult)
                            yield
                            P.op("dve", "tensor_tensor_scan", t2[:], ga[:], gx[:], hC.k(pc)[:, pc:pc + 1], ALU.mult, ALU.add)
                            yield
                            P.op("act", "activation", hC.k(pc)[:, pc:pc + 1], t2[:, T - 1:T], AF.Copy)
                            P.op("dve", "tensor_tensor", hy.k(pc)[:, pc, :], t2[:], yg[:], ALU.mult)

                        run_rr([c_pc(pc) for pc in range(8)], CW)
                        proj_and_gate(l, st, ring, "C", 8, 128, lambda k: hy.k(k)[:, k, :], True, hT, merged, False)
                        P.barrier()
                    if stop == "C":
                        dump_merged(l, cs, merged)
                        continue
                    with ExitStack() as st:
                        E = dict(bk=bk, vec=vec, drvB=drvB, cst=cst, identb=identb, onesb=onesb, ring=ring, lor=lor, gup=gup, hT=hT, merged=merged, prevq=prevq,
                                 pextw=pextw, pexta=pexta, pextg=pextg, Sf=Sf, Sb=Sb, MUT=MUT, MLT=MLT, MUI=MUI, IDH=IDH, RST=RST,
                                 bankT=bankT, src_ap_buf=src_ap_buf, w_in_ap=w_in_ap, vcol=vcol, drv=drv, w_up_d=w_up_d, a_up_d=a_up_d,
                                 g_up_d=g_up_d, proj_b_d=proj_b_d, proj_and_gate=proj_and_gate)
                        rwkv_chunk(P, l, c, st, E)
                        P.barrier()
                    dump_merged(l, cs, merged)
                    with ExitStack() as st:
                        xch = P.sb([128, KC, T], F32, "xch2", st)
                        P.dma("sp", xch[:], xs.view(xs.t[:, cs].rearrange("(k p) t -> p k t", p=128)))
                        mergedb = P.sb([128, KC, T], BF16, "mergedb", st)
                        for kc in range(KC):
                            P.op("act", "activation", mergedb.k(kc)[:, kc, :], merged.k(kc)[:, kc, :], AF.Copy)
                        for oc in range(8):
                            if oc % 4 == 0:
                                if oc:
                                    ring.release(jo)
                                jo, wo = ring.get(f"O_{oc // 4}")
                            ocl = oc % 4
                            pso = bk()
                            for kc in range(KC):
                                P.mm(pso[:], wo[:, kc, ocl * 128:(ocl + 1) * 128], mergedb.k(kc)[:, kc, :], kc == 0, kc == KC - 1)
                            P.op("dve", "scalar_tensor_tensor", xch.k(oc)[:, oc, :], pso[:], mc(16 + oc), xch.k(oc)[:, oc, :], ALU.mult, ALU.add)
                        ring.release(jo)
                        P.dma("sp", xs.view(xs.t[:, cs].rearrange("(k p) t -> p k t", p=128)), xch[:])
                        P.barrier()
                P.barrier()
            dump(f"x1_{l}", lambda kc: xs.view(xs.t[kc * 128:(kc + 1) * 128, :]))
            if stop in ("A", "C", "mix"):
                P.barrier()
                continue
            with ExitStack() as mst:
                h2 = P.sb([128, KC, S], BF16, "h2", mst)
                xT = P.sb([128, KC, S], F32, "xT", mst)
                cwT = P.sb([64, 2, S], BF16, "cwT", mst)
                rwb = P.sb([128, KC, 64], BF16, "rwb", mst)
                rbias = P.sb([128, 64], F32, "rbias", mst)
                P.dma("pool", rwb[:], V(src_ap_buf, rw_d.t[l].rearrange("(k p) n -> p k n", p=128), None))
                P.dma("sp", rbias[:], rb_d.view(rb_d.t[l]))
                for kc in range(KC):
                    P.dma("sp", xT.k(kc)[:, kc, :], xs.view(xs.t[kc * 128:(kc + 1) * 128, :]))
                for c in range(NCH):
                    cs = slice(c * T, (c + 1) * T)
                    with ExitStack() as st:
                        rmsnorm_mod(lambda kc: xT.k(kc)[:, kc, cs], lambda kc: drv[l][:, 8 + kc:9 + kc], lambda kc: mc(24 + kc), lambda kc: h2.k((kc, c))[:, kc, cs], st)
                        R = {n: P.sb([128, 64], F32, f"r_{n}", st) for n in ("sc", "bs", "b2", "mk", "sel")}
                        r8 = {n: P.sb([128, 8], F32, f"r8_{n}", st) for n in ("m1", "m2", "o8", "gm", "o8b", "ss")}
                        for tt in range(4):
                            ts_ = slice(c * T + tt * 128, c * T + (tt + 1) * 128)
                            psl = bk()
                            for kc in range(KC):
                                P.mm(psl[:, 0:64], h2.k((kc, c))[:, kc, ts_], rwb[:, kc, :], kc == 0, kc == KC - 1)
                            v3 = lambda b: V(b, b.t[:, :].rearrange("p (g e) -> p g e", e=8), None)
                            bc8 = lambda b: V(b, b.t[:, 0:8].unsqueeze(2).to_broadcast([128, 8, 8]), None)
                            P.op("act", "activation", R["sc"][:], psl[:, 0:64], AF.Sigmoid)
                            P.op("dve", "tensor_tensor", R["bs"][:], R["sc"][:], rbias[:], ALU.add)
                            P.op("dve", "tensor_reduce", r8["m1"][:], v3(R["bs"]), mybir.AxisListType.X, ALU.max)
                            P.op("dve", "tensor_tensor", v3(R["b2"]), v3(R["bs"]), bc8(r8["m1"]), ALU.is_equal)
                            P.op("dve", "scalar_tensor_tensor", R["b2"][:], R["b2"][:], -1e9, R["bs"][:], ALU.mult, ALU.add)
                            P.op("dve", "tensor_reduce", r8["m2"][:], v3(R["b2"]), mybir.AxisListType.X, ALU.max)
                            P.op("dve", "tensor_tensor", r8["m1"][:], r8["m1"][:], r8["m2"][:], ALU.add)
                            P.op("dve", "max", r8["o8"][:], r8["m1"][:])
                            P.op("dve", "tensor_scalar", r8["gm"][:], r8["m1"][:], r8["o8"][:, 3:4], None, ALU.is_ge)
                            P.op("dve", "tensor_scalar", r8["gm"][:], r8["gm"][:], -1.0, 1e9, ALU.add, ALU.mult)
                            P.op("dve", "tensor_tensor", v3(R["mk"]), v3(R["bs"]), bc8(r8["gm"]), ALU.add)
                            P.op("dve", "max", r8["o8b"][:], R["mk"][:])
                            P.op("dve", "tensor_scalar", R["sel"][:], R["mk"][:], r8["o8b"][:, 7:8], None, ALU.is_ge)
                            P.op("dve", "tensor_tensor", R["sel"][:], R["sel"][:], R["sc"][:], ALU.mult)
                            P.op("dve", "tensor_reduce", r8["ss"][:, 0:1], R["sel"][:], mybir.AxisListType.X, ALU.add)
                            P.op("dve", "reciprocal", r8["ss"][:, 0:1], r8["ss"][:, 0:1])
                            P.op("dve", "tensor_scalar", R["sel"][:], R["sel"][:], r8["ss"][:, 0:1], 2.5, ALU.mult, ALU.mult)
                            pst = bk()
                            P.transpose(pst[0:64, 0:128], R["sel"][:], ident())
                            P.op("act", "activation", cwT.k(c * 4 + tt)[0:64, 0, ts_], pst[0:64, 0:128], AF.Copy)
                            P.op("dve", "tensor_tensor", cwT.k(c * 4 + tt)[0:64, 1, ts_], pst[0:64, 0:128], cwT.k(c * 4 + tt)[0:64, 0, ts_], ALU.subtract)
                        P.barrier()
                if f"h2_{l}" in dbg_d:
                    with ExitStack() as st:
                        tf = P.sb([128, S], F32, "dbgtf2", st)
                        for kc in range(KC):
                            P.op("dve", "tensor_copy", tf[:], h2[:, kc, :])
                            P.dma("sp", dbg_d[f"h2_{l}"].view(dbg_d[f"h2_{l}"].t[kc * 128:(kc + 1) * 128, :]), tf[:])
                        P.barrier()
                if f"cw{l}" in dbg_d:
                    pass
                with ExitStack() as st:
                    EW = [P.sb([128, 6144], BF16, f"ew{i}", st) for i in range(2)]
                    SEL = [P.sb([64, 128], BF16, f"sel{i}", st) for i in range(2)]
                    cwb = [P.sb([128, T], F32, f"cwb{i}", st) for i in range(2)]
                    sa = [P.sb([128, T], F32, f"m_sa{i}", st) for i in range(2)]
                    gT = [P.sb([128, 2, T], BF16, f"m_gT{i}", st) for i in range(2)]
                    NE = 65 if stop != "noexp" else 0
                    mb = banks
                    porot = [0]

                    def ew_views(e):
                        ew = EW[e % 2]
                        w1t = lambda k, f: V(ew, ew.t[:, k * 256 + f * 128:k * 256 + (f + 1) * 128], None)
                        w3t = lambda k, f: V(ew, ew.t[:, 2048 + k * 256 + f * 128:2048 + k * 256 + (f + 1) * 128], None)
                        w2t = lambda f, oc: V(ew, ew.t[:, 4096 + f * 1024 + oc * 128:4096 + f * 1024 + (oc + 1) * 128], None)
                        return ew, w1t, w3t, w2t

                    def load_expert(e):
                        ew = EW[e % 2]
                        w1 = V(ew, ew.t[:, 0:2048].rearrange("p (k n) -> p k n", k=8), None)
                        w3 = V(ew, ew.t[:, 2048:4096].rearrange("p (k n) -> p k n", k=8), None)
                        w2 = V(ew, ew.t[:, 4096:6144].rearrange("p (k n) -> p k n", k=2), None)
                        if e < 64:
                            s1, s3, s2 = e1_d.t[l][e], e3_d.t[l][e], e2_d.t[l][e]
                        else:
                            s1, s3, s2 = s1_d.t[l], s3_d.t[l], s2_d.t[l]
                        P.dma("pool", w1, V(src_ap_buf, s1.rearrange("(k p) n -> p k n", p=128), None))
                        P.dma("pool", w3, V(src_ap_buf, s3.rearrange("(k p) n -> p k n", p=128), None))
                        P.dma("pool", w2, V(src_ap_buf, s2.rearrange("(k p) n -> p k n", p=128), None))

                    def stage1(i):
                        e, c = divmod(i, NCH)
                        cs = slice(c * T, (c + 1) * T)
                        ew, w1t, w3t, w2t = ew_views(e)
                        i2 = i % 2
                        if c == 0:
                            if e < 64:
                                P.op("dve", "tensor_scalar", SEL[e % 2][:], ones(64, 128), cst[0:64, e:e + 1], None, ALU.mult)
                        if e < 64:
                            P.mm(mb[0][:], SEL[e % 2][:], cwT[0:64, 0, cs], True, False)
                            P.mm(mb[0][:], SEL[e % 2][:], cwT[0:64, 1, cs], False, True)
                            P.op("act", "activation", cwb[i2][:], mb[0][:], AF.Copy)
                        for f in range(2):
                            pa = mb[1 + 2 * f]; pb = mb[2 + 2 * f]
                            for kc in range(KC):
                                P.mm(pa[:], w1t(kc, f), h2[:, kc, cs], kc == 0, kc == KC - 1)
                            for kc in range(KC):
                                P.mm(pb[:], w3t(kc, f), h2[:, kc, cs], kc == 0, kc == KC - 1)
                            s_ = sa[f]
                            P.op("act", "activation", s_[:], pa[:], AF.Silu)
                            if e < 64:
                                P.op("dve", "tensor_tensor", s_[:], s_[:], pb[:], ALU.mult)
                                P.op("pool", "tensor_tensor", gT[i2].k(f)[:, f, :], s_[:], cwb[i2][:], ALU.mult)
                            else:
                                P.op("dve", "tensor_tensor", gT[i2].k(f)[:, f, :], s_[:], pb[:], ALU.mult)

                    def stage2(i):
                        e, c = divmod(i, NCH)
                        cs = slice(c * T, (c + 1) * T)
                        ew, w1t, w3t, w2t = ew_views(e)
                        i2 = i % 2
                        for oc in range(8):
                            po = mb[5 + porot[0] % 3]
                            porot[0] += 1
                            for f in range(2):
                                P.mm(po[:], w2t(f, oc), gT[i2].k(f)[:, f, :], f == 0, f == 1)
                            P.op("dve", "scalar_tensor_tensor", xT.k(oc)[:, oc, cs], po[:], mc(40 + oc), xT.k(oc)[:, oc, cs], ALU.mult, ALU.add)

                    NI = NE * NCH
                    if NI:
                        load_expert(0)
                        load_expert(1)
                        stage1(0)
                    for i in range(NI):
                        if i + 1 < NI:
                            stage1(i + 1)
                        stage2(i)
                        if i % NCH == NCH - 1 and i // NCH + 2 < NE:
                            load_expert(i // NCH + 2)
                    P.barrier()
                for kc in range(KC):
                    P.dma("sp", xs.view(xs.t[kc * 128:(kc + 1) * 128, :]), xT.k(kc)[:, kc, :])
                P.barrier()
            dump(f"x2_{l}", lambda kc: xs.view(xs.t[kc * 128:(kc + 1) * 128, :]))
            P.barrier()

    with ExitStack() as st:
        fn = P.sb([128, 8], F32, "fn", st)
        ob = [P.sb([128, T], F32, f"ob{i}", st) for i in range(4)]
        P.dma("sp", fn[:], fn_d[:])
        cnt = [0]
        for c in range(NCH):
            cs = slice(c * T, (c + 1) * T)
            with ExitStack() as st2:
                def dst(kc):
                    return ob[kc % 4][:]
                xf = P.sb([128, KC, T], F32, "xfin", st2)
                P.dma("sp", xf[:], xs.view(xs.t[:, cs].rearrange("(k p) t -> p k t", p=128)))
                sq = [P.sb([128, T], F32, f"fsq{i}", st2) for i in range(2)]
                rs = P.sb([128, T], F32, "frs", st2)
                ss = bk()
                for kc in range(KC):
                    q = sq[kc % 2]
                    P.op("act", "activation", q[:], xf[:, kc, :], AF.Square)
                    P.mm(ss[:], ones(), q[:], kc == 0, kc == KC - 1)
                P.op("act", "activation", rs[:], ss[:], AF.Ln, scale=1.0 / D, bias=EPS)
                P.op("act", "activation", rs[:], rs[:], AF.Exp, scale=-0.5)
                for kc in range(KC):
                    q = sq[kc % 2]
                    o = ob[kc % 4]
                    P.op("dve", "tensor_tensor", q[:], xf[:, kc, :], rs[:], ALU.mult)
                    P.op("act", "activation", o[:], q[:], AF.Identity, scale=fn[:, kc:kc + 1])
                    P.dma("sp", out_d.view(out_d.t[kc * 128:(kc + 1) * 128, cs]), o[:])
                P.barrier()
    P.finish()
    return P


def _pack_vec(inp, l):
    v = np.zeros((128, NV), np.float32)

    def put(name, arr):
        o, w = VOFF[name]
        arr = np.asarray(arr, np.float32)
        assert arr.shape[1] == w, (name, arr.shape)
        v[:arr.shape[0], o:o + w] = arr

    c128 = lambda a, n: np.asarray(a).reshape(n, 128).T
    c64 = lambda a: np.asarray(a).reshape(8, 64).T
    put("ada_b", c128(inp["ada_b"][l], 48)); put("norm1", c128(inp["norm1"][l], 8)); put("norm2", c128(inp["norm2"][l], 8))
    put("caw", np.asarray(inp["conv_a_w"][l]).T.reshape(4, 128, 31).transpose(1, 0, 2).reshape(128, 124))
    put("cab", c128(inp["conv_a_b"][l], 4)); put("lng", c128(inp["ln_a_g"][l], 4)); put("lnb", c128(inp["ln_a_b"][l], 4))
    put("ccw", np.asarray(inp["conv_c_w"][l]).T.reshape(8, 128, 4).transpose(1, 0, 2).reshape(128, 32))
    put("ccb", c128(inp["conv_c_b"][l], 8)); put("lba", c128(inp["lru_ba"][l], 8)); put("lbx", c128(inp["lru_bx"][l], 8))
    put("lam", c128(inp["lru_lambda"][l], 8))
    mu = np.asarray(inp["mu_b"][l])
    put("mu_w", mu[1536:1600].reshape(64, 1)); put("mu_a", mu[1600:1664].reshape(64, 1)); put("mu_g", mu[1664:1792].reshape(128, 1))
    put("mu_r", c64(mu[0:512])); put("mu_k", c64(mu[512:1024])); put("mu_v", c64(mu[1024:1536]))
    for n, k in (("w0", "w0"), ("a0", "a0"), ("k_k", "k_k"), ("k_a", "k_a"), ("gn_g", "gn_b_g"), ("gn_b", "gn_b_b")):
        put(n, c64(inp[k][l]))
    put("r_k", np.asarray(inp["r_k"][l]).T)
    return v


def _consts():
    c = np.zeros((128, NCST), np.float32)
    c[:, 0:128] = np.eye(128, dtype=np.float32)
    c[:, 128:256] = 1.0
    s = np.arange(64)[:, None]; t = np.arange(64)[None, :]
    c[0:64, 256:320] = (s < t); c[0:64, 320:384] = (s > t); c[0:64, 384:448] = (s <= t)
    c[:, 448:512] = 1.0; c[:, 448] = 0.0
    return c


_PROG_CACHE = {}


def _get_prog(dbg=(), stop=None, nlayers=2):
    key = (tuple(dbg), stop, nlayers)
    if key not in _PROG_CACHE:
        _PROG_CACHE[key] = build(dbg, stop, nlayers)
    return _PROG_CACHE[key]


def _in_maps(inp, cores):
    f = lambda a: np.ascontiguousarray(np.asarray(a, np.float32))
    vec = np.stack([_pack_vec(inp, l) for l in range(2)])
    shared = {
        "vec": vec, "fnorm": f(np.asarray(inp["final_norm"]).reshape(8, 128).T),
        "rbias": f(np.broadcast_to(np.asarray(inp["router_bias"])[:, None, :], (2, 128, 64))), "cst": _consts(),
        "ada_w": f(inp["ada_w"]), "w_in": f(inp["w_in"]), "proj_a": f(inp["proj_a"]), "proj_b": f(inp["proj_b"]),
        "proj_c": f(inp["proj_c"]), "w_out": f(inp["w_out"]), "w_up": f(inp["w_up"]), "a_up": f(inp["a_up"]),
        "g_up": f(inp["g_up"]), "lru_wa": f(inp["lru_wa"]), "lru_wx": f(inp["lru_wx"]), "router_w": f(inp["router_w"]),
        "exp_w1": f(inp["exp_w1"]), "exp_w3": f(inp["exp_w3"]), "exp_w2": f(inp["exp_w2"]),
        "sh_w1": f(inp["sh_w1"]), "sh_w3": f(inp["sh_w3"]), "sh_w2": f(inp["sh_w2"]),
    }
    maps = []
    for b in cores:
        m = dict(shared)
        m["xT"] = f(np.asarray(inp["x"][b]).T)
        m["cT"] = f(np.asarray(inp["c"][b]).reshape(8, 128).T)
        maps.append(m)
    return maps


def kernel(**inputs):
    P = _get_prog()
    maps = _in_maps(inputs, range(8))
    res = run_bass_kernel_spmd(P.nc, maps, core_ids=list(range(8)))
    out = np.stack([np.asarray(r["outT"]).T for r in res.results]).astype(np.float32)
    return out
```

```python
import numpy as np
from contextlib import ExitStack
import concourse.bass as bass
import concourse.mybir as mybir
from concourse.bass_utils import run_bass_kernel_spmd

F32 = mybir.dt.float32
BF16 = mybir.dt.bfloat16
AF = mybir.ActivationFunctionType
ALU = mybir.AluOpType

EPOCH = 30000
NDS = 24


class Tok:
    __slots__ = ("eng", "sem", "sid", "val")

    def __init__(self, eng, sem, sid, val):
        self.eng, self.sem, self.sid, self.val = eng, sem, sid, val


class V:
    __slots__ = ("buf", "ap", "key")

    def __init__(self, buf, ap, key):
        self.buf, self.ap, self.key = buf, ap, key


class _Keyed:
    def __init__(self, buf, key):
        self.buf, self.key = buf, key

    def __getitem__(self, idx):
        return V(self.buf, self.buf.t[idx], self.key)


class Buf:
    def __init__(self, t, name, psum=False):
        self.t = t
        self.name = name
        self.psum = psum
        self.st = {}

    def __getitem__(self, idx):
        return V(self, self.t[idx], None)

    def k(self, key):
        return _Keyed(self, key)

    def view(self, ap, key=None):
        return V(self, ap, key)

    def _keys(self, key):
        if key is None:
            return list(self.st.keys())
        ks = []
        if key in self.st:
            ks.append(key)
        if None in self.st:
            ks.append(None)
        return ks

    def rdeps(self, key):
        if self.psum:
            return self.wdeps(key)
        return [self.st[k][0] for k in self._keys(key) if self.st[k][0] is not None]

    def wdeps(self, key):
        out = []
        for k in self._keys(key):
            w, rs = self.st[k]
            if w is not None:
                out.append(w)
            out.extend(rs)
        return out

    def add_read(self, key, tok):
        self.st.setdefault(key, [None, []])[1].append(tok)

    def add_write(self, key, tok):
        if key is None:
            self.st = {None: [tok, []]}
        else:
            self.st[key] = [tok, []]


class Prog:
    def __init__(self):
        self.nc = bass.Bass("TRN2", target_bir_lowering=False)
        nc = self.nc
        self.es = ExitStack()
        self.eng = {"pe": nc.tensor, "act": nc.scalar, "dve": nc.vector,
                    "pool": nc.gpsimd, "sp": nc.sync}
        self.esem = {e: [] for e in self.eng}
        self.ecnt = {e: 0 for e in self.eng}
        self.seen = {e: {} for e in self.eng}
        self.dsem = [self.es.enter_context(nc.semaphore(f"dq{i}")) for i in range(4 * NDS)]
        self.dcnt = [0] * (4 * NDS)
        self.drr = {"hw": 0, "sw": 0, "rg": 0, "rh": 0}
        self.nbuf = 0
        self.psum_rot = []
        self.psum_i = 0
        self.ninst = 0

    def sb(self, shape, dtype, name=None, stack=None):
        self.nbuf += 1
        name = f"{name or 'sb'}_{self.nbuf}"
        t = (stack or self.es).enter_context(self.nc.sbuf_tensor(name, list(shape), dtype))
        return Buf(t, name)

    def ps(self, shape, dtype=F32, name=None, stack=None):
        self.nbuf += 1
        name = f"{name or 'ps'}_{self.nbuf}"
        t = (stack or self.es).enter_context(self.nc.psum_tensor(name, list(shape), dtype))
        return Buf(t, name, psum=True)

    def dram(self, name, shape, dtype, kind):
        t = self.nc.dram_tensor(name, list(shape), dtype, kind=kind)
        return Buf(t.ap(), name)

    def _etok(self, e, n):
        idx = (n - 1) // EPOCH
        while len(self.esem[e]) <= idx:
            s = self.es.enter_context(self.nc.semaphore(f"e_{e}_{len(self.esem[e])}"))
            self.esem[e].append(s)
        return Tok(e, self.esem[e][idx], ("e", e, idx), (n - 1) % EPOCH + 1)

    def wait(self, e, tok):
        if tok is None:
            return
        if tok.eng == "pe" and e == "pe":
            return
        if self.seen[e].get(tok.sid, 0) >= tok.val:
            return
        self.eng[e].wait_ge(tok.sem, tok.val)
        self.seen[e][tok.sid] = tok.val

    def issue(self, e, fn, reads, writes):
        toks = []
        for v in reads:
            toks.extend(v.buf.rdeps(v.key))
        for v in writes:
            toks.extend(v.buf.wdeps(v.key))
        for t in toks:
            self.wait(e, t)
        inst = fn()
        self.ecnt[e] += 1
        self.ninst += 1
        tok = self._etok(e, self.ecnt[e])
        inst.then_inc(tok.sem, 1)
        for v in reads:
            v.buf.add_read(v.key, tok)
        for v in writes:
            v.buf.add_write(v.key, tok)
        return tok

    def op(self, e, fname, out, *args, **kw):
        reads = [a for a in args if isinstance(a, V)] + [a for a in kw.values() if isinstance(a, V)]
        cv = lambda a: a.ap if isinstance(a, V) else a
        f = getattr(self.eng[e], fname)
        fn = lambda: f(cv(out), *[cv(a) for a in args], **{k: cv(v) for k, v in kw.items()})
        return self.issue(e, fn, reads, [out])

    def mm(self, out, lhsT, rhs, start=True, stop=True, **kw):
        fn = lambda: self.nc.tensor.matmul(out.ap, lhsT.ap, rhs.ap, start=start, stop=stop, **kw)
        return self.issue("pe", fn, [lhsT, rhs], [out])

    def transpose(self, out, in_, ident):
        fn = lambda: self.nc.tensor.transpose(out.ap, in_.ap, ident.ap)
        return self.issue("pe", fn, [in_, ident], [out])

    def dma(self, q, out, in_, ring=False, **kw):
        toks = list(in_.buf.rdeps(in_.key)) + list(out.buf.wdeps(out.key))
        kind = ("rg" if ring else "sw") if q == "pool" else ("rh" if ring else "hw")
        i = self.drr[kind] + {"hw": 0, "sw": NDS, "rg": 2 * NDS, "rh": 3 * NDS}[kind]
        self.drr[kind] = (self.drr[kind] + 1) % NDS
        if self.dcnt[i] > 0:
            toks.append(Tok("dma", self.dsem[i], ("d", i), 16 * self.dcnt[i]))
        for t in toks:
            self.wait(q, t)
        inst = self.eng[q].dma_start(out=out.ap, in_=in_.ap, **kw)
        self.dcnt[i] += 1
        self.ninst += 1
        inst.then_inc(self.dsem[i], 16)
        tok = Tok("dma", self.dsem[i], ("d", i), 16 * self.dcnt[i])
        in_.buf.add_read(in_.key, tok)
        out.buf.add_write(out.key, tok)
        return tok

    def barrier(self):
        toks = [self._etok(e, self.ecnt[e]) for e in self.eng if self.ecnt[e] > 0]
        toks += [Tok("dma", self.dsem[i], ("d", i), 16 * self.dcnt[i]) for i in range(2 * NDS) if self.dcnt[i] > 0]
        for e in self.eng:
            for t in toks:
                if t.eng == e:
                    continue
                self.wait(e, t)

    def finish(self):
        toks = [Tok("dma", self.dsem[i], ("d", i), 16 * self.dcnt[i]) for i in range(4 * NDS) if self.dcnt[i] > 0]
        toks += [self._etok(e, self.ecnt[e]) for e in self.eng if self.ecnt[e] > 0 and e != "sp"]
        for t in toks:
            self.wait("sp", t)


def run_rr(gens, width):
    pending = list(gens)
    active = []
    while pending or active:
        while pending and len(active) < width:
            active.append(pending.pop(0))
        for g in list(active):
            try:
                next(g)
            except StopIteration:
                active.remove(g)


class WRing:
    def __init__(self, P, nslot, stack):
        self.P = P
        self.n = nslot
        self.slots = [P.sb([128, 8, 512], BF16, f"wr{i}", stack) for i in range(nslot)]
        self.plan = []
        self.issued = 0
        self.got = 0
        self.rel = []

    def add(self, name, loader):
        self.plan.append((name, loader))
        self.rel.append(False)

    def pump(self):
        while self.issued < len(self.plan):
            j = self.issued
            if j >= self.n and not self.rel[j - self.n]:
                break
            self.plan[j][1](self.slots[j % self.n])
            self.issued += 1

    def get(self, name):
        j = self.got
        assert self.plan[j][0] == name, (self.plan[j][0], name)
        self.pump()
        assert self.issued > j, ("weight ring stall", name)
        self.got += 1
        return j, self.slots[j % self.n]

    def release(self, j):
        self.rel[j] = True
        self.pump()


D = 1024
S = 2048
T = 512
NCH = S // T
KC = 8
TB = 128
L = 64
EPS = 1e-6
CDEC = 0.6065306597126334

VOFF = {}
_o = 0
for _n, _w in (("ada_b", 48), ("norm1", 8), ("norm2", 8), ("caw", 124), ("cab", 4), ("lng", 4), ("lnb", 4),
               ("ccw", 32), ("ccb", 8), ("lba", 8), ("lbx", 8), ("lam", 8), ("mu_w", 1), ("mu_a", 1), ("mu_g", 1),
               ("mu_r", 8), ("mu_k", 8), ("mu_v", 8), ("w0", 8), ("a0", 8), ("k_k", 8), ("k_a", 8),
               ("r_k", 8), ("gn_g", 8), ("gn_b", 8)):
    VOFF[_n] = (_o, _w)
    _o += _w
NV = _o
COFF = {"ident": (0, 128), "ones": (128, 128), "mut": (256, 64), "mlt": (320, 64), "mui": (384, 64), "rst": (448, 64)}
NCST = 512


def rwkv_chunk(P, l, c, st, E):
    bk, vec, drvB, cst, identb, onesb = E["bk"], E["vec"], E["drvB"], E["cst"], E["identb"], E["onesb"]
    ring, hT, merged, lor, gup = E["ring"], E["hT"], E["merged"], E["lor"], E["gup"]
    prevq, pextw, pexta, pextg, Sf, Sb = E["prevq"], E["pextw"], E["pexta"], E["pextg"], E["Sf"], E["Sb"]
    MUT, MLT, MUI, IDH, RST, bankT = E["MUT"], E["MLT"], E["MUI"], E["IDH"], E["RST"], E["bankT"]
    src, w_in_ap = E["src_ap_buf"], E["w_in_ap"]
    vcol, drv = E["vcol"], E["drv"]
    w_up_d, a_up_d, g_up_d, proj_b_d = E["w_up_d"], E["a_up_d"], E["g_up_d"], E["proj_b_d"]
    ones = lambda n=128, m=128: cst[0:n, 128:128 + m]
    HB = [64, 8, TB]

    def bc(name, n=TB):
        o, w = VOFF[name]
        b = vec[l]
        return V(b, b.t[0:64, o:o + 8].unsqueeze(2).to_broadcast([64, 8, n]), None)

    def bcd(j0, n=TB):
        b = drvB[l]
        return V(b, b.t[0:64, j0:j0 + 8].unsqueeze(2).to_broadcast([64, 8, n]), None)

    jr, wr_ = ring.get("B_r"); jk, wk_ = ring.get("B_k"); jv_, wv_ = ring.get("B_v"); jl, wl_ = ring.get("B_lora")
    yg = P.sb([64, 8, T], BF16, "b_yg", st)
    F = {n: P.sb(HB, F32, f"b_{n}", st) for n in ("r", "k", "v", "a", "sg", "e1", "e2", "e3", "kk", "t0", "g")}
    F["t1"] = F["sg"]; F["bon"] = F["a"]; F["y"] = F["r"]
    pq = P.sb([64, 8, 1 + TB], F32, "b_pq", st)
    mu3 = {"w": (vcol(l, "mu_w", 0, 1, 64), drv[l][0:64, 32:33]), "a": (vcol(l, "mu_a", 0, 1, 64), drv[l][0:64, 33:34]),
           "g": (vcol(l, "mu_g"), drv[l][:, 34:35])}
    H = {n: P.sb(HB, BF16, f"b_{n}", st) for n in ("rt", "kkt", "bt", "kt", "vb")}
    X = {n: P.sb([64, 512], BF16, f"b_{n}", st) for n in ("XTn", "UT")}
    XJ = []
    for j_ in range(TB // L):
        d_ = {n: P.sb([64, 512], BF16, f"b_{n}{j_}", st) for n in ("Am", "ATm", "NKm", "MBR", "MKR", "A2", "A2T", "A2b", "A2Tb", "Tb", "VT", "bT", "kT")}
        d_["Tf"] = P.sb([64, 512], F32, f"b_Tf{j_}", st)
        XJ.append(d_)
    lt = {n: P.sb([128, TB], F32, f"b_l{n}", st) for n in ("w", "a", "g", "t")}
    lb = {n: P.sb([128, TB], BF16, f"b_lb{n}", st) for n in ("w", "a", "g")}
    colbase = {"r": (wr_, 0), "k": (wk_, 0), "v": (wv_, 0)}
    v4 = lambda b, hh: V(b, b.t[0:64, 0:512].rearrange("p (h t) -> p h t", h=4), None)
    fl = lambda b: V(b, b.t[0:64].rearrange("p h t -> p (h t)"), None)
    hv = lambda b, hh: b[0:64, hh * 4:(hh + 1) * 4, :]

    for blk in range(T // TB):
        t0 = blk * TB
        hs = lambda kc: hT[:, kc, t0:t0 + TB]
        for q in "rkv":
            ws, cb = colbase[q]
            for hh in range(2):
                b_ = bk()
                for hl in range(4):
                    h = hh * 4 + hl
                    for kc in range(KC):
                        P.mm(b_[0:64, hl * TB:(hl + 1) * TB], ws[:, kc, cb + h * 64:cb + (h + 1) * 64], hs(kc), kc == 0, kc == KC - 1)
                P.op("act", "activation", pq[0:64, hh * 4:(hh + 1) * 4, 1:1 + TB], v4(b_, hh), AF.Copy)
            qi = "rkv".index(q)
            P.op("pool", "tensor_copy", pq[0:64, :, 0:1], V(prevq, prevq.t[0:64, qi, :].unsqueeze(2), None))
            mu = {"r": "mu_r", "k": "mu_k", "v": "mu_v"}[q]
            om = {"r": 0, "k": 8, "v": 16}[q]
            eq_ = "dve" if q == "r" else "pool"
            tq_ = F["t0"] if q == "r" else F["kk"]
            P.op(eq_, "tensor_tensor", tq_[:], pq[0:64, :, 0:TB], bc(mu), ALU.mult)
            P.op(eq_, "tensor_tensor", F[q][:], pq[0:64, :, 1:1 + TB], bcd(om), ALU.mult)
            P.op(eq_, "tensor_tensor", F[q][:], F[q][:], tq_[:], ALU.add)
            P.op("act", "activation", V(prevq, prevq.t[0:64, qi, :].unsqueeze(2), None), pq[0:64, :, TB:TB + 1], AF.Copy)
        b_ = bk()
        for kc in range(KC):
            P.mm(b_[0:64, 0:TB], wl_[:, kc, 0:64], hs(kc), kc == 0, kc == KC - 1)
        for kc in range(KC):
            P.mm(b_[0:64, TB:2 * TB], wl_[:, kc, 64:128], hs(kc), kc == 0, kc == KC - 1)
        for kc in range(KC):
            P.mm(b_[:, 2 * TB:3 * TB], wl_[:, kc, 128:256], hs(kc), kc == 0, kc == KC - 1)
        for (pe_, n, off, np_) in ((pextw, "w", 0, 64), (pexta, "a", TB, 64), (pextg, "g", 2 * TB, 128)):
            P.op("act", "activation", pe_[0:np_, 1:1 + TB], b_[0:np_, off:off + TB], AF.Copy)
            P.op("dve", "tensor_scalar", lt["t"][0:np_, :], pe_[0:np_, 0:TB], mu3[n][0], None, ALU.mult)
            P.op("dve", "scalar_tensor_tensor", lt[n][0:np_, :], pe_[0:np_, 1:1 + TB], mu3[n][1], lt["t"][0:np_, :], ALU.mult, ALU.add)
            P.op("act", "activation", pe_[0:np_, 0:1], pe_[0:np_, TB:TB + 1], AF.Copy)
        P.op("act", "activation", lb["w"][0:64, :], lt["w"][0:64, :], AF.Tanh)
        P.op("act", "activation", lb["a"][0:64, :], lt["a"][0:64, :], AF.Copy)
        P.op("act", "activation", lb["g"][:], lt["g"][:], AF.Sigmoid)
        for hh in range(2):
            bw = bk(); ba = bk(); bg = bk()
            for hl in range(4):
                h = hh * 4 + hl
                P.mm(bw[0:64, hl * TB:(hl + 1) * TB], lor[0:64, h * 64:(h + 1) * 64], lb["w"][0:64, :])
                P.mm(ba[0:64, hl * TB:(hl + 1) * TB], lor[0:64, 512 + h * 64:512 + (h + 1) * 64], lb["a"][0:64, :])
                P.mm(bg[0:64, hl * TB:(hl + 1) * TB], gup[:, h * 64:(h + 1) * 64], lb["g"][:])
            o, _ = VOFF["w0"]
            w0b = V(vec[l], vec[l].t[0:64, o + hh * 4:o + hh * 4 + 4].unsqueeze(2).to_broadcast([64, 4, TB]), None)
            o, _ = VOFF["a0"]
            a0b = V(vec[l], vec[l].t[0:64, o + hh * 4:o + hh * 4 + 4].unsqueeze(2).to_broadcast([64, 4, TB]), None)
            P.op("dve", "tensor_tensor", hv(F["sg"], hh), v4(bw, hh), w0b, ALU.add)
            P.op("dve", "tensor_tensor", hv(F["a"], hh), v4(ba, hh), a0b, ALU.add)
            P.op("act", "activation", hv(F["g"], hh), v4(bg, hh), AF.Copy)
        P.op("act", "activation", F["sg"][:], F["sg"][:], AF.Sigmoid)
        P.op("act", "activation", F["a"][:], F["a"][:], AF.Sigmoid)
        P.op("dve", "tensor_tensor_scan", fl(F["e1"]), RST[0:64, :], fl(F["sg"]), 0.0, ALU.mult, ALU.add)
        P.op("dve", "tensor_tensor", F["e3"][:], F["e1"][:], F["sg"][:], ALU.subtract)
        P.op("act", "activation", F["e2"][:], F["e1"][:], AF.Exp, scale=CDEC)
        P.op("act", "activation", F["e1"][:], F["e1"][:], AF.Exp, scale=-CDEC)
        P.op("act", "activation", F["e3"][:], F["e3"][:], AF.Exp, scale=-CDEC)
        P.op("dve", "tensor_tensor", F["kk"][:], F["k"][:], bc("k_k"), ALU.mult)
        P.op("act", "activation", H["rt"][:], F["kk"][:], AF.Square)
        for hh in range(2):
            b_ = bk()
            P.mm(b_[0:64, :], onesb[:], V(H["rt"], H["rt"].t[0:64, hh * 4:(hh + 1) * 4, :].rearrange("p h t -> p (h t)"), None))
            P.op("dve", "tensor_scalar", hv(F["t1"], hh), v4(b_, hh), 1e-24, None, ALU.max)
        P.op("act", "activation", F["t1"][:], F["t1"][:], AF.Ln)
        P.op("act", "activation", F["t1"][:], F["t1"][:], AF.Exp, scale=-0.5)
        P.op("dve", "tensor_tensor", F["kk"][:], F["kk"][:], F["t1"][:], ALU.mult)
        P.op("dve", "tensor_scalar", F["t0"][:], F["a"][:], -1.0, None, ALU.add)
        P.op("dve", "tensor_tensor", F["t0"][:], F["t0"][:], bc("k_a"), ALU.mult)
        P.op("dve", "scalar_tensor_tensor", fl(F["k"]), fl(F["t0"]), 1.0, fl(F["k"]), ALU.add, ALU.mult)
        P.op("dve", "tensor_tensor", H["rt"][:], F["r"][:], F["e1"][:], ALU.mult)
        P.op("pool", "tensor_tensor", H["kkt"][:], F["kk"][:], F["e3"][:], ALU.mult)
        P.op("dve", "tensor_tensor", F["t0"][:], F["kk"][:], F["a"][:], ALU.mult)
        P.op("pool", "tensor_tensor", H["bt"][:], F["t0"][:], F["e2"][:], ALU.mult)
        P.op("dve", "tensor_tensor", H["kt"][:], F["k"][:], F["e2"][:], ALU.mult)
        P.op("act", "activation", H["vb"][:], F["v"][:], AF.Copy)
        P.op("dve", "tensor_tensor", F["t0"][:], F["r"][:], F["k"][:], ALU.mult)
        P.op("dve", "tensor_tensor", F["t0"][:], F["t0"][:], bc("r_k"), ALU.mult)
        for hh in range(2):
            b_ = bk()
            P.mm(b_[0:64, :], ones(64, 64), V(F["t0"], F["t0"].t[0:64, hh * 4:(hh + 1) * 4, :].rearrange("p h t -> p (h t)"), None))
            P.op("dve", "tensor_tensor", hv(F["bon"], hh), v4(b_, hh), hv(F["v"], hh), ALU.mult)
        hm = lambda b, h: b[0:64, h * 64:(h + 1) * 64]

        def nonseq(j):
            sl = slice(j * L, (j + 1) * L)
            Xj = XJ[j]
            for (srcb, dstb, half) in ((H["vb"], Xj["VT"], 0), (H["bt"], Xj["bT"], 1)):
                for h in range(8):
                    P.transpose(bankT[0:64, half * 512 + h * 64:half * 512 + (h + 1) * 64], srcb[0:64, h, sl], identb[0:64, 0:64])
                P.op("act", "activation", dstb[:], bankT[0:64, half * 512:(half + 1) * 512], AF.Copy)
            yield
            for h in range(8):
                P.transpose(bankT[0:64, h * 64:(h + 1) * 64], H["kt"][0:64, h, sl], identb[0:64, 0:64])
            P.op("act", "activation", Xj["kT"][:], bankT[0:64, 0:512], AF.Copy)
            yield
            specs = (("Am", H["bt"], H["kkt"], MUT), ("ATm", H["kkt"], H["bt"], MLT), ("NKm", H["kt"], H["kkt"], MUT),
                     ("MBR", H["bt"], H["rt"], MUI), ("MKR", H["kt"], H["rt"], MUI))
            pend = []
            for (nm, lh, rh, msk) in specs[:2]:
                b_ = bk()
                for h in range(8):
                    P.mm(hm(b_, h), lh[0:64, h, sl], rh[0:64, h, sl])
                pend.append((nm, b_, msk))
            yield
            for (nm, b_, msk) in pend:
                P.op("dve", "tensor_tensor", Xj[nm][:], b_[0:64, :], msk[:], ALU.mult)
            P.op("dve", "tensor_tensor", Xj["Tf"][:], IDH[:], Xj["Am"][:], ALU.subtract)
            P.op("act", "activation", Xj["Tb"][:], Xj["Tf"][:], AF.Copy)
            pend = []
            for (nm, lh, rh, msk) in specs[2:]:
                b_ = bk()
                for h in range(8):
                    P.mm(hm(b_, h), lh[0:64, h, sl], rh[0:64, h, sl])
                pend.append((nm, b_, msk))
            yield
            for (nm, b_, msk) in pend:
                P.op("dve", "tensor_tensor", Xj[nm][:], b_[0:64, :], msk[:], ALU.mult)
            Ak, AkT = Xj["Am"], Xj["ATm"]
            for lev in range(5):
                b2t = bk()
                for h in range(8):
                    P.mm(hm(b2t, h), hm(Ak, h), hm(AkT, h))
                if lev < 4:
                    b2 = bk()
                    for h in range(8):
                        P.mm(hm(b2, h), hm(AkT, h), hm(Ak, h))
                yield
                n2, n2t = ("A2", "A2T") if lev % 2 == 0 else ("A2b", "A2Tb")
                P.op("act", "activation", Xj[n2t][:], b2t[0:64, :], AF.Copy)
                if lev < 4:
                    P.op("dve", "tensor_copy", Xj[n2][:], b2[0:64, :])
                yield
                btp = bk()
                for h in range(8):
                    P.mm(hm(btp, h), hm(Xj[n2t], h), hm(Xj["Tb"], h))
                yield
                P.op("dve", "tensor_tensor", Xj["Tf"][:], Xj["Tf"][:], btp[0:64, :], ALU.add)
                P.op("act", "activation", Xj["Tb"][:], Xj["Tf"][:], AF.Copy)
                Ak, AkT = Xj[n2], Xj[n2t]

        def seq(j):
            sl = slice(j * L, (j + 1) * L)
            Xj = XJ[j]
            bx = bk()
            for h in range(8):
                P.mm(hm(bx, h), H["kkt"][0:64, h, sl], hm(Sb, h), True, False)
                P.mm(hm(bx, h), hm(Xj["NKm"], h), hm(Xj["VT"], h), False, True)
            P.op("act", "activation", X["XTn"][:], bx[0:64, :], AF.Identity, scale=-1.0)
            bu = bk()
            for h in range(8):
                P.mm(hm(bu, h), hm(Xj["Tb"], h), hm(X["XTn"], h))
            P.op("act", "activation", X["UT"][:], bu[0:64, :], AF.Copy)
            by = bk()
            for h in range(8):
                P.mm(hm(by, h), hm(Sb, h), H["rt"][0:64, h, sl], True, False)
                P.mm(hm(by, h), hm(X["UT"], h), hm(Xj["MBR"], h), False, False)
                P.mm(hm(by, h), hm(Xj["VT"], h), hm(Xj["MKR"], h), False, True)
            P.op("act", "activation", F["y"][0:64, :, sl], V(by, by.t[0:64, 0:512].rearrange("p (h t) -> p h t", h=8), None), AF.Copy)
            bs_ = bk()
            for h in range(8):
                P.mm(hm(bs_, h), hm(Xj["bT"], h), hm(X["UT"], h), True, False)
                P.mm(hm(bs_, h), hm(Xj["kT"], h), hm(Xj["VT"], h), False, True)
            P.op("dve", "tensor_tensor", Sf[:], Sf[:], bs_[0:64, :], ALU.add)
            pl = V(F["e1"], F["e1"].t[0:64, :, j * L + L - 1:j * L + L].to_broadcast([64, 8, 64]), None)
            P.op("dve", "tensor_tensor", V(Sf, Sf.t[0:64, :].rearrange("p (h v) -> p h v", h=8), None),
                 V(Sf, Sf.t[0:64, :].rearrange("p (h v) -> p h v", h=8), None), pl, ALU.mult)
            P.op("act", "activation", Sb[:], Sf[:], AF.Copy)

        run_rr([nonseq(j) for j in range(TB // L)], 2)
        for j in range(TB // L):
            seq(j)
        P.op("act", "activation", H["rt"][:], F["y"][:], AF.Square)
        P.op("act", "activation", H["kkt"][:], F["y"][:], AF.Copy)
        for hh in range(2):
            b1 = bk(); b2_ = bk()
            P.mm(b1[0:64, :], onesb[:], V(H["kkt"], H["kkt"].t[0:64, hh * 4:(hh + 1) * 4, :].rearrange("p h t -> p (h t)"), None))
            P.mm(b2_[0:64, :], onesb[:], V(H["rt"], H["rt"].t[0:64, hh * 4:(hh + 1) * 4, :].rearrange("p h t -> p (h t)"), None))
            P.op("act", "activation", hv(F["t1"], hh), v4(b1, hh), AF.Identity, scale=1.0 / 64)
            P.op("act", "activation", hv(F["e2"], hh), v4(b2_, hh), AF.Identity, scale=1.0 / 64)
        P.op("dve", "tensor_tensor", F["e3"][:], F["t1"][:], F["t1"][:], ALU.mult)
        P.op("dve", "tensor_tensor", F["e2"][:], F["e2"][:], F["e3"][:], ALU.subtract)
        P.op("act", "activation", F["e2"][:], F["e2"][:], AF.Ln, bias=64e-5)
        P.op("act", "activation", F["e2"][:], F["e2"][:], AF.Exp, scale=-0.5)
        P.op("dve", "tensor_tensor", F["y"][:], F["y"][:], F["t1"][:], ALU.subtract)
        P.op("dve", "tensor_tensor", F["y"][:], F["y"][:], F["e2"][:], ALU.mult)
        P.op("dve", "tensor_tensor", F["y"][:], F["y"][:], bc("gn_g"), ALU.mult)
        P.op("dve", "tensor_tensor", F["y"][:], F["y"][:], bc("gn_b"), ALU.add)
        P.op("dve", "tensor_tensor", F["y"][:], F["y"][:], F["bon"][:], ALU.add)
        P.op("dve", "tensor_tensor", yg[0:64, :, t0:t0 + TB], F["y"][:], F["g"][:], ALU.mult)
    for j_ in (jr, jk, jv_, jl):
        ring.release(j_)
    E["proj_and_gate"](l, st, ring, "B", 8, 64, lambda k: yg[0:64, k, :], True, hT, merged, False)


def build(dbg=(), stop=None, nlayers=2):
    P = Prog()
    nc = P.nc

    def din(name, shape):
        return P.dram(name, shape, F32, "ExternalInput")

    xT_d = din("xT", [D, S]); cT_d = din("cT", [128, 8]); vec_d = din("vec", [2, 128, NV])
    fn_d = din("fnorm", [128, 8]); rb_d = din("rbias", [2, 128, 64]); cst_d = din("cst", [128, NCST])
    ada_w_d = din("ada_w", [2, D, 6 * D]); w_in_d = din("w_in", [2, D, 7936])
    proj_a_d = din("proj_a", [2, 512, D]); proj_b_d = din("proj_b", [2, 512, D]); proj_c_d = din("proj_c", [2, D, D])
    w_out_d = din("w_out", [2, D, D]); w_up_d = din("w_up", [2, 64, 512]); a_up_d = din("a_up", [2, 64, 512])
    g_up_d = din("g_up", [2, 128, 512]); lwa_d = din("lru_wa", [2, 8, 128, 128]); lwx_d = din("lru_wx", [2, 8, 128, 128])
    rw_d = din("router_w", [2, D, 64]); e1_d = din("exp_w1", [2, 64, D, 256]); e3_d = din("exp_w3", [2, 64, D, 256])
    e2_d = din("exp_w2", [2, 64, 256, D]); s1_d = din("sh_w1", [2, D, 256]); s3_d = din("sh_w3", [2, D, 256])
    s2_d = din("sh_w2", [2, 256, D])
    out_d = P.dram("outT", [D, S], F32, "ExternalOutput")
    dbg_d = {n: P.dram("dbg_" + n, [D, S], F32, "ExternalOutput") for n in dbg}

    xs = P.dram("xs_scratch", [D, S], F32, "Internal")
    cst = P.sb([128, NCST], F32, "cst")
    vec = [P.sb([128, NV], F32, f"vec{l}") for l in range(2)]
    modT = [P.sb([128, 48], F32, f"modT{l}") for l in range(2)]
    drv = [P.sb([128, 40], F32, f"drv{l}") for l in range(2)]
    drvB = [P.sb([64, 24], F32, f"drvB{l}") for l in range(2)]
    identb = P.sb([128, 128], BF16, "identb")
    onesb = P.sb([64, 64], BF16, "onesb")
    onesb128 = P.sb([128, 128], BF16, "onesb128")
    MUT = P.sb([64, 512], BF16, "MUT"); MLT = P.sb([64, 512], BF16, "MLT"); MUI = P.sb([64, 512], BF16, "MUI")
    IDH = P.sb([64, 512], BF16, "IDH"); RST = P.sb([64, 8 * TB], BF16, "RST")
    banks = [P.ps([128, 512], F32, f"bank{i}") for i in range(8)]
    bankT = Buf(banks[7].t.bitcast(BF16), "bankT", psum=True)
    rot = [0]

    def bk():
        b = banks[rot[0] % 7]
        rot[0] += 1
        return b

    ident = lambda n=128: cst[0:n, 0:n]
    ones = lambda n=128, m=128: cst[0:n, 128:128 + m]

    def vcol(l, name, j=0, n=1, parts=128):
        o, w = VOFF[name]
        return vec[l][0:parts, o + j:o + j + n]

    def ld_w(dst, src_ap, q="pool"):
        return P.dma(q, dst, V(src_ap_buf, src_ap, None))

    src_ap_buf = Buf(None, "wsrc")

    def dump(name, src_fn):
        if name in dbg_d:
            for kc in range(KC):
                P.dma("sp", dbg_d[name].view(dbg_d[name].t[kc * 128:(kc + 1) * 128, :]), src_fn(kc))

    P.dma("sp", cst[:], cst_d[:])
    P.dma("sp", xs[:], xT_d[:])
    for l in range(2):
        P.dma("sp", vec[l][:], vec_d.view(vec_d.t[l]))
    P.op("dve", "tensor_copy", identb[:], cst[:, 0:128])
    P.op("dve", "tensor_copy", onesb[:], cst[0:64, 128:192])
    P.op("dve", "tensor_copy", onesb128[:], cst[:, 128:256])
    for h in range(8):
        for (dst, nm) in ((MUT, "mut"), (MLT, "mlt"), (MUI, "mui"), (IDH, "ident")):
            o = COFF[nm][0]
            P.op("dve", "tensor_copy", dst[0:64, h * 64:(h + 1) * 64], cst[0:64, o:o + 64])
    for j in range(8 * TB // 64):
        P.op("dve", "tensor_copy", RST[0:64, j * 64:(j + 1) * 64], cst[0:64, 448:512])

    with ExitStack() as st:
        cT = P.sb([128, 8], F32, "cT", st); cond2 = P.sb([128, 8, 2], F32, "cond2", st)
        aw = [P.sb([128, KC, 512], F32, f"aw{i}", st) for i in range(2)]
        P.dma("sp", cT[:], cT_d[:])
        P.op("act", "activation", cond2[:, :, 0], cT[:], AF.Silu)
        P.op("act", "activation", cond2[:, :, 1], cT[:], AF.Silu)
        for l in range(nlayers):
            pm = bk()
            for g in range(12):
                a = aw[g % 2]
                P.dma("sp", a[:], V(src_ap_buf, ada_w_d.t[l][:, g * 512:(g + 1) * 512].rearrange("(k p) n -> p k n", p=128), None))
                for j in range(4):
                    col = (g * 4 + j) * 2
                    for kc in range(KC):
                        P.mm(pm[:, col:col + 2], a[:, kc, j * 128:(j + 1) * 128], cond2[:, kc, :], kc == 0, kc == KC - 1)
            P.op("dve", "tensor_tensor", modT[l][:], V(pm, pm.t[:, 0:96].rearrange("p (j two) -> p j two", two=2)[:, :, 0], None),
                 vcol(l, "ada_b", 0, 48), ALU.add)
            P.op("dve", "tensor_scalar", drv[l][:, 0:8], modT[l][:, 8:16], 1.0, None, ALU.add)
            P.op("dve", "tensor_tensor", drv[l][:, 0:8], drv[l][:, 0:8], vcol(l, "norm1", 0, 8), ALU.mult)
            P.op("dve", "tensor_scalar", drv[l][:, 8:16], modT[l][:, 32:40], 1.0, None, ALU.add)
            P.op("dve", "tensor_tensor", drv[l][:, 8:16], drv[l][:, 8:16], vcol(l, "norm2", 0, 8), ALU.mult)
            P.op("act", "activation", drv[l][:, 16:24], vcol(l, "lam", 0, 8), AF.Exp, scale=-1.0)
            P.op("act", "activation", drv[l][:, 16:24], drv[l][:, 16:24], AF.Ln, bias=1.0)
            P.op("dve", "tensor_scalar", drv[l][:, 24:32], drv[l][:, 16:24], -16.0, None, ALU.mult)
            P.op("dve", "tensor_scalar", drv[l][:, 16:24], drv[l][:, 16:24], -8.0, None, ALU.mult)
            P.op("dve", "tensor_scalar", drv[l][:, 32:35], vcol(l, "mu_w", 0, 3), -1.0, 1.0, ALU.mult, ALU.add)
            P.op("dve", "tensor_scalar", drvB[l][0:64, 0:24], vcol(l, "mu_r", 0, 24, 64), -1.0, 1.0, ALU.mult, ALU.add)
        P.barrier()

    def rmsnorm_mod(xsrc, gm, sh, dst_fn, st):
        sq = [P.sb([128, T], F32, f"nsq{i}", st) for i in range(2)]
        sqb = [P.sb([128, T], BF16, f"nsqb{i}", st) for i in range(2)]
        rs = P.sb([128, T], F32, "nrs", st)
        ss = bk()
        for kc in range(KC):
            q = sqb[kc % 2]
            P.op("act", "activation", q[:], xsrc(kc), AF.Square)
            P.mm(ss[:], onesb128[:], q[:], kc == 0, kc == KC - 1)
        P.op("act", "activation", rs[:], ss[:], AF.Ln, scale=1.0 / D, bias=EPS)
        P.op("act", "activation", rs[:], rs[:], AF.Exp, scale=-0.5)
        for kc in range(KC):
            q = sq[kc % 2]
            P.op("dve", "tensor_tensor", q[:], xsrc(kc), rs[:], ALU.mult)
            if gm is None:
                P.op("act", "activation", dst_fn(kc), q[:], AF.Identity, scale=sh(kc))
            else:
                P.op("act", "activation", dst_fn(kc), q[:], AF.Identity, scale=gm(kc), bias=sh(kc))

    GB = 4864

    def dump_merged(l, cs, merged):
        n = f"mg{l}"
        if n in dbg_d:
            for kc in range(KC):
                P.dma("sp", dbg_d[n].view(dbg_d[n].t[kc * 128:(kc + 1) * 128, cs]), merged[:, kc, :])
            P.barrier()


    def w_in_ap(l, c0, n):
        return w_in_d.t[l][:, c0:c0 + n].rearrange("(k p) n -> p k n", p=128)

    def proj_and_gate(l, st, ring, mname, nk, np_, rhs_fn, split, hT, merged, first):
        gt = [P.sb([128, T], F32, f"pg_gt{i}", st) for i in range(2)]
        tmp = [P.sb([128, T], F32, f"pg_tmp{i}", st) for i in range(2)]
        pj = pp = None
        for half in range(2):
            if split or half == 0:
                if pj is not None:
                    ring.release(pj)
                pj, pp = ring.get(f"{mname}_proj{half}" if split else f"{mname}_proj")
            gj, gp = ring.get(f"{mname}_g{half}")
            for ocl in range(4):
                oc = half * 4 + ocl
                pso = bk(); psg = bk()
                for k in range(nk):
                    if split:
                        lh = pp[0:np_, k, ocl * 128:(ocl + 1) * 128]
                    else:
                        lh = V(pp, pp.t[0:np_].rearrange("p k n -> p (k n)")[:, k * 1024 + oc * 128:k * 1024 + (oc + 1) * 128], None)
                    P.mm(pso[:], lh, rhs_fn(k), k == 0, k == nk - 1)
                for kc in range(KC):
                    P.mm(psg[:], gp[:, kc, ocl * 128:(ocl + 1) * 128], hT[:, kc, :], kc == 0, kc == KC - 1)
                g = gt[oc % 2]
                P.op("act", "activation", g[:], psg[:], AF.Sigmoid)
                if first:
                    P.op("dve", "tensor_tensor", merged.k(oc)[:, oc, :], pso[:], g[:], ALU.mult)
                else:
                    t_ = tmp[oc % 2]
                    P.op("dve", "tensor_tensor", t_[:], pso[:], g[:], ALU.mult)
                    P.op("pool", "tensor_tensor", merged.k(oc)[:, oc, :], merged.k(oc)[:, oc, :], t_[:], ALU.add)
            ring.release(gj)
        ring.release(pj)

    wcache = P.dram("wcache", [23, 128, 4096], BF16, "Internal")

    def plan_chunk(ring, l, c):
        full = lambda slot: (slot, slot.t[:].rearrange("p k n -> p (k n)"), 4096, 128)
        pieces = []

        def add(name, sv, src):
            pieces.append((name, sv, src))

        wl = lambda c0, n: ((lambda slot: (slot.t[:, :, 0:n], 128, 8, n)), w_in_ap(l, c0, n))
        kp = lambda d, h: ((lambda slot: (slot.t[:, :, :], 128, 8, 512)), d.t[l][:, h * 512:(h + 1) * 512].rearrange("(k p) n -> p k n", p=128))
        pbp = lambda h: ((lambda slot: (slot.t[0:64, :, :], 64, 8, 512)), proj_b_d.t[l][:, h * 512:(h + 1) * 512].rearrange("(hh i) n -> i hh n", i=64))
        add("A_val", *wl(0, 512)); add("A_sig", *wl(512, 512))
        add("A_proj", (lambda slot: (slot.t[:].rearrange("p k n -> p (k n)").rearrange("p (k n) -> p k n", k=4), 128, 4, 1024)),
            proj_a_d.t[l].rearrange("(k p) n -> p k n", p=128))
        add("A_g0", *wl(GB, 512)); add("A_g1", *wl(GB + 512, 512))
        add("C_y0", *wl(2816, 512)); add("C_x0", *wl(3840, 512)); add("C_y1", *wl(2816 + 512, 512)); add("C_x1", *wl(3840 + 512, 512))
        add("C_proj0", *kp(proj_c_d, 0)); add("C_g0", *wl(GB + 2048, 512))
        add("C_proj1", *kp(proj_c_d, 1)); add("C_g1", *wl(GB + 2048 + 512, 512))
        add("B_r", *wl(1024, 512)); add("B_k", *wl(1536, 512)); add("B_v", *wl(2048, 512)); add("B_lora", *wl(2560, 256))
        add("B_proj0", *pbp(0)); add("B_g0", *wl(GB + 1024, 512))
        add("B_proj1", *pbp(1)); add("B_g1", *wl(GB + 1024 + 512, 512))
        add("O_0", *kp(w_out_d, 0)); add("O_1", *kp(w_out_d, 1))
        for idx, (name, sv, src) in enumerate(pieces):
            def loader(slot, idx=idx, sv=sv, src=src):
                ap, np_, a, b = sv(slot)
                cv = wcache.t[idx, 0:np_, 0:a * b].rearrange("p (k n) -> p k n", k=a)
                if c == 0:
                    P.dma("pool", V(slot, ap, None), V(src_ap_buf, src, None), ring=True)
                    P.dma("sp", V(wcache, cv, idx), V(slot, ap, None), ring=True)
                else:
                    P.dma("sp", V(slot, ap, None), V(wcache, cv, idx), ring=True)
            ring.add(name, loader)

    for l in range(nlayers):
        with ExitStack() as lst:
            uext = P.sb([128, 4, 30 + T], BF16, "uext", lst)
            xh = P.sb([128, 8, 3], BF16, "xh", lst)
            hC = P.sb([128, 8], F32, "hC", lst)
            prevq = P.sb([64, 3, 8], F32, "prevq", lst)
            pextw = P.sb([64, 1 + TB], F32, "pextw", lst); pexta = P.sb([64, 1 + TB], F32, "pexta", lst)
            pextg = P.sb([128, 1 + TB], F32, "pextg", lst)
            Sf = P.sb([64, 512], F32, "Sf", lst); Sb = P.sb([64, 512], BF16, "Sb", lst)
            lw = P.sb([128, 2, 8, 128], BF16, "c_lw", lst)
            P.dma("pool", lw[:, 0], V(src_ap_buf, lwa_d.t[l].rearrange("h i j -> i h j"), None))
            P.dma("pool", lw[:, 1], V(src_ap_buf, lwx_d.t[l].rearrange("h i j -> i h j"), None))
            lor = P.sb([128, 1024], BF16, "b_lor", lst)
            gup = P.sb([128, 512], BF16, "b_gup", lst)
            P.dma("pool", lor[0:64, 0:512], V(src_ap_buf, w_up_d.t[l], None))
            P.dma("pool", lor[0:64, 512:1024], V(src_ap_buf, a_up_d.t[l], None))
            P.dma("pool", gup[:], V(src_ap_buf, g_up_d.t[l], None))
            P.op("pool", "memset", uext[:, :, 0:30], 0.0)
            P.op("pool", "memset", xh[:], 0.0)
            P.op("pool", "memset", hC[:], 0.0)
            P.op("pool", "memset", prevq[:], 0.0)
            P.op("pool", "memset", pextw[:, 0:1], 0.0); P.op("pool", "memset", pexta[:, 0:1], 0.0)
            P.op("pool", "memset", pextg[:, 0:1], 0.0)
            P.op("pool", "memset", Sf[:], 0.0); P.op("pool", "memset", Sb[:], 0.0)
            mc = lambda j, n=1, l=l: modT[l][:, j:j + n]

            with ExitStack() as mst:
                ring = WRing(P, 5, mst)
                for _c in range(NCH):
                    plan_chunk(ring, l, _c)
                ring.pump()
                hT = P.sb([128, KC, T], BF16, "hT", mst)
                merged = P.sb([128, KC, T], F32, "merged", mst)
                for c in range(NCH):
                    cs = slice(c * T, (c + 1) * T)
                    with ExitStack() as st:
                        xch = P.sb([128, KC, T], F32, "xch", st)
                        P.dma("sp", xch[:], xs.view(xs.t[:, cs].rearrange("(k p) t -> p k t", p=128)))
                        rmsnorm_mod(lambda kc: xch[:, kc, :], lambda kc: drv[l][:, kc:kc + 1], lambda kc: mc(kc), lambda kc: hT[:, kc, :], st)
                        P.barrier()
                    if f"h{l}" in dbg_d:
                        with ExitStack() as st:
                            tf = P.sb([128, KC, T], F32, "dbgtf", st)
                            P.op("dve", "tensor_copy", tf[:], hT[:])
                            for kc in range(KC):
                                P.dma("sp", dbg_d[f"h{l}"].view(dbg_d[f"h{l}"].t[kc * 128:(kc + 1) * 128, cs]), tf[:, kc, :])
                            P.barrier()
                    with ExitStack() as st:
                        jv, wv = ring.get("A_val"); jg, wg = ring.get("A_sig")
                        sg = [P.sb([128, T], F32, f"a_sg{i}", st) for i in range(2)]
                        cv = P.sb([128, 4, T], F32, "a_cv", st)
                        dg = P.sb([128, 31, 128], BF16, "a_dg", st)
                        sA = P.sb([128, 4, T], BF16, "a_sA", st)
                        dgs = [dg, P.sb([128, 31, 128], BF16, "a_dg2", st)]

                        def a_in(pc):
                            psv = bk(); psg = bk()
                            for kc in range(KC):
                                P.mm(psv[:], wv[:, kc, pc * 128:(pc + 1) * 128], hT[:, kc, :], kc == 0, kc == KC - 1)
                            for kc in range(KC):
                                P.mm(psg[:], wg[:, kc, pc * 128:(pc + 1) * 128], hT[:, kc, :], kc == 0, kc == KC - 1)
                            yield
                            g = sg[pc % 2]
                            P.op("act", "activation", g[:], psg[:], AF.Sigmoid)
                            yield
                            P.op("dve", "tensor_tensor", uext.k(pc)[:, pc, 30:30 + T], psv[:], g[:], ALU.mult)

                        def a_conv(pc):
                            d_ = dgs[pc % 2]
                            for j in range(31):
                                P.op("dve" if j % 2 else "pool", "tensor_scalar", d_.k(j)[:, j, :], identb[:], vcol(l, "caw", pc * 31 + j), None, ALU.mult)
                            yield
                            psc = bk()
                            for j in range(31):
                                P.mm(psc[:], d_.k(j)[:, j, :], uext.k(pc)[:, pc, j:j + T], j == 0, j == 30)
                            yield
                            P.op("act", "activation", cv.k(pc)[:, pc, :], psc[:], AF.Identity, bias=vcol(l, "cab", pc))

                        run_rr([a_in(pc) for pc in range(4)], 2)
                        ring.release(jv); ring.release(jg)
                        run_rr([a_conv(pc) for pc in range(4)], 2)
                        P.op("dve", "tensor_copy", uext[:, :, 0:30], uext[:, :, T:T + 30])
                        pss = bk(); pss2 = bk()
                        for pc in range(4):
                            q = sg[pc % 2]
                            P.mm(pss[:], ones(), cv.k(pc)[:, pc, :], pc == 0, pc == 3)
                            P.op("act", "activation", q[:], cv.k(pc)[:, pc, :], AF.Square)
                            P.mm(pss2[:], ones(), q[:], pc == 0, pc == 3)
                        mean = P.sb([128, T], F32, "a_mean", st); rstd = P.sb([128, T], F32, "a_rstd", st)
                        P.op("act", "activation", mean[:], pss[:], AF.Identity, scale=1.0 / 512)
                        P.op("dve", "tensor_tensor", rstd[:], mean[:], mean[:], ALU.mult)
                        P.op("dve", "scalar_tensor_tensor", rstd[:], pss2[:], 1.0 / 512, rstd[:], ALU.mult, ALU.subtract)
                        P.op("act", "activation", rstd[:], rstd[:], AF.Ln, bias=1e-5)
                        P.op("act", "activation", rstd[:], rstd[:], AF.Exp, scale=-0.5)
                        for pc in range(4):
                            q = sg[pc % 2]
                            P.op("dve", "tensor_tensor", q[:], cv.k(pc)[:, pc, :], mean[:], ALU.subtract)
                            P.op("dve", "tensor_tensor", q[:], q[:], rstd[:], ALU.mult)
                            P.op("act", "activation", sA.k(pc)[:, pc, :], q[:], AF.Silu, scale=vcol(l, "lng", pc), bias=vcol(l, "lnb", pc))
                        proj_and_gate(l, st, ring, "A", 4, 128, lambda k: sA.k(k)[:, k, :], False, hT, merged, True)
                        P.barrier()
                    if stop == "A":
                        dump_merged(l, cs, merged)
                        continue
                    with ExitStack() as st:
                        hy = P.sb([128, 8, T], BF16, "c_hy", st)
                        CW = 3
                        xe = [P.sb([128, 3 + T], BF16, f"c_xe{i}", st) for i in range(CW)]
                        dgcs = [P.sb([128, 4, 128], BF16, f"c_dg{i}", st) for i in range(CW)]
                        W = {n: [P.sb([128, T], F32, f"c_{n}{i}", st) for i in range(CW)] for n in ("yg", "t1", "t2", "xc", "ga", "gx", "ys")}
                        xcb = [P.sb([128, T], BF16, f"c_xcb{i}", st) for i in range(CW)]
                        cpieces = {}

                        def c_pc(pc):
                            i2 = pc % CW
                            dgc = dgcs[i2]
                            if pc % 4 == 0:
                                cpieces[pc // 4] = (ring.get(f"C_y{pc // 4}"), ring.get(f"C_x{pc // 4}"))
                            (jy, wy), (jx, wx) = cpieces[pc // 4]
                            pcl = pc % 4
                            psy = bk(); psx = bk()
                            for kc in range(KC):
                                P.mm(psy[:], wy[:, kc, pcl * 128:(pcl + 1) * 128], hT[:, kc, :], kc == 0, kc == KC - 1)
                            for kc in range(KC):
                                P.mm(psx[:], wx[:, kc, pcl * 128:(pcl + 1) * 128], hT[:, kc, :], kc == 0, kc == KC - 1)
                            if pcl == 3:
                                ring.release(jy); ring.release(jx)
                            for j in range(4):
                                P.op("pool", "tensor_scalar", dgc.k(j)[:, j, :], identb[:], vcol(l, "ccw", pc * 4 + j), None, ALU.mult)
                            P.op("pool", "tensor_copy", xe[i2][:, 0:3], xh.k(pc)[:, pc, :])
                            yield
                            yg, t1, t2 = W["yg"][i2], W["t1"][i2], W["t2"][i2]
                            ys = W["ys"][i2]
                            P.op("act", "activation", t1[:], psy[:], AF.Square)
                            P.op("dve", "tensor_copy", ys[:], psy[:])
                            P.op("act", "activation", xe[i2][:, 3:3 + T], psx[:], AF.Copy)
                            yield
                            P.op("dve", "tensor_scalar", t1[:], t1[:], 0.044715, 1.0, ALU.mult, ALU.add)
                            psc = bk()
                            for j in range(4):
                                P.mm(psc[:], dgc.k(j)[:, j, :], xe[i2][:, j:j + T], j == 0, j == 3)
                            P.op("pool", "tensor_copy", xh.k(pc)[:, pc, :], xe[i2][:, T:T + 3])
                            yield
                            P.op("dve", "tensor_tensor", t1[:], t1[:], ys[:], ALU.mult)
                            xc = W["xc"][i2]
                            P.op("act", "activation", xc[:], psc[:], AF.Identity, bias=vcol(l, "ccb", pc))
                            yield
                            P.op("act", "activation", t1[:], t1[:], AF.Sigmoid, scale=1.5957691216057308)
                            P.op("dve", "tensor_copy", xcb[i2][:], xc[:])
                            yield
                            P.op("dve", "tensor_tensor", yg[:], t1[:], ys[:], ALU.mult)
                            psa = bk(); psb = bk()
                            P.mm(psa[:], lw[:, 0, pc, :], xcb[i2][:])
                            P.mm(psb[:], lw[:, 1, pc, :], xcb[i2][:])
                            yield
                            ga, gx = W["ga"][i2], W["gx"][i2]
                            P.op("act", "activation", ga[:], psa[:], AF.Sigmoid, bias=vcol(l, "lba", pc))
                            P.op("act", "activation", gx[:], psb[:], AF.Sigmoid, bias=vcol(l, "lbx", pc))
                            yield
                            P.op("act", "activation", t2[:], ga[:], AF.Exp, scale=drv[l][:, 24 + pc:25 + pc])
                            P.op("dve", "tensor_tensor", gx[:], gx[:], xc[:], ALU.mult)
                            yield
                            P.op("act", "activation", t2[:], t2[:], AF.Sqrt, scale=-1.0, bias=1.0)
                            P.op("act", "activation", ga[:], ga[:], AF.Exp, scale=drv[l][:, 16 + pc:17 + pc])
                            yield
                            P.op("dve", "tensor_tensor", gx[:], gx[:], t2[:], ALU.mult)
                            yield
                            P.op("dve", "tensor_tensor_scan", t2[:], ga[:], gx[:], hC.k(pc)[:, pc:pc + 1], ALU.mult, ALU.add)
                            yield
                            P.op("act", "activation", hC.k(pc)[:, pc:pc + 1], t2[:, T - 1:T], AF.Copy)
                            P.op("dve", "tensor_tensor", hy.k(pc)[:, pc, :], t2[:], yg[:], ALU.mult)

                        run_rr([c_pc(pc) for pc in range(8)], CW)
                        proj_and_gate(l, st, ring, "C", 8, 128, lambda k: hy.k(k)[:, k, :], True, hT, merged, False)
                        P.barrier()
                    if stop == "C":
                        dump_merged(l, cs, merged)
                        continue
                    with ExitStack() as st:
                        E = dict(bk=bk, vec=vec, drvB=drvB, cst=cst, identb=identb, onesb=onesb, ring=ring, lor=lor, gup=gup, hT=hT, merged=merged, prevq=prevq,
                                 pextw=pextw, pexta=pexta, pextg=pextg, Sf=Sf, Sb=Sb, MUT=MUT, MLT=MLT, MUI=MUI, IDH=IDH, RST=RST,
                                 bankT=bankT, src_ap_buf=src_ap_buf, w_in_ap=w_in_ap, vcol=vcol, drv=drv, w_up_d=w_up_d, a_up_d=a_up_d,
                                 g_up_d=g_up_d, proj_b_d=proj_b_d, proj_and_gate=proj_and_gate)
                        rwkv_chunk(P, l, c, st, E)
                        P.barrier()
                    dump_merged(l, cs, merged)
                    with ExitStack() as st:
                        xch = P.sb([128, KC, T], F32, "xch2", st)
                        P.dma("sp", xch[:], xs.view(xs.t[:, cs].rearrange("(k p) t -> p k t", p=128)))
                        mergedb = P.sb([128, KC, T], BF16, "mergedb", st)
                        for kc in range(KC):
                            P.op("act", "activation", mergedb.k(kc)[:, kc, :], merged.k(kc)[:, kc, :], AF.Copy)
                        for oc in range(8):
                            if oc % 4 == 0:
                                if oc:
                                    ring.release(jo)
                                jo, wo = ring.get(f"O_{oc // 4}")
                            ocl = oc % 4
                            pso = bk()
                            for kc in range(KC):
                                P.mm(pso[:], wo[:, kc, ocl * 128:(ocl + 1) * 128], mergedb.k(kc)[:, kc, :], kc == 0, kc == KC - 1)
                            P.op("dve", "scalar_tensor_tensor", xch.k(oc)[:, oc, :], pso[:], mc(16 + oc), xch.k(oc)[:, oc, :], ALU.mult, ALU.add)
                        ring.release(jo)
                        P.dma("sp", xs.view(xs.t[:, cs].rearrange("(k p) t -> p k t", p=128)), xch[:])
                        P.barrier()
                P.barrier()
            dump(f"x1_{l}", lambda kc: xs.view(xs.t[kc * 128:(kc + 1) * 128, :]))
            if stop in ("A", "C", "mix"):
                P.barrier()
                continue
            with ExitStack() as mst:
                h2 = P.sb([128, KC, S], BF16, "h2", mst)
                xT = P.sb([128, KC, S], F32, "xT", mst)
                cwT = P.sb([64, 2, S], BF16, "cwT", mst)
                rwb = P.sb([128, KC, 64], BF16, "rwb", mst)
                rbias = P.sb([128, 64], F32, "rbias", mst)
                P.dma("pool", rwb[:], V(src_ap_buf, rw_d.t[l].rearrange("(k p) n -> p k n", p=128), None))
                P.dma("sp", rbias[:], rb_d.view(rb_d.t[l]))
                for kc in range(KC):
                    P.dma("sp", xT.k(kc)[:, kc, :], xs.view(xs.t[kc * 128:(kc + 1) * 128, :]))
                for c in range(NCH):
                    cs = slice(c * T, (c + 1) * T)
                    with ExitStack() as st:
                        rmsnorm_mod(lambda kc: xT.k(kc)[:, kc, cs], lambda kc: drv[l][:, 8 + kc:9 + kc], lambda kc: mc(24 + kc), lambda kc: h2.k((kc, c))[:, kc, cs], st)
                        R = {n: P.sb([128, 64], F32, f"r_{n}", st) for n in ("sc", "bs", "b2", "mk", "sel")}
                        r8 = {n: P.sb([128, 8], F32, f"r8_{n}", st) for n in ("m1", "m2", "o8", "gm", "o8b", "ss")}
                        for tt in range(4):
                            ts_ = slice(c * T + tt * 128, c * T + (tt + 1) * 128)
                            psl = bk()
                            for kc in range(KC):
                                P.mm(psl[:, 0:64], h2.k((kc, c))[:, kc, ts_], rwb[:, kc, :], kc == 0, kc == KC - 1)
                            v3 = lambda b: V(b, b.t[:, :].rearrange("p (g e) -> p g e", e=8), None)
                            bc8 = lambda b: V(b, b.t[:, 0:8].unsqueeze(2).to_broadcast([128, 8, 8]), None)
                            P.op("act", "activation", R["sc"][:], psl[:, 0:64], AF.Sigmoid)
                            P.op("dve", "tensor_tensor", R["bs"][:], R["sc"][:], rbias[:], ALU.add)
                            P.op("dve", "tensor_reduce", r8["m1"][:], v3(R["bs"]), mybir.AxisListType.X, ALU.max)
                            P.op("dve", "tensor_tensor", v3(R["b2"]), v3(R["bs"]), bc8(r8["m1"]), ALU.is_equal)
                            P.op("dve", "scalar_tensor_tensor", R["b2"][:], R["b2"][:], -1e9, R["bs"][:], ALU.mult, ALU.add)
                            P.op("dve", "tensor_reduce", r8["m2"][:], v3(R["b2"]), mybir.AxisListType.X, ALU.max)
                            P.op("dve", "tensor_tensor", r8["m1"][:], r8["m1"][:], r8["m2"][:], ALU.add)
                            P.op("dve", "max", r8["o8"][:], r8["m1"][:])
                            P.op("dve", "tensor_scalar", r8["gm"][:], r8["m1"][:], r8["o8"][:, 3:4], None, ALU.is_ge)
                            P.op("dve", "tensor_scalar", r8["gm"][:], r8["gm"][:], -1.0, 1e9, ALU.add, ALU.mult)
                            P.op("dve", "tensor_tensor", v3(R["mk"]), v3(R["bs"]), bc8(r8["gm"]), ALU.add)
                            P.op("dve", "max", r8["o8b"][:], R["mk"][:])
                            P.op("dve", "tensor_scalar", R["sel"][:], R["mk"][:], r8["o8b"][:, 7:8], None, ALU.is_ge)
                            P.op("dve", "tensor_tensor", R["sel"][:], R["sel"][:], R["sc"][:], ALU.mult)
                            P.op("dve", "tensor_reduce", r8["ss"][:, 0:1], R["sel"][:], mybir.AxisListType.X, ALU.add)
                            P.op("dve", "reciprocal", r8["ss"][:, 0:1], r8["ss"][:, 0:1])
                            P.op("dve", "tensor_scalar", R["sel"][:], R["sel"][:], r8["ss"][:, 0:1], 2.5, ALU.mult, ALU.mult)
                            pst = bk()
                            P.transpose(pst[0:64, 0:128], R["sel"][:], ident())
                            P.op("act", "activation", cwT.k(c * 4 + tt)[0:64, 0, ts_], pst[0:64, 0:128], AF.Copy)
                            P.op("dve", "tensor_tensor", cwT.k(c * 4 + tt)[0:64, 1, ts_], pst[0:64, 0:128], cwT.k(c * 4 + tt)[0:64, 0, ts_], ALU.subtract)
                        P.barrier()
                if f"h2_{l}" in dbg_d:
                    with ExitStack() as st:
                        tf = P.sb([128, S], F32, "dbgtf2", st)
                        for kc in range(KC):
                            P.op("dve", "tensor_copy", tf[:], h2[:, kc, :])
                            P.dma("sp", dbg_d[f"h2_{l}"].view(dbg_d[f"h2_{l}"].t[kc * 128:(kc + 1) * 128, :]), tf[:])
                        P.barrier()
                if f"cw{l}" in dbg_d:
                    pass
                with ExitStack() as st:
                    EW = [P.sb([128, 6144], BF16, f"ew{i}", st) for i in range(2)]
                    SEL = [P.sb([64, 128], BF16, f"sel{i}", st) for i in range(2)]
                    cwb = [P.sb([128, T], F32, f"cwb{i}", st) for i in range(2)]
                    sa = [P.sb([128, T], F32, f"m_sa{i}", st) for i in range(2)]
                    gT = [P.sb([128, 2, T], BF16, f"m_gT{i}", st) for i in range(2)]
                    NE = 65 if stop != "noexp" else 0
                    mb = banks
                    porot = [0]

                    def ew_views(e):
                        ew = EW[e % 2]
                        w1t = lambda k, f: V(ew, ew.t[:, k * 256 + f * 128:k * 256 + (f + 1) * 128], None)
                        w3t = lambda k, f: V(ew, ew.t[:, 2048 + k * 256 + f * 128:2048 + k * 256 + (f + 1) * 128], None)
                        w2t = lambda f, oc: V(ew, ew.t[:, 4096 + f * 1024 + oc * 128:4096 + f * 1024 + (oc + 1) * 128], None)
                        return ew, w1t, w3t, w2t

                    def load_expert(e):
                        ew = EW[e % 2]
                        w1 = V(ew, ew.t[:, 0:2048].rearrange("p (k n) -> p k n", k=8), None)
                        w3 = V(ew, ew.t[:, 2048:4096].rearrange("p (k n) -> p k n", k=8), None)
                        w2 = V(ew, ew.t[:, 4096:6144].rearrange("p (k n) -> p k n", k=2), None)
                        if e < 64:
                            s1, s3, s2 = e1_d.t[l][e], e3_d.t[l][e], e2_d.t[l][e]
                        else:
                            s1, s3, s2 = s1_d.t[l], s3_d.t[l], s2_d.t[l]
                        P.dma("pool", w1, V(src_ap_buf, s1.rearrange("(k p) n -> p k n", p=128), None))
                        P.dma("pool", w3, V(src_ap_buf, s3.rearrange("(k p) n -> p k n", p=128), None))
                        P.dma("pool", w2, V(src_ap_buf, s2.rearrange("(k p) n -> p k n", p=128), None))

                    def stage1(i):
                        e, c = divmod(i, NCH)
                        cs = slice(c * T, (c + 1) * T)
                        ew, w1t, w3t, w2t = ew_views(e)
                        i2 = i % 2
                        if c == 0:
                            if e < 64:
                                P.op("dve", "tensor_scalar", SEL[e % 2][:], ones(64, 128), cst[0:64, e:e + 1], None, ALU.mult)
                        if e < 64:
                            P.mm(mb[0][:], SEL[e % 2][:], cwT[0:64, 0, cs], True, False)
                            P.mm(mb[0][:], SEL[e % 2][:], cwT[0:64, 1, cs], False, True)
                            P.op("act", "activation", cwb[i2][:], mb[0][:], AF.Copy)
                        for f in range(2):
                            pa = mb[1 + 2 * f]; pb = mb[2 + 2 * f]
                            for kc in range(KC):
                                P.mm(pa[:], w1t(kc, f), h2[:, kc, cs], kc == 0, kc == KC - 1)
                            for kc in range(KC):
                                P.mm(pb[:], w3t(kc, f), h2[:, kc, cs], kc == 0, kc == KC - 1)
                            s_ = sa[f]
                            P.op("act", "activation", s_[:], pa[:], AF.Silu)
                            if e < 64:
                                P.op("dve", "tensor_tensor", s_[:], s_[:], pb[:], ALU.mult)
                                P.op("pool", "tensor_tensor", gT[i2].k(f)[:, f, :], s_[:], cwb[i2][:], ALU.mult)
                            else:
                                P.op("dve", "tensor_tensor", gT[i2].k(f)[:, f, :], s_[:], pb[:], ALU.mult)

                    def stage2(i):
                        e, c = divmod(i, NCH)
                        cs = slice(c * T, (c + 1) * T)
                        ew, w1t, w3t, w2t = ew_views(e)
                        i2 = i % 2
                        for oc in range(8):
                            po = mb[5 + porot[0] % 3]
                            porot[0] += 1
                            for f in range(2):
                                P.mm(po[:], w2t(f, oc), gT[i2].k(f)[:, f, :], f == 0, f == 1)
                            P.op("dve", "scalar_tensor_tensor", xT.k(oc)[:, oc, cs], po[:], mc(40 + oc), xT.k(oc)[:, oc, cs], ALU.mult, ALU.add)

                    NI = NE * NCH
                    if NI:
                        load_expert(0)
                        load_expert(1)
                        stage1(0)
                    for i in range(NI):
                        if i + 1 < NI:
                            stage1(i + 1)
                        stage2(i)
                        if i % NCH == NCH - 1 and i // NCH + 2 < NE:
                            load_expert(i // NCH + 2)
                    P.barrier()
                for kc in range(KC):
                    P.dma("sp", xs.view(xs.t[kc * 128:(kc + 1) * 128, :]), xT.k(kc)[:, kc, :])
                P.barrier()
            dump(f"x2_{l}", lambda kc: xs.view(xs.t[kc * 128:(kc + 1) * 128, :]))
            P.barrier()

    with ExitStack() as st:
        fn = P.sb([128, 8], F32, "fn", st)
        ob = [P.sb([128, T], F32, f"ob{i}", st) for i in range(4)]
        P.dma("sp", fn[:], fn_d[:])
        cnt = [0]
        for c in range(NCH):
            cs = slice(c * T, (c + 1) * T)
            with ExitStack() as st2:
                def dst(kc):
                    return ob[kc % 4][:]
                xf = P.sb([128, KC, T], F32, "xfin", st2)
                P.dma("sp", xf[:], xs.view(xs.t[:, cs].rearrange("(k p) t -> p k t", p=128)))
                sq = [P.sb([128, T], F32, f"fsq{i}", st2) for i in range(2)]
                sqb = [P.sb([128, T], BF16, f"fsqb{i}", st2) for i in range(2)]
                rs = P.sb([128, T], F32, "frs", st2)
                ss = bk()
                for kc in range(KC):
                    q = sqb[kc % 2]
                    P.op("act", "activation", q[:], xf[:, kc, :], AF.Square)
                    P.mm(ss[:], onesb128[:], q[:], kc == 0, kc == KC - 1)
                P.op("act", "activation", rs[:], ss[:], AF.Ln, scale=1.0 / D, bias=EPS)
                P.op("act", "activation", rs[:], rs[:], AF.Exp, scale=-0.5)
                for kc in range(KC):
                    q = sq[kc % 2]
                    o = ob[kc % 4]
                    P.op("dve", "tensor_tensor", q[:], xf[:, kc, :], rs[:], ALU.mult)
                    P.op("act", "activation", o[:], q[:], AF.Identity, scale=fn[:, kc:kc + 1])
                    P.dma("sp", out_d.view(out_d.t[kc * 128:(kc + 1) * 128, cs]), o[:])
                P.barrier()
    P.finish()
    return P


def _pack_vec(inp, l):
    v = np.zeros((128, NV), np.float32)

    def put(name, arr):
        o, w = VOFF[name]
        arr = np.asarray(arr, np.float32)
        assert arr.shape[1] == w, (name, arr.shape)
        v[:arr.shape[0], o:o + w] = arr

    c128 = lambda a, n: np.asarray(a).reshape(n, 128).T
    c64 = lambda a: np.asarray(a).reshape(8, 64).T
    put("ada_b", c128(inp["ada_b"][l], 48)); put("norm1", c128(inp["norm1"][l], 8)); put("norm2", c128(inp["norm2"][l], 8))
    put("caw", np.asarray(inp["conv_a_w"][l]).T.reshape(4, 128, 31).transpose(1, 0, 2).reshape(128, 124))
    put("cab", c128(inp["conv_a_b"][l], 4)); put("lng", c128(inp["ln_a_g"][l], 4)); put("lnb", c128(inp["ln_a_b"][l], 4))
    put("ccw", np.asarray(inp["conv_c_w"][l]).T.reshape(8, 128, 4).transpose(1, 0, 2).reshape(128, 32))
    put("ccb", c128(inp["conv_c_b"][l], 8)); put("lba", c128(inp["lru_ba"][l], 8)); put("lbx", c128(inp["lru_bx"][l], 8))
    put("lam", c128(inp["lru_lambda"][l], 8))
    mu = np.asarray(inp["mu_b"][l])
    put("mu_w", mu[1536:1600].reshape(64, 1)); put("mu_a", mu[1600:1664].reshape(64, 1)); put("mu_g", mu[1664:1792].reshape(128, 1))
    put("mu_r", c64(mu[0:512])); put("mu_k", c64(mu[512:1024])); put("mu_v", c64(mu[1024:1536]))
    for n, k in (("w0", "w0"), ("a0", "a0"), ("k_k", "k_k"), ("k_a", "k_a"), ("gn_g", "gn_b_g"), ("gn_b", "gn_b_b")):
        put(n, c64(inp[k][l]))
    put("r_k", np.asarray(inp["r_k"][l]).T)
    return v


def _consts():
    c = np.zeros((128, NCST), np.float32)
    c[:, 0:128] = np.eye(128, dtype=np.float32)
    c[:, 128:256] = 1.0
    s = np.arange(64)[:, None]; t = np.arange(64)[None, :]
    c[0:64, 256:320] = (s < t); c[0:64, 320:384] = (s > t); c[0:64, 384:448] = (s <= t)
    c[:, 448:512] = 1.0; c[:, 448] = 0.0
    return c


_PROG_CACHE = {}


def _get_prog(dbg=(), stop=None, nlayers=2):
    key = (tuple(dbg), stop, nlayers)
    if key not in _PROG_CACHE:
        _PROG_CACHE[key] = build(dbg, stop, nlayers)
    return _PROG_CACHE[key]


def _in_maps(inp, cores):
    f = lambda a: np.ascontiguousarray(np.asarray(a, np.float32))
    vec = np.stack([_pack_vec(inp, l) for l in range(2)])
    shared = {
        "vec": vec, "fnorm": f(np.asarray(inp["final_norm"]).reshape(8, 128).T),
        "rbias": f(np.broadcast_to(np.asarray(inp["router_bias"])[:, None, :], (2, 128, 64))), "cst": _consts(),
        "ada_w": f(inp["ada_w"]), "w_in": f(inp["w_in"]), "proj_a": f(inp["proj_a"]), "proj_b": f(inp["proj_b"]),
        "proj_c": f(inp["proj_c"]), "w_out": f(inp["w_out"]), "w_up": f(inp["w_up"]), "a_up": f(inp["a_up"]),
        "g_up": f(inp["g_up"]), "lru_wa": f(inp["lru_wa"]), "lru_wx": f(inp["lru_wx"]), "router_w": f(inp["router_w"]),
        "exp_w1": f(inp["exp_w1"]), "exp_w3": f(inp["exp_w3"]), "exp_w2": f(inp["exp_w2"]),
        "sh_w1": f(inp["sh_w1"]), "sh_w3": f(inp["sh_w3"]), "sh_w2": f(inp["sh_w2"]),
    }
    maps = []
    for b in cores:
        m = dict(shared)
        m["xT"] = f(np.asarray(inp["x"][b]).T)
        m["cT"] = f(np.asarray(inp["c"][b]).reshape(8, 128).T)
        maps.append(m)
    return maps


def kernel(**inputs):
    P = _get_prog()
    maps = _in_maps(inputs, range(8))
    res = run_bass_kernel_spmd(P.nc, maps, core_ids=list(range(8)))
    out = np.stack([np.asarray(r["outT"]).T for r in res.results]).astype(np.float32)
    return out
```
